# Optimizing a Trainium2 kernel written in Bass

```python
import jax, jax.numpy as jnp
from jax import lax
import numpy as np

D_MODEL = 1024
BATCH = 16
SEQ = 2048
DEPTH = 4

N_MIXERS = 2
N_ML_LAYERS = (DEPTH + 1) // 2
N_NSA_LAYERS = DEPTH // 2
RMS_EPS = 1e-6
LN_EPS = 1e-6
NEG_INF = -1e30

ML_INNER = 2 * D_MODEL
ML_HEADS = 4
ML_DH = ML_INNER // ML_HEADS
ML_CONV = 4
ML_QKV_BLK = 4
ML_NBLK = ML_INNER // ML_QKV_BLK
ML_CHUNK = 64

NSA_HEADS = 16
NSA_GROUPS = 4
NSA_HPG = NSA_HEADS // NSA_GROUPS
NSA_DK = 64
NSA_DV = 64
NSA_INNER = NSA_HEADS * NSA_DV
NSA_PROJ = NSA_HEADS * NSA_DK + 6 * NSA_GROUPS * NSA_DK + NSA_INNER + 3 * NSA_HEADS
ROPE_DIM = NSA_DK // 4
ROPE_THETA = 500000.0
CMP_BLOCK = 32
CMP_STRIDE = 16
CMP_HIDDEN = 2 * NSA_DK
SEL_BLOCK = 64
SEL_TOPK = 16
SEL_LOCAL = 2
SEL_FORCE = 1e9
SEL_Q_BLOCK = 16
WINDOW = 512
WIN_Q_BLOCK = 128

kernel_name = 'mlstm_nsa_interleaved_hybrid'


def rmsnorm(x, g):
    xf = x.astype(jnp.float32)
    y = xf * lax.rsqrt(jnp.mean(xf * xf, axis=-1, keepdims=True) + RMS_EPS)
    return y.astype(x.dtype) * g


def rope_partial(t, cos, sin):
    half = ROPE_DIM // 2
    shape = (t.shape[1],) + (1,) * (t.ndim - 3) + (half,)
    c = cos.reshape(shape).astype(t.dtype)
    s = sin.reshape(shape).astype(t.dtype)
    t1, t2, rest = t[..., :half], t[..., half:ROPE_DIM], t[..., ROPE_DIM:]
    return jnp.concatenate([t1 * c - t2 * s, t2 * c + t1 * s, rest], axis=-1)


def masked_softmax(s, mask):
    s = jnp.where(mask, s.astype(jnp.float32), NEG_INF)
    return jax.nn.softmax(s, axis=-1)


def mlstm_chunkwise(q, k, v, i_pre, log_f):
    B, NH, S, DH = q.shape
    nc = S // ML_CHUNK

    def to_chunks(t):
        return jnp.moveaxis(t.reshape(t.shape[:2] + (nc, ML_CHUNK) + t.shape[3:]), 2, 0)

    causal = jnp.tril(jnp.ones((ML_CHUNK, ML_CHUNK), dtype=bool))

    def step(carry, xs):
        C, n, m = carry
        qc, kc, vc, ic, fc = xs
        b = jnp.cumsum(fc, axis=-1)
        dmat = jnp.where(causal, b[..., :, None] - b[..., None, :] + ic[..., None, :], -jnp.inf)
        inter = b + m[..., None]
        m_row = jnp.maximum(inter, dmat.max(-1))
        w_intra = jnp.exp(dmat - m_row[..., None])
        w_inter = jnp.exp(inter - m_row)
        s = jnp.einsum('bhid,bhjd->bhij', qc, kc) * w_intra
        num = jnp.einsum('bhij,bhjd->bhid', s, vc) + w_inter[..., None] * jnp.einsum('bhvk,bhik->bhiv', C, qc)
        den = s.sum(-1) + w_inter * jnp.einsum('bhk,bhik->bhi', n, qc)
        h = num / jnp.maximum(jnp.abs(den), jnp.exp(-m_row))[..., None]
        b_last = b[..., -1]
        a = b_last[..., None] - b + ic
        m_new = jnp.maximum(b_last + m, a.max(-1))
        w_state = jnp.exp(a - m_new[..., None])
        decay = jnp.exp(b_last + m - m_new)
        C = decay[..., None, None] * C + jnp.einsum('bhjv,bhjk->bhvk', vc * w_state[..., None], kc)
        n = decay[..., None] * n + jnp.einsum('bhj,bhjk->bhk', w_state, kc)
        return (C, n, m_new), h

    init = (jnp.zeros((B, NH, DH, DH), jnp.float32),
            jnp.zeros((B, NH, DH), jnp.float32),
            jnp.zeros((B, NH), jnp.float32))
    _, h = lax.scan(step, init, tuple(to_chunks(t) for t in (q, k, v, i_pre, log_f)))
    return jnp.moveaxis(h, 0, 2).reshape(B, NH, S, DH)


def mlstm_mixer(h, w_in, conv_w, conv_b, w_q, w_k, w_v, w_if, b_if, ln_w, skip, w_out):
    B, S, _ = h.shape
    x_in, z = jnp.split(h @ w_in, 2, axis=-1)
    x_pad = jnp.pad(x_in, ((0, 0), (ML_CONV - 1, 0), (0, 0)))
    conv = sum(x_pad[:, tap:tap + S] * conv_w[tap] for tap in range(ML_CONV))
    x_conv = jax.nn.silu(conv + conv_b)

    def headwise(t, w):
        return jnp.einsum('bsnj,nij->bsni', t.reshape(B, S, ML_NBLK, ML_QKV_BLK), w).reshape(B, S, ML_INNER)

    q = headwise(x_conv, w_q)
    k = headwise(x_conv, w_k)
    v = headwise(x_in, w_v)
    gates = (jnp.concatenate([q, k, v], axis=-1) @ w_if + b_if).astype(jnp.float32)
    i_pre, f_pre = jnp.split(gates, 2, axis=-1)

    def heads(t):
        return t.reshape(B, S, ML_HEADS, ML_DH).transpose(0, 2, 1, 3).astype(jnp.float32)

    hh = mlstm_chunkwise(heads(q) * ML_DH ** -0.5, heads(k), heads(v),
                         i_pre.transpose(0, 2, 1), jax.nn.log_sigmoid(f_pre).transpose(0, 2, 1))
    mu = hh.mean(-1, keepdims=True)
    var = jnp.mean(jnp.square(hh - mu), axis=-1, keepdims=True)
    hn = ((hh - mu) * lax.rsqrt(var + LN_EPS)).transpose(0, 2, 1, 3).reshape(B, S, ML_INNER)
    hn = hn.astype(h.dtype) * ln_w
    return ((hn + skip * x_conv) * jax.nn.silu(z)) @ w_out


def nsa_mixer(h, w_in, b_gate, cmp_pe, cmp_w1, cmp_w2, w_out, cos, sin):
    B, S, _ = h.shape
    G, HPG, DK, DV = NSA_GROUPS, NSA_HPG, NSA_DK, NSA_DV
    sizes = [NSA_HEADS * DK] + [G * DK] * 6 + [NSA_INNER]
    q, k_c, v_c, k_s, v_s, k_w, v_w, z, g_logit = jnp.split(h @ w_in, np.cumsum(sizes).tolist(), axis=-1)
    q = q.reshape(B, S, G, HPG, DK)
    k_c, v_c, k_s, v_s, k_w, v_w = (t.reshape(B, S, G, DK) for t in (k_c, v_c, k_s, v_s, k_w, v_w))
    scale = DK ** -0.5
    pos = jnp.arange(S)

    n_cmp = (S - CMP_BLOCK) // CMP_STRIDE + 1
    cmp_start = jnp.arange(n_cmp) * CMP_STRIDE
    tok_idx = cmp_start[:, None] + jnp.arange(CMP_BLOCK)[None, :]

    def compress(t, pe, w1, w2):
        blocks = t[:, tok_idx] + pe[:, None, :]
        flat = blocks.transpose(0, 1, 3, 2, 4).reshape(B, n_cmp, G, CMP_BLOCK * DK)
        return jax.nn.silu(flat @ w1) @ w2

    k_cmp = compress(k_c, cmp_pe[0], cmp_w1[0], cmp_w2[0])
    v_cmp = compress(v_c, cmp_pe[1], cmp_w1[1], cmp_w2[1])
    cmp_mask = (cmp_start + CMP_BLOCK - 1)[None, :] <= pos[:, None]
    p_cmp = masked_softmax(jnp.einsum('bsghd,bcgd->bghsc', q, k_cmp) * scale, cmp_mask)
    p_cmp = p_cmp * cmp_mask.any(-1)[:, None].astype(jnp.float32)
    o_cmp = jnp.einsum('bghsc,bcgd->bsghd', p_cmp.astype(v_cmp.dtype), v_cmp)

    q_r = rope_partial(q, cos, sin)
    k_s = rope_partial(k_s, cos, sin)
    k_w = rope_partial(k_w, cos, sin)

    n_sel = S // SEL_BLOCK
    sel_start = jnp.arange(n_sel) * SEL_BLOCK
    overlap = jnp.clip(jnp.minimum(cmp_start[:, None] + CMP_BLOCK, sel_start[None, :] + SEL_BLOCK)
                       - jnp.maximum(cmp_start[:, None], sel_start[None, :]), 0, None)
    overlap = overlap.astype(jnp.float32) / CMP_STRIDE
    importance = jnp.einsum('bghsc,cn->bgsn', p_cmp, overlap)
    blk = jnp.arange(n_sel)
    dist = (pos // SEL_BLOCK)[:, None] - blk[None, :]
    forced = (blk[None, :] == 0) | ((dist >= 0) & (dist < SEL_LOCAL))
    importance = jnp.where(forced, SEL_FORCE, jnp.where(dist >= 0, importance, -1.0))
    top_k = min(SEL_TOPK, n_sel)
    top_val, top_idx = lax.top_k(importance, top_k)
    ks_blk = k_s.transpose(0, 2, 1, 3).reshape(B, G, n_sel, SEL_BLOCK, DK)
    vs_blk = v_s.transpose(0, 2, 1, 3).reshape(B, G, n_sel, SEL_BLOCK, DV)
    QB = SEL_Q_BLOCK
    n_qb = S // QB
    bi = jnp.arange(B)[:, None, None]
    gi = jnp.arange(G)[None, :, None]

    def sel_step(args):
        qb, idx, ok, qpos = args
        flat = idx.reshape(B, G, QB * top_k)
        kg = ks_blk[bi, gi, flat].reshape(B, G, QB, top_k * SEL_BLOCK, DK)
        vg = vs_blk[bi, gi, flat].reshape(B, G, QB, top_k * SEL_BLOCK, DV)
        kpos = idx[..., None] * SEL_BLOCK + jnp.arange(SEL_BLOCK)
        mask = (ok[..., None] & (kpos <= qpos[:, None, None])).reshape(B, G, 1, QB, top_k * SEL_BLOCK)
        p = masked_softmax(jnp.einsum('bqghd,bgqkd->bghqk', qb, kg) * scale, mask)
        return jnp.einsum('bghqk,bgqkd->bqghd', p.astype(vg.dtype), vg)

    xs = (jnp.moveaxis(q_r.reshape(B, n_qb, QB, G, HPG, DK), 1, 0),
          jnp.moveaxis(top_idx.reshape(B, G, n_qb, QB, top_k), 2, 0),
          jnp.moveaxis((top_val >= 0).reshape(B, G, n_qb, QB, top_k), 2, 0),
          pos.reshape(n_qb, QB))
    o_sel = jnp.moveaxis(lax.map(sel_step, xs), 0, 1).reshape(B, S, G, HPG, DV)

    k_wp = jnp.pad(k_w, ((0, 0), (WINDOW, 0), (0, 0), (0, 0)))
    v_wp = jnp.pad(v_w, ((0, 0), (WINDOW, 0), (0, 0), (0, 0)))
    span = WIN_Q_BLOCK + WINDOW

    def win_step(start):
        qb = lax.dynamic_slice_in_dim(q_r, start, WIN_Q_BLOCK, axis=1)
        kb = lax.dynamic_slice_in_dim(k_wp, start, span, axis=1)
        vb = lax.dynamic_slice_in_dim(v_wp, start, span, axis=1)
        kpos = start - WINDOW + jnp.arange(span)
        diff = (start + jnp.arange(WIN_Q_BLOCK))[:, None] - kpos[None, :]
        mask = (diff >= 0) & (diff < WINDOW) & (kpos >= 0)[None, :]
        p = masked_softmax(jnp.einsum('bqghd,bkgd->bghqk', qb, kb) * scale, mask)
        return jnp.einsum('bghqk,bkgd->bqghd', p.astype(vb.dtype), vb)

    o_win = lax.map(win_step, jnp.arange(S // WIN_Q_BLOCK) * WIN_Q_BLOCK)
    o_win = jnp.moveaxis(o_win, 0, 1).reshape(B, S, G, HPG, DV)

    gate = jax.nn.sigmoid((g_logit + b_gate).astype(jnp.float32)).reshape(B, S, NSA_HEADS, 3).astype(h.dtype)

    def heads(o):
        return o.reshape(B, S, NSA_HEADS, DV)

    o = gate[..., 0:1] * heads(o_cmp) + gate[..., 1:2] * heads(o_sel) + gate[..., 2:3] * heads(o_win)
    return (o.reshape(B, S, NSA_INNER) * jax.nn.silu(z)) @ w_out


def setup_inputs(seed: int = 0) -> dict:
    key = jax.random.key(seed)
    ks = jax.random.split(key, 24)
    f32 = jnp.float32

    def nrm(k, shape, fan):
        return jax.random.normal(k, shape, f32) * fan ** -0.5

    def gain(k, shape):
        return 1.0 + 0.02 * jax.random.normal(k, shape, f32)

    def small(k, shape, s=0.02):
        return s * jax.random.normal(k, shape, f32)

    LM, LN = N_ML_LAYERS, N_NSA_LAYERS
    b_if = jnp.concatenate([small(ks[10], (LM, ML_HEADS), 0.1),
                            jnp.linspace(3.0, 6.0, ML_HEADS, dtype=f32)[None, :]
                            + small(ks[11], (LM, ML_HEADS), 0.01)], axis=-1)
    return {
        'x': jax.random.normal(ks[0], (BATCH, SEQ, D_MODEL), f32),
        'ml_norm': gain(ks[1], (LM, D_MODEL)),
        'ml_w_in': nrm(ks[2], (LM, D_MODEL, 2 * ML_INNER), D_MODEL),
        'ml_conv_w': nrm(ks[3], (LM, ML_CONV, ML_INNER), ML_CONV),
        'ml_conv_b': small(ks[4], (LM, ML_INNER)),
        'ml_w_q': nrm(ks[5], (LM, ML_NBLK, ML_QKV_BLK, ML_QKV_BLK), ML_QKV_BLK),
        'ml_w_k': nrm(ks[6], (LM, ML_NBLK, ML_QKV_BLK, ML_QKV_BLK), ML_QKV_BLK),
        'ml_w_v': nrm(ks[7], (LM, ML_NBLK, ML_QKV_BLK, ML_QKV_BLK), ML_QKV_BLK),
        'ml_w_if': nrm(ks[8], (LM, 3 * ML_INNER, 2 * ML_HEADS), 3 * ML_INNER),
        'ml_b_if': b_if,
        'ml_ln_w': gain(ks[12], (LM, ML_INNER)),
        'ml_skip': gain(ks[13], (LM, ML_INNER)),
        'ml_w_out': nrm(ks[14], (LM, ML_INNER, D_MODEL), ML_INNER),
        'nsa_norm': gain(ks[15], (LN, D_MODEL)),
        'nsa_w_in': nrm(ks[16], (LN, D_MODEL, NSA_PROJ), D_MODEL),
        'nsa_b_gate': small(ks[17], (LN, 3 * NSA_HEADS)),
        'nsa_cmp_pe': small(ks[18], (LN, 2, CMP_BLOCK, NSA_DK), 0.1),
        'nsa_cmp_w1': nrm(ks[19], (LN, 2, CMP_BLOCK * NSA_DK, CMP_HIDDEN), CMP_BLOCK * NSA_DK),
        'nsa_cmp_w2': nrm(ks[20], (LN, 2, CMP_HIDDEN, NSA_DK), CMP_HIDDEN),
        'nsa_w_out': nrm(ks[21], (LN, NSA_INNER, D_MODEL), NSA_INNER),
        'final_norm': gain(ks[22], (D_MODEL,)),
    }


def reference(x, ml_norm, ml_w_in, ml_conv_w, ml_conv_b, ml_w_q, ml_w_k, ml_w_v, ml_w_if, ml_b_if,
              ml_ln_w, ml_skip, ml_w_out, nsa_norm, nsa_w_in, nsa_b_gate, nsa_cmp_pe, nsa_cmp_w1,
              nsa_cmp_w2, nsa_w_out, final_norm):
    S = x.shape[1]
    pos_f = jnp.arange(S, dtype=jnp.float32)
    inv_freq = ROPE_THETA ** (-jnp.arange(0, ROPE_DIM, 2, dtype=jnp.float32) / ROPE_DIM)
    ang = pos_f[:, None] * inv_freq[None, :]
    cos, sin = jnp.cos(ang), jnp.sin(ang)
    for i in range(DEPTH):
        j = i // N_MIXERS
        if i % N_MIXERS == 0:
            x = x + mlstm_mixer(rmsnorm(x, ml_norm[j]), ml_w_in[j], ml_conv_w[j], ml_conv_b[j],
                                ml_w_q[j], ml_w_k[j], ml_w_v[j], ml_w_if[j], ml_b_if[j],
                                ml_ln_w[j], ml_skip[j], ml_w_out[j])
        else:
            x = x + nsa_mixer(rmsnorm(x, nsa_norm[j]), nsa_w_in[j], nsa_b_gate[j], nsa_cmp_pe[j],
                              nsa_cmp_w1[j], nsa_cmp_w2[j], nsa_w_out[j], cos, sin)
    return rmsnorm(x, final_norm)
```

```python
from contextlib import ExitStack
import numpy as np
import concourse.bass as bass
import concourse.mybir as mybir
from concourse.bass_utils import run_bass_kernel_spmd

F32 = mybir.dt.float32
BF16 = mybir.dt.bfloat16
AF = mybir.ActivationFunctionType
ALU = mybir.AluOpType
AX = mybir.AxisListType

S = 2048
D = 1024
NT = S // 128
NSEQ = 2
NCORES = 8
ML_INNER = 2048
NH = 4
DH = 512
RMS_EPS = 1e-6
LN_EPS = 1e-6
NEG = -30000.0


class Tok:
    __slots__ = ("name", "w", "r", "x")

    def __init__(self, name="", x=False):
        self.name = name
        self.w = None
        self.r = {}
        self.x = x


class Prog:
    ENG = ("pe", "dve", "act", "pool", "sp")

    def __init__(self, nc, n_dma_ring=12):
        self.nc = nc
        self.q = {e: [] for e in self.ENG}
        self.sems = {}
        self.cnt = {}
        for e in self.ENG:
            self.sems[e] = nc.alloc_semaphore(name=f"c_{e}")
            self.cnt[e] = 0
        self.ring = {}
        self.ring_pos = {}
        for qn in ("sp", "act", "pool"):
            ks = []
            for i in range(n_dma_ring):
                k = f"d_{qn}{i}"
                self.sems[k] = nc.alloc_semaphore(name=k)
                self.cnt[k] = 0
                ks.append(k)
            self.ring[qn] = ks
            self.ring_pos[qn] = 0
        self.seen = {e: {} for e in self.ENG}
        self.n_ins = 0

    def _deps(self, r, w):
        deps = {}

        def add(k, v):
            if deps.get(k, 0) < v:
                deps[k] = v
        for t in r:
            if t.w is not None:
                add(*t.w)
            if t.x:
                for k, v in t.r.items():
                    add(k, v)
        for t in w:
            if t.w is not None:
                add(*t.w)
            for k, v in t.r.items():
                add(k, v)
        return deps

    def _emit_waits(self, e, deps):
        seen = self.seen[e]
        for k, v in deps.items():
            if k == e and e == "pe":
                continue
            if seen.get(k, 0) >= v:
                continue
            seen[k] = v
            self.q[e].append(("w", self.sems[k], v))

    limit = None

    def op(self, e, fn, r=(), w=()):
        if self.limit is not None and self.n_ins >= self.limit:
            return
        self._emit_waits(e, self._deps(r, w))
        self.cnt[e] += 1
        c = self.cnt[e]
        self.q[e].append(("i", fn, self.sems[e]))
        self.n_ins += 1
        for t in w:
            t.w = (e, c)
            t.r = {}
        for t in r:
            if t.r.get(e, 0) < c:
                t.r[e] = c

    def dma(self, qn, out, in_, r=(), w=()):
        if self.limit is not None and self.n_ins >= self.limit:
            return
        deps = self._deps(r, w)
        k = self.ring[qn][self.ring_pos[qn]]
        self.ring_pos[qn] = (self.ring_pos[qn] + 1) % len(self.ring[qn])
        if self.cnt[k] > 0 and deps.get(k, 0) < self.cnt[k]:
            deps[k] = self.cnt[k]
        self._emit_waits(qn, deps)
        self.cnt[k] += 16
        c = self.cnt[k]
        self.q[qn].append(("d", out, in_, self.sems[k]))
        self.n_ins += 1
        for t in w:
            t.w = (k, c)
            t.r = {}
        for t in r:
            if t.r.get(k, 0) < c:
                t.r[k] = c

    def barrier(self):
        deps = {k: v for k, v in self.cnt.items() if v > 0}
        for e in self.ENG:
            self._emit_waits(e, dict(deps))

    def emit(self):
        nc = self.nc
        with nc.Block() as block:
            def mk(e):
                def body(engine):
                    for it in self.q[e]:
                        if it[0] == "w":
                            engine.wait_ge(it[1], it[2])
                        elif it[0] == "i":
                            it[1](engine).then_inc(it[2], 1)
                        else:
                            engine.dma_start(out=it[1], in_=it[2]).then_inc(it[3], 16)
                return body
            block.tensor(mk("pe"))
            block.vector(mk("dve"))
            block.scalar(mk("act"))
            block.gpsimd(mk("pool"))
            block.sync(mk("sp"))


class Ring:
    def __init__(self, aps):
        self.items = [(a, Tok()) for a in aps]
        self.i = 0

    def next(self):
        it = self.items[self.i]
        self.i = (self.i + 1) % len(self.items)
        return it


class K:
    def __init__(self, nc, nseq):
        self.nc = nc
        self.P = Prog(nc)
        self.nseq = nseq

    def mm(self, out, lhsT, rhs, start, stop, r, w):
        self.P.op("pe", lambda e: e.matmul(out, lhsT=lhsT, rhs=rhs, start=start, stop=stop), r, w)

    def tr(self, out, in_, ident, r, w):
        self.P.op("pe", lambda e: e.transpose(out, in_, ident), r, w)

    def cp(self, eng, out, in_, r, w):
        if eng == "act":
            self.P.op("act", lambda e: e.activation(out, in_, AF.Copy), r, w)
        else:
            self.P.op(eng, lambda e: e.tensor_copy(out, in_), r, w)

    def act(self, out, in_, func, r, w, bias=None, scale=None, accum=None):
        kw = {}
        if bias is not None:
            kw["bias"] = bias
        if scale is not None:
            kw["scale"] = scale
        if accum is not None:
            kw["accum_out"] = accum
        self.P.op("act", lambda e: e.activation(out, in_, func, **kw), r, w)

    def tt(self, eng, out, in0, in1, op, r, w):
        self.P.op(eng, lambda e: e.tensor_tensor(out, in0, in1, op), r, w)

    def ts(self, eng, out, in0, s1, s2, op0, op1, r, w):
        if s2 is None:
            self.P.op(eng, lambda e: e.tensor_scalar(out, in0, s1, None, op0), r, w)
        else:
            self.P.op(eng, lambda e: e.tensor_scalar(out, in0, s1, s2, op0, op1), r, w)

    def stt(self, eng, out, in0, scalar, in1, op0, op1, r, w):
        self.P.op(eng, lambda e: e.scalar_tensor_tensor(out, in0, scalar, in1, op0, op1), r, w)

    def memset(self, eng, out, val, w):
        self.P.op(eng, lambda e: e.memset(out, val), (), w)

    def recip(self, out, in_, r, w):
        self.P.op("dve", lambda e: e.reciprocal(out, in_), r, w)

    def ld(self, out, in_, w, r=()):
        self.P.dma("sp", out, in_, r, w)

    def st(self, out, in_, r, w=()):
        self.P.dma("pool", out, in_, r, w)


def bcast_last(ap, n):
    shp = list(ap.shape)
    return ap.unsqueeze(len(shp)).to_broadcast(shp + [n])


def ml_phase_a(k, li, x_src, T):
    nc, P = k.nc, k.P
    ps, tps = k.ps, k.tps
    with ExitStack() as es:
        def A(name, shape, dt):
            return es.enter_context(nc.sbuf_tensor(f"mA{li}_{name}", shape, dt)).ap()
        w_in = A("win", [128, 8, 4096], BF16)
        t_win = [Tok() for _ in range(8)]
        bd = A("bd", [128, 48, 128], BF16)
        t_bd = Tok()
        vec = A("vec", [128, 16, 8], F32)
        t_vec = Tok()
        wif32 = A("wif32", [128, 48, 8], F32)
        wif = A("wif", [128, 48, 8], BF16)
        t_wif = Tok()
        bif = A("bif", [8, 1], F32)
        t_bif = Tok()
        gb = A("gb", [128, 1024], F32)
        t_gb = Tok()
        stg = Ring([A(f"stg{i}", [128, 2048], F32) for i in range(2)])
        xr_ = Ring([A(f"xt{i}", [128, 1024], F32) for i in range(2)])
        hb_ = Ring([A(f"hb{i}", [128, 1024], BF16) for i in range(2)])
        junk = A("junk", [128, 1024], BF16)
        t_junk = Tok()
        st_ = Ring([A(f"st{i}", [128, 4], F32) for i in range(2)])
        hT = A("hT", [128, 8, 512], BF16)
        t_hT = Tok()
        xin = A("xin", [128, 16, 515], BF16)
        t_xin = [Tok() for _ in range(16)]
        acc_ = Ring([A(f"acc{i}", [128, 512], F32) for i in range(2)])
        xc = A("xc", [128, 16, 512], BF16)
        t_xc = [Tok() for _ in range(16)]
        sx_ = Ring([A(f"sx{i}", [128, 512], BF16) for i in range(2)])
        sz_ = Ring([A(f"sz{i}", [128, 512], BF16) for i in range(2)])
        qkv_ = Ring([A(f"qkv{i}", [128, 512], BF16) for i in range(4)])
        tok_ = Ring([A(f"tokm{i}", [128, 2048], BF16) for i in range(3)])
        gsb = A("gsb", [8, 2, 512], F32)
        t_gsb = Tok()
        gt_ = Ring([A(f"gt{i}", [128, 16], F32) for i in range(2)])

        for kc in range(8):
            for hf in range(2):
                s_ap, s_t = stg.next()
                k.ld(s_ap, k.ml_w_in[li, kc * 128:(kc + 1) * 128, hf * 2048:(hf + 1) * 2048], w=[s_t])
                k.cp(("dve", "pool")[hf], w_in[:, kc, hf * 2048:(hf + 1) * 2048], s_ap, r=[s_t], w=[t_win[kc]])
        for g3 in range(3):
            s_ap, s_t = stg.next()
            sv = s_ap.rearrange("p (c n) -> p c n", c=16)
            k.ld(sv, k.ml_bd[li, g3 * 16:(g3 + 1) * 16].rearrange("c p n -> p c n"), w=[s_t])
            k.cp("dve", bd[:, g3 * 16:(g3 + 1) * 16, :], sv, r=[s_t], w=[t_bd])
        k.ld(vec, k.ml_vec[li], w=[t_vec])
        k.ld(wif32, k.ml_wif[li], w=[t_wif])
        k.ts("dve", wif32[:, 0:16, :], wif32[:, 0:16, :], float(DH ** 0.5), None, ALU.mult, None, r=[t_wif], w=[t_wif])
        k.cp("dve", wif, wif32, r=[t_wif], w=[t_wif])
        k.ld(bif, k.ml_bif[li], w=[t_bif])
        k.ld(gb, k.ml_norm[li].partition_broadcast(128), w=[t_gb])

        for s in range(k.nseq):
            for st in range(4):
                t0 = st * 512
                for sub in range(4):
                    r0 = t0 + sub * 128
                    xt, t_xt = xr_.next()
                    k.ld(xt, x_src[s, r0:r0 + 128, :], w=[t_xt])
                    sv, t_sv = st_.next()
                    k.act(junk, xt, AF.Square, r=[t_xt], w=[t_junk, t_sv], accum=sv[:, 0:1])
                    k.act(sv[:, 1:2], sv[:, 0:1], AF.Ln, r=[t_sv], w=[t_sv], scale=1.0 / D, bias=k.c_eps[:, 0:1])
                    k.act(sv[:, 2:3], sv[:, 1:2], AF.Exp, r=[t_sv], w=[t_sv], scale=-0.5)
                    hb, t_hb = hb_.next()
                    k.stt("dve", hb, xt, sv[:, 2:3], gb, ALU.mult, ALU.mult, r=[t_xt, t_sv, t_gb], w=[t_hb])
                    pv = ps[5].bitcast(BF16).rearrange("p (a t) -> p a t", a=8)
                    for kc in range(8):
                        k.tr(pv[:, kc, :], hb[:, kc * 128:(kc + 1) * 128], k.ident_bf, r=[t_hb], w=[tps[5]])
                    k.cp("dve", hT[:, :, sub * 128:(sub + 1) * 128], pv, r=[tps[5]], w=[t_hT])
                if st == 0:
                    k.memset("pool", xin[:, :, 0:3], 0.0, w=t_xin)
                else:
                    k.cp("pool", xin[:, :, 0:3], xin[:, :, 512:515], r=t_xin, w=t_xin)
                for fc in range(16):
                    bi = fc % 2
                    for kc in range(8):
                        k.mm(ps[bi], w_in[:, kc, fc * 128:(fc + 1) * 128], hT[:, kc, :], kc == 0, kc == 7,
                             r=[t_win[kc], t_hT], w=[tps[bi]])
                    k.cp("act", xin[:, fc, 3:515], ps[bi], r=[tps[bi]], w=[t_xin[fc]])
                    acc, t_acc = acc_.next()
                    k.ts("dve", acc, xin[:, fc, 0:512], vec[:, fc, 0:1], None, ALU.mult, None,
                         r=[t_xin[fc], t_vec], w=[t_acc])
                    for tap in range(1, 4):
                        k.stt("dve", acc, xin[:, fc, tap:tap + 512], vec[:, fc, tap:tap + 1], acc, ALU.mult, ALU.add,
                              r=[t_xin[fc], t_acc], w=[t_acc])
                    k.act(xc[:, fc, :], acc, AF.Silu, r=[t_acc], w=[t_xc[fc]], bias=vec[:, fc, 4:5])
                    sx, t_sx = sx_.next()
                    k.ts("pool", sx, xc[:, fc, :], vec[:, fc, 6:7], None, ALU.mult, None, r=[t_xc[fc]], w=[t_sx])
                    k.st(T["sxT"][s, fc * 128:(fc + 1) * 128, t0:t0 + 512], sx, r=[t_sx])
                    for which in range(3):
                        pb = 2 + (which % 2)
                        src = xc[:, fc, :] if which < 2 else xin[:, fc, 3:515]
                        tsrc = t_xc[fc] if which < 2 else t_xin[fc]
                        k.mm(ps[pb], bd[:, which * 16 + fc, :], src, True, True, r=[t_bd, tsrc], w=[tps[pb]])
                        qt, t_qt = qkv_.next()
                        if which == 0:
                            k.act(qt, ps[pb], AF.Copy, r=[tps[pb]], w=[t_qt], scale=float(DH ** -0.5))
                            k.st(T["qT"][s, fc * 128:(fc + 1) * 128, t0:t0 + 512], qt, r=[t_qt])
                        elif which == 1:
                            k.cp("dve", qt, ps[pb], r=[tps[pb]], w=[t_qt])
                            k.st(T["kT"][s, fc * 128:(fc + 1) * 128, t0:t0 + 512], qt, r=[t_qt])
                        else:
                            k.cp("dve", qt, ps[pb], r=[tps[pb]], w=[t_qt])
                        first = (fc == 0 and which == 0)
                        last = (fc == 15 and which == 2)
                        k.mm(ps[4][0:8, :], wif[:, which * 16 + fc, :], qt, first, last, r=[t_wif, t_qt], w=[tps[4]])
                for fc in range(16, 32):
                    bi = fc % 2
                    for kc in range(8):
                        k.mm(ps[bi], w_in[:, kc, fc * 128:(fc + 1) * 128], hT[:, kc, :], kc == 0, kc == 7,
                             r=[t_win[kc], t_hT], w=[tps[bi]])
                    sz, t_sz = sz_.next()
                    k.act(sz, ps[bi], AF.Silu, r=[tps[bi]], w=[t_sz])
                    k.st(T["szT"][s, (fc - 16) * 128:(fc - 15) * 128, t0:t0 + 512], sz, r=[t_sz])
                for which in (1, 2):
                    for sub in range(4):
                        tm, t_tm = tok_.next()
                        for fg in range(4):
                            pb = 6 + (fg % 2)
                            for f4 in range(4):
                                fc = fg * 4 + f4
                                if which == 1:
                                    lhsT = xc[:, fc, sub * 128:(sub + 1) * 128]
                                    tsrc = t_xc[fc]
                                else:
                                    lhsT = xin[:, fc, 3 + sub * 128:3 + (sub + 1) * 128]
                                    tsrc = t_xin[fc]
                                k.mm(ps[pb][:, f4 * 128:(f4 + 1) * 128], lhsT, bd[:, which * 16 + fc, :], True, True,
                                     r=[t_bd, tsrc], w=[tps[pb]])
                            k.cp(("act", "dve")[fg % 2], tm[:, fg * 512:(fg + 1) * 512], ps[pb], r=[tps[pb]], w=[t_tm])
                        dst = T["ktok"] if which == 1 else T["vtok"]
                        r0 = t0 + sub * 128
                        k.st(dst[s, r0:r0 + 128, :], tm, r=[t_tm])
                k.act(gsb[:, 0, :], ps[4][0:8, :], AF.Identity, r=[tps[4], t_bif], w=[t_gsb], bias=bif[:, 0:1])
                k.act(gsb[:, 1, :], gsb[:, 0, :], AF.Exp, r=[t_gsb], w=[t_gsb], scale=-1.0)
                k.act(gsb[:, 1, :], gsb[:, 1, :], AF.Ln, r=[t_gsb], w=[t_gsb], bias=k.c_one[0:8, 0:1])
                for sub in range(4):
                    for a in range(2):
                        k.tr(ps[5][:, a * 8:(a + 1) * 8], gsb[:, a, sub * 128:(sub + 1) * 128], k.ident_f[0:8, 0:8],
                             r=[t_gsb], w=[tps[5]])
                    gt, t_gt = gt_.next()
                    k.cp("dve", gt, ps[5][:, 0:16], r=[tps[5]], w=[t_gt])
                    r0 = t0 + sub * 128
                    k.st(T["gtok"][s, r0:r0 + 128, :], gt, r=[t_gt])
    P.barrier()


def ml_phase_b(k, li, x_src, x_dst, T):
    nc, P = k.nc, k.P
    ps, tps = k.ps, k.tps
    with ExitStack() as es:
        def A(name, shape, dt):
            return es.enter_context(nc.sbuf_tensor(f"mB{li}_{name}", shape, dt)).ap()
        w_out = A("wout", [128, 16, 1024], BF16)
        t_wout = Tok()
        vec = A("vec", [128, 16, 8], F32)
        t_vec = Tok()
        stg = Ring([A(f"stg{i}", [128, 2048], F32) for i in range(2)])
        X = A("X", [128, 16, 513], F32)
        t_X = [Tok() for _ in range(4)]
        STb = A("STb", [128, 16, 513], BF16)
        t_STb = [Tok() for _ in range(4)]
        qT_ = Ring([A(f"qT{i}", [128, 16, 128], BF16) for i in range(2)])
        kT_ = Ring([A(f"kT{i}", [128, 16, 128], BF16) for i in range(2)])
        kt_ = Ring([A(f"kt{i}", [128, 2048], BF16) for i in range(2)])
        vt_ = Ring([A(f"vt{i}", [128, 2048], BF16) for i in range(2)])
        gt_ = Ring([A(f"gt{i}", [128, 16], F32) for i in range(2)])
        xr_ = Ring([A(f"xr{i}", [128, 1024], F32) for i in range(2)])
        sx_ = Ring([A(f"sx{i}", [128, 16, 128], BF16) for i in range(2)])
        sz_ = Ring([A(f"sz{i}", [128, 16, 128], BF16) for i in range(2)])
        ve_ = Ring([A(f"ve{i}", [128, 513], BF16) for i in range(3)])
        PT_ = Ring([A(f"PT{i}", [128, 128], BF16) for i in range(2)])
        gm_ = Ring([A(f"gm{i}", [128, 4, 4], F32) for i in range(3)])
        sm_ = Ring([A(f"sm{i}", [128, 16], F32) for i in range(4)])
        bs_ = Ring([A(f"bs{i}", [128, 6], F32) for i in range(4)])
        hn_ = Ring([A(f"hn{i}", [128, 2048], BF16) for i in range(2)])
        t1 = A("t1", [128, 16, 128], F32)
        t_t1 = Tok()
        gT_ = Ring([A(f"gT{i}", [128, 16, 128], BF16) for i in range(2)])
        yo_ = Ring([A(f"yo{i}", [128, 1024], F32) for i in range(2)])
        ncol = A("ncol", [128, 16], F32)
        t_ncol = Tok()

        for fc in range(16):
            hf = fc % 2
            if hf == 0:
                s_ap, s_t = stg.next()
                sv = s_ap.rearrange("p (c n) -> p c n", c=2)
                k.ld(sv, k.ml_w_out[li, fc * 128:(fc + 2) * 128, :].rearrange("(c p) n -> p c n", p=128), w=[s_t])
                k.cp("dve", w_out[:, fc:fc + 2, :], sv, r=[s_t], w=[t_wout])
        k.ld(vec, k.ml_vec[li], w=[t_vec])

        for s in range(k.nseq):
            prev_gm = None
            for c in range(NT):
                r0 = c * 128
                qT, t_qT = qT_.next()
                kT, t_kT = kT_.next()
                kt, t_kt = kt_.next()
                vt, t_vt = vt_.next()
                gt, t_gt = gt_.next()
                xr, t_xr = xr_.next()
                sx, t_sx = sx_.next()
                sz, t_sz = sz_.next()
                k.ld(gt, T["gtok"][s, r0:r0 + 128, :], w=[t_gt])
                k.ld(kT, T["kT"][s, :, r0:r0 + 128].rearrange("(a p) t -> p a t", p=128), w=[t_kT])
                k.ld(qT, T["qT"][s, :, r0:r0 + 128].rearrange("(a p) t -> p a t", p=128), w=[t_qT])
                k.ld(vt, T["vtok"][s, r0:r0 + 128, :], w=[t_vt])
                k.ld(kt, T["ktok"][s, r0:r0 + 128, :], w=[t_kt])
                k.ld(sx, T["sxT"][s, :, r0:r0 + 128].rearrange("(a p) t -> p a t", p=128), w=[t_sx])
                k.ld(sz, T["szT"][s, :, r0:r0 + 128].rearrange("(a p) t -> p a t", p=128), w=[t_sz])
                k.ld(xr, x_src[s, r0:r0 + 128, :], w=[t_xr])

                gm, t_gm = gm_.next()
                k.mm(ps[0][:, 0:4], k.caus_f, gt[:, 12:16], True, True, r=[t_gt], w=[tps[0]])
                k.mm(ps[0][:, 4:8], k.ones_f, gt[:, 12:16], True, True, r=[t_gt], w=[tps[0]])
                k.tt("dve", gm[:, 3, :], ps[0][:, 0:4], gt[:, 0:4], ALU.add, r=[tps[0], t_gt], w=[t_gm])
                k.act(gm[:, 0, :], gm[:, 3, :], AF.Exp, r=[t_gm], w=[t_gm])
                k.act(gm[:, 1:3, :], ps[0][:, 0:8].rearrange("p (a b) -> p a b", a=2), AF.Exp, r=[tps[0]], w=[t_gm],
                      scale=-1.0)
                hn, t_hn = hn_.next()
                for h in range(NH):
                    ve, t_ve = ve_.next()
                    k.act(ve[:, 0:512], vt[:, h * 512:(h + 1) * 512], AF.Copy, r=[t_vt, t_gm], w=[t_ve],
                          scale=gm[:, 0, h:h + 1])
                    k.cp("pool", ve[:, 512:513], gm[:, 0, h:h + 1], r=[t_gm], w=[t_ve])
                    for dc in range(4):
                        k.mm(ps[1][:, 0:128], kT[:, h * 4 + dc, :], qT[:, h * 4 + dc, :], dc == 0, dc == 3,
                             r=[t_kT, t_qT], w=[tps[1]])
                    PT, t_PT = PT_.next()
                    k.tt("dve", PT, ps[1][:, 0:128], k.caus_f, ALU.mult, r=[tps[1]], w=[t_PT])
                    nb = 2 + (h % 2)
                    k.mm(ps[nb], PT, ve[:, 0:512], True, c == 0, r=[t_PT, t_ve], w=[tps[nb]])
                    if c > 0:
                        for dc in range(4):
                            k.mm(ps[nb], qT[:, h * 4 + dc, :], STb[:, h * 4 + dc, 0:512], False, dc == 3,
                                 r=[t_qT, t_STb[h]], w=[tps[nb]])
                    k.mm(ps[0][:, 8 + h:9 + h], PT, ve[:, 512:513], True, c == 0, r=[t_PT, t_ve], w=[tps[0]])
                    if c > 0:
                        for dc in range(4):
                            k.mm(ps[0][:, 8 + h:9 + h], qT[:, h * 4 + dc, :], STb[:, h * 4 + dc, 512:513], False, dc == 3,
                                 r=[t_qT, t_STb[h]], w=[tps[0]])
                    sm, t_sm = sm_.next()
                    bs, t_bs = bs_.next()
                    k.P.op("dve", (lambda o, i: (lambda e: e.bn_stats(o, i)))(bs, ps[nb]), r=[tps[nb]], w=[t_bs])
                    k.P.op("dve", (lambda o, i: (lambda e: e.bn_aggr(o, i)))(sm[:, 0:2], bs), r=[t_bs], w=[t_sm])
                    k.tt("dve", sm[:, 2:3], ps[0][:, 8 + h:9 + h], gm[:, 1, h:h + 1], ALU.mult, r=[tps[0], t_gm], w=[t_sm])
                    k.stt("dve", sm[:, 8:9], sm[:, 2:3], -1.0, sm[:, 2:3], ALU.mult, ALU.max, r=[t_sm], w=[t_sm])
                    k.ts("dve", sm[:, 2:3], sm[:, 8:9], 1.0, None, ALU.max, None, r=[t_sm], w=[t_sm])
                    k.recip(sm[:, 9:10], sm[:, 2:3], r=[t_sm], w=[t_sm])
                    k.tt("dve", sm[:, 3:4], gm[:, 1, h:h + 1], sm[:, 9:10], ALU.mult, r=[t_sm, t_gm], w=[t_sm])
                    k.tt("dve", sm[:, 4:5], sm[:, 3:4], sm[:, 3:4], ALU.mult, r=[t_sm], w=[t_sm])
                    k.tt("dve", sm[:, 4:5], sm[:, 4:5], sm[:, 1:2], ALU.mult, r=[t_sm], w=[t_sm])
                    k.act(sm[:, 5:6], sm[:, 4:5], AF.Ln, r=[t_sm], w=[t_sm], bias=k.c_eps[:, 0:1])
                    k.act(sm[:, 5:6], sm[:, 5:6], AF.Exp, r=[t_sm], w=[t_sm], scale=-0.5)
                    k.tt("dve", sm[:, 6:7], sm[:, 5:6], sm[:, 3:4], ALU.mult, r=[t_sm], w=[t_sm])
                    k.stt("dve", sm[:, 7:8], sm[:, 0:1], -1.0, sm[:, 6:7], ALU.mult, ALU.mult, r=[t_sm], w=[t_sm])
                    k.act(hn[:, h * 512:(h + 1) * 512], ps[nb], AF.Identity, r=[tps[nb], t_sm], w=[t_hn],
                          scale=sm[:, 6:7], bias=sm[:, 7:8])
                    for dc in range(4):
                        ub = 4 + (dc % 2)
                        lhsT = kt[:, h * 512 + dc * 128:h * 512 + (dc + 1) * 128]
                        k.mm(ps[ub], lhsT, ve[:, 0:512], True, True, r=[t_kt, t_ve], w=[tps[ub]])
                        k.mm(ps[0][:, 16 + h * 4 + dc:17 + h * 4 + dc], lhsT, ve[:, 512:513], True, True,
                             r=[t_kt, t_ve], w=[tps[0]])
                        if c == 0:
                            k.cp("dve", X[:, h * 4 + dc, 0:512], ps[ub], r=[tps[ub]], w=[t_X[h]])
                        else:
                            k.stt("dve", X[:, h * 4 + dc, 0:512], X[:, h * 4 + dc, 0:512], prev_gm[0][:, 2, h:h + 1], ps[ub],
                                  ALU.mult, ALU.add, r=[tps[ub], prev_gm[1], t_X[h]], w=[t_X[h]])
                    xn = X[:, h * 4:(h + 1) * 4, 512:513].rearrange("p a b -> p (a b)")
                    if c == 0:
                        k.cp("dve", xn, ps[0][:, 16 + h * 4:20 + h * 4], r=[tps[0]], w=[t_X[h]])
                    else:
                        k.stt("dve", xn, xn, prev_gm[0][:, 2, h:h + 1], ps[0][:, 16 + h * 4:20 + h * 4],
                              ALU.mult, ALU.add, r=[tps[0], prev_gm[1], t_X[h]], w=[t_X[h]])
                    if c < NT - 1:
                        for dc in range(4):
                            eng = ("act", "pool")[dc % 2]
                            if eng == "act":
                                k.act(STb[:, h * 4 + dc, :], X[:, h * 4 + dc, :], AF.Copy, r=[t_X[h], t_gm], w=[t_STb[h]],
                                      scale=gm[:, 2, h:h + 1])
                            else:
                                k.ts("pool", STb[:, h * 4 + dc, :], X[:, h * 4 + dc, :], gm[:, 2, h:h + 1], None, ALU.mult, None,
                                     r=[t_X[h], t_gm], w=[t_STb[h]])
                prev_gm = (gm, t_gm)
                for half in range(2):
                    pb = 6 + half
                    pv = ps[pb].bitcast(BF16).rearrange("p (a t) -> p a t", a=8)
                    for a in range(8):
                        fc = half * 8 + a
                        k.tr(pv[:, a, :], hn[:, fc * 128:(fc + 1) * 128], k.ident_bf, r=[t_hn], w=[tps[pb]])
                    k.tt("dve", t1[:, half * 8:(half + 1) * 8, :], pv, bcast_last(vec[:, half * 8:(half + 1) * 8, 5], 128),
                         ALU.mult, r=[tps[pb], t_vec], w=[t_t1])
                gT, t_gT = gT_.next()
                k.tt("pool", t1, t1, sx, ALU.add, r=[t_sx, t_t1], w=[t_t1])
                k.tt("pool", gT, t1, sz, ALU.mult, r=[t_sz, t_t1], w=[t_gT])
                yo, t_yo = yo_.next()
                for half in range(2):
                    pb = 2 + half
                    for fc in range(16):
                        k.mm(ps[pb], gT[:, fc, :], w_out[:, fc, half * 512:(half + 1) * 512], fc == 0, fc == 15,
                             r=[t_gT, t_wout], w=[tps[pb]])
                    k.tt("dve", yo[:, half * 512:(half + 1) * 512], ps[pb], xr[:, half * 512:(half + 1) * 512], ALU.add,
                         r=[tps[pb], t_xr], w=[t_yo])
                k.st(x_dst[s, r0:r0 + 128, :], yo, r=[t_yo])
    P.barrier()


NSA_FM = [(0, 1024), (1024, 1280), (1280, 1536), (1536, 1792), (2048, 2304)]
SCALE = 0.125


def nsa_phase_a(k, li, x_src, T):
    nc, P = k.nc, k.P
    ps, tps = k.ps, k.tps
    with ExitStack() as es:
        def A(name, shape, dt):
            return es.enter_context(nc.sbuf_tensor(f"nA{li}_{name}", shape, dt)).ap()
        w_in = A("win", [128, 8, 3648], BF16)
        t_win = [Tok() for _ in range(8)]
        gb = A("gb", [128, 1024], F32)
        t_gb = Tok()
        bg = A("bg", [128, 48], F32)
        t_bg = Tok()
        rot = A("rot", [128, 128], BF16)
        cosf = A("cosf", [128, S], F32)
        sinf = A("sinf", [128, S], F32)
        t_c = Tok()
        stg = Ring([A(f"stg{i}", [128, 1816], F32) for i in range(2)])
        xr_ = Ring([A(f"xt{i}", [128, 1024], F32) for i in range(2)])
        hb_ = Ring([A(f"hb{i}", [128, 1024], BF16) for i in range(2)])
        junk = A("junk", [128, 1024], BF16)
        t_junk = Tok()
        st_ = Ring([A(f"st{i}", [128, 4], F32) for i in range(2)])
        hT = A("hT", [128, 8, 512], BF16)
        t_hT = Tok()
        xb_ = Ring([A(f"xb{i}", [128, 512], BF16) for i in range(3)])
        ta_ = Ring([A(f"ta{i}", [128, 512], F32) for i in range(2)])
        tb_ = Ring([A(f"tb{i}", [128, 512], F32) for i in range(2)])
        ro_ = Ring([A(f"ro{i}", [128, 512], BF16) for i in range(3)])
        tm_ = Ring([A(f"tm{i}", [128, 1536], BF16) for i in range(2)])
        gl_ = Ring([A(f"gl{i}", [128, 48], F32) for i in range(2)])

        for kc in range(8):
            for hf in range(2):
                s_ap, s_t = stg.next()
                k.ld(s_ap, k.nsa_w_in[li, kc * 128:(kc + 1) * 128, hf * 1816:(hf + 1) * 1816], w=[s_t])
                k.cp(("dve", "pool")[hf], w_in[:, kc, hf * 1816:(hf + 1) * 1816], s_ap, r=[s_t], w=[t_win[kc]])
        k.ld(gb, k.nsa_norm[li].partition_broadcast(128), w=[t_gb])
        k.ld(bg, k.nsa_b_gate[li].partition_broadcast(128), w=[t_bg])
        s_ap, s_t = stg.next()
        k.ld(s_ap[:, 0:128], k.c_rot, w=[s_t])
        k.cp("dve", rot, s_ap[:, 0:128], r=[s_t], w=[t_c])
        k.ld(cosf, k.c_cos, w=[t_c])
        k.ld(sinf, k.c_sin, w=[t_c])

        fm = []
        for c8 in range(8):
            fm.append((c8 * 128, "qT", c8 * 128, True))
        for c2 in range(2):
            fm.append((1024 + c2 * 128, "kcT", c2 * 128, False))
            fm.append((1280 + c2 * 128, "vcT", c2 * 128, False))
            fm.append((1536 + c2 * 128, "ksT", c2 * 128, True))
            fm.append((2048 + c2 * 128, "kwT", c2 * 128, True))

        for s in range(k.nseq):
            for st in range(4):
                t0 = st * 512
                for sub in range(4):
                    r0 = t0 + sub * 128
                    xt, t_xt = xr_.next()
                    k.ld(xt, x_src[s, r0:r0 + 128, :], w=[t_xt])
                    sv, t_sv = st_.next()
                    k.act(junk, xt, AF.Square, r=[t_xt], w=[t_junk, t_sv], accum=sv[:, 0:1])
                    k.act(sv[:, 1:2], sv[:, 0:1], AF.Ln, r=[t_sv], w=[t_sv], scale=1.0 / D, bias=k.c_eps[:, 0:1])
                    k.act(sv[:, 2:3], sv[:, 1:2], AF.Exp, r=[t_sv], w=[t_sv], scale=-0.5)
                    hb, t_hb = hb_.next()
                    k.stt("dve", hb, xt, sv[:, 2:3], gb, ALU.mult, ALU.mult, r=[t_xt, t_sv, t_gb], w=[t_hb])
                    pv = ps[5].bitcast(BF16).rearrange("p (a t) -> p a t", a=8)
                    for kc in range(8):
                        k.tr(pv[:, kc, :], hb[:, kc * 128:(kc + 1) * 128], k.ident_bf, r=[t_hb], w=[tps[5]])
                    k.cp("dve", hT[:, :, sub * 128:(sub + 1) * 128], pv, r=[tps[5]], w=[t_hT])
                for i, (c0, dst, d0, rope) in enumerate(fm):
                    bi = i % 2
                    for kc in range(8):
                        k.mm(ps[bi], w_in[:, kc, c0:c0 + 128], hT[:, kc, :], kc == 0, kc == 7,
                             r=[t_win[kc], t_hT], w=[tps[bi]])
                    xb, t_xb = xb_.next()
                    k.cp("act", xb, ps[bi], r=[tps[bi]], w=[t_xb])
                    if dst == "qT" or not rope:
                        k.st(T[dst][s, d0:d0 + 128, t0:t0 + 512], xb, r=[t_xb])
                    if rope:
                        pb = 2 + (i % 2)
                        k.mm(ps[pb], rot, xb, True, True, r=[t_c, t_xb], w=[tps[pb]])
                        ta, t_ta = ta_.next()
                        tb, t_tb = tb_.next()
                        k.tt("dve", ta, ps[bi], cosf[:, t0:t0 + 512], ALU.mult, r=[tps[bi], t_c, t_xb], w=[t_ta])
                        k.tt("dve", tb, ps[pb], sinf[:, t0:t0 + 512], ALU.mult, r=[tps[pb], t_c], w=[t_tb])
                        ro, t_ro = ro_.next()
                        k.tt("pool", ro, ta, tb, ALU.add, r=[t_ta, t_tb], w=[t_ro])
                        rd = "qrT" if dst == "qT" else dst
                        k.st(T[rd][s, d0:d0 + 128, t0:t0 + 512], ro, r=[t_ro])
                for sub in range(4):
                    r0 = t0 + sub * 128
                    tm, t_tm = tm_.next()
                    lh = [hT[:, kc, sub * 128:(sub + 1) * 128] for kc in range(8)]
                    for half, c0 in enumerate((1792, 2304)):
                        for kc in range(8):
                            k.mm(ps[6][:, half * 256:(half + 1) * 256], lh[kc], w_in[:, kc, c0:c0 + 256], kc == 0, kc == 7,
                                 r=[t_win[kc], t_hT], w=[tps[6]])
                    k.cp("dve", tm[:, 0:512], ps[6], r=[tps[6]], w=[t_tm])
                    for zi in range(2):
                        pb = 7 if zi == 0 else 6
                        c0 = 2560 + zi * 512
                        for kc in range(8):
                            k.mm(ps[pb], lh[kc], w_in[:, kc, c0:c0 + 512], kc == 0, kc == 7,
                                 r=[t_win[kc], t_hT], w=[tps[pb]])
                        k.act(tm[:, 512 + zi * 512:1024 + zi * 512], ps[pb], AF.Silu, r=[tps[pb]], w=[t_tm])
                    for kc in range(8):
                        k.mm(ps[7][:, 0:48], lh[kc], w_in[:, kc, 3584:3632], kc == 0, kc == 7,
                             r=[t_win[kc], t_hT], w=[tps[7]])
                    gl, t_gl = gl_.next()
                    k.tt("dve", gl, ps[7][:, 0:48], bg, ALU.add, r=[tps[7], t_bg], w=[t_gl])
                    k.act(gl, gl, AF.Sigmoid, r=[t_gl], w=[t_gl])
                    k.st(T["vsw"][s, r0:r0 + 128, :], tm[:, 0:512], r=[t_tm])
                    k.st(T["szt"][s, r0:r0 + 128, :], tm[:, 512:1536], r=[t_tm])
                    k.st(T["gate"][s, r0:r0 + 128, :], gl, r=[t_gl])
    P.barrier()


def nsa_phase_c(k, li, x_src, x_dst, T):
    nc, P = k.nc, k.P
    ps, tps = k.ps, k.tps
    with ExitStack() as es0:
        def A0(name, shape, dt):
            return es0.enter_context(nc.sbuf_tensor(f"nC{li}_{name}", shape, dt)).ap()
        w_out = A0("wout", [128, 8, 1024], BF16)
        t_wout = Tok()
        kcmpT = A0("kcmpT", [128, k.nseq, 4, 128], BF16)
        vcmp = A0("vcmp", [128, k.nseq, 4, 128], BF16)
        t_cmp = Tok()
        with ExitStack() as es:
            def A(name, shape, dt):
                return es.enter_context(nc.sbuf_tensor(f"nB{li}_{name}", shape, dt)).ap()
            stg = Ring([A(f"stg{i}", [128, 2048], F32) for i in range(2)])
            w1 = A("w1", [128, 2, 32, 128], BF16)
            w2k = A("w2k", [128, 128], BF16)
            w2v = A("w2v", [128, 64], BF16)
            peT = A("peT", [128, 2, 32], BF16)
            bh = A("bh", [128, 2], F32)
            t_w = Tok()
            kc2 = A("kc2", [128, 2, S], BF16)
            vc2 = A("vc2", [128, 2, S], BF16)
            t_kv = Tok()
            hTs_ = Ring([A(f"hTs{i}", [128, 128], BF16) for i in range(2)])
            for c8 in range(0, 8, 2):
                s_ap, s_t = stg.next()
                sv = s_ap.rearrange("p (c n) -> p c n", c=2)
                k.ld(sv, k.nsa_w_out[li, c8 * 128:(c8 + 2) * 128, :].rearrange("(c p) n -> p c n", p=128), w=[s_t])
                k.cp("dve", w_out[:, c8:c8 + 2, :], sv, r=[s_t], w=[t_wout])
            for kv in range(2):
                for hf in range(2):
                    s_ap, s_t = stg.next()
                    sv = s_ap.rearrange("p (l n) -> p l n", l=16)
                    src = k.nsa_cmp_w1[li, kv, hf * 1024:(hf + 1) * 1024, :].rearrange("(l d) n -> d l n", d=64)
                    k.ld(sv[0:64], src, w=[s_t])
                    k.ld(sv[64:128], src, w=[s_t])
                    k.cp("dve", w1[:, kv, hf * 16:(hf + 1) * 16, :], sv, r=[s_t], w=[t_w])
            s_ap, s_t = stg.next()
            k.ld(s_ap[:, 0:64], k.nsa_cmp_w2[li, 0], w=[s_t])
            k.ld(s_ap[:, 64:128], k.nsa_cmp_w2[li, 1], w=[s_t])
            pe_v = s_ap[:, 128:192].rearrange("p (a l) -> p a l", a=2)
            k.ld(pe_v[0:64], k.nsa_peT[li].rearrange("a d l -> d a l"), w=[s_t])
            k.ld(pe_v[64:128], k.nsa_peT[li].rearrange("a d l -> d a l"), w=[s_t])
            k.cp("dve", w2k[:, 0:64], s_ap[:, 0:64], r=[s_t], w=[t_w])
            k.cp("dve", w2k[:, 64:128], s_ap[:, 0:64], r=[s_t], w=[t_w])
            k.cp("dve", w2v, s_ap[:, 64:128], r=[s_t], w=[t_w])
            k.cp("dve", peT, pe_v, r=[s_t], w=[t_w])
            for kv in range(2):
                for l in range(32):
                    k.mm(ps[7][:, kv:kv + 1], w1[0:64, kv, l, :], peT[0:64, kv, l:l + 1], l == 0, l == 31, r=[t_w], w=[tps[7]])
            k.cp("dve", bh, ps[7][:, 0:2], r=[tps[7]], w=[t_w])
            for s in range(k.nseq):
                k.ld(vcmp[:, s, :, 64:97], k.c_ovl.rearrange("p (g n) -> p g n", g=4), w=[t_cmp])
                k.ld(kc2, T["kcT"][s].rearrange("(c p) t -> p c t", p=128), w=[t_kv])
                k.ld(vc2, T["vcT"][s].rearrange("(c p) t -> p c t", p=128), w=[t_kv])
                for kv in range(2):
                    src = kc2 if kv == 0 else vc2
                    for g in range(4):
                        base = (g % 2) * 64
                        for l in range(32):
                            k.mm(ps[6][:, 0:127], w1[base:base + 64, kv, l, :], src[base:base + 64, g // 2, l:l + 16 * 126 + 1:16],
                                 l == 0, l == 31, r=[t_w, t_kv], w=[tps[6]])
                        hTs, t_hTs = hTs_.next()
                        k.act(hTs[:, 0:127], ps[6][:, 0:127], AF.Silu, r=[tps[6], t_w], w=[t_hTs], bias=bh[:, kv:kv + 1])
                        if kv == 0:
                            k.mm(ps[7][:, 0:127], w2k, hTs[:, 0:127], True, True, r=[t_w, t_hTs], w=[tps[7]])
                            k.cp("dve", kcmpT[:, s, g, 0:127], ps[7][:, 0:127], r=[tps[7]], w=[t_cmp])
                        else:
                            k.mm(ps[7][0:127, 0:64], hTs[:, 0:127], w2v, True, True, r=[t_w, t_hTs], w=[tps[7]])
                            k.cp("dve", vcmp[0:127, s, g, 0:64], ps[7][0:127, 0:64], r=[tps[7]], w=[t_cmp])
        P.barrier()
        with ExitStack() as es:
            def A(name, shape, dt):
                return es.enter_context(nc.sbuf_tensor(f"nC{li}_{name}", shape, dt)).ap()
            t_c = Tok()
            Ef = A("Ef", [128, 4, S], BF16)
            causn = A("causn", [128, 4, 512], BF16)
            lown = A("lown", [128, 4, 512], BF16)
            cmpn = A("cmpn", [128, S], BF16)
            fmul = A("fmul", [128, 16, 32], F32)
            fadd = A("fadd", [128, 16, 32], F32)
            ks2 = A("ks2", [128, 4, S], BF16)
            kw2 = A("kw2", [128, 4, S], BF16)
            vs = A("vs", [128, 16, 4, 65], BF16)
            vw = A("vw", [128, 16, 4, 65], BF16)
            t_res = Tok()
            q_ = Ring([A(f"q{i}", [128, 8, 512], BF16) for i in range(1)])
            qr_ = Ring([A(f"qr{i}", [128, 8, 512], BF16) for i in range(1)])
            ex_ = Ring([A(f"ex{i}", [128, 512], BF16) for i in range(3)])
            oacc = A("oacc", [128, 4, 16, 64], F32)
            t_oacc = [Tok() for _ in range(4)]
            ucmp = A("ucmp", [128, 4, 4, 33], F32)
            rdc = A("rdc", [128, 4, 4], F32)
            t_ucmp = Tok()
            tmp_ = Ring([A(f"tmp{i}", [128, 4, 64], F32) for i in range(3)])
            imp_ = Ring([A(f"imp{i}", [128, 32], F32) for i in range(2)])
            rk_ = Ring([A(f"rk{i}", [128, 32, 32], F32) for i in range(2)])
            rs_ = Ring([A(f"rs{i}", [128, 32], F32) for i in range(2)])
            rd_ = Ring([A(f"rd{i}", [128, 16], F32) for i in range(4)])
            negm = A("negm", [128, 4, 4, 32], BF16)
            t_negm = [Tok() for _ in range(4)]
            negT = A("negT", [128, 512], BF16)
            t_negT = Tok()
            gat_ = Ring([A(f"gat{i}", [128, 4, 48], F32) for i in range(2)])
            szt_ = Ring([A(f"szt{i}", [128, 1024], BF16) for i in range(2)])
            xr_ = Ring([A(f"xr{i}", [128, 1024], F32) for i in range(2)])
            og_ = Ring([A(f"og{i}", [128, 1024], BF16) for i in range(2)])
            ogT_ = Ring([A(f"ogT{i}", [128, 8, 128], BF16) for i in range(2)])
            yo_ = Ring([A(f"yo{i}", [128, 1024], F32) for i in range(2)])

            k.ld(Ef, k.c_E.rearrange("p (g n) -> p g n", g=4), w=[t_c])
            k.ld(causn, k.c_causn.rearrange("p (a n) -> p a n", a=4), w=[t_c])
            k.ld(lown, k.c_lown.rearrange("p (a n) -> p a n", a=4), w=[t_c])
            k.ld(cmpn, k.c_cmpn, w=[t_c])
            k.ld(fmul, k.c_fmul, w=[t_c])
            k.ld(fadd, k.c_fadd, w=[t_c])
            k.memset("pool", vs[:, :, :, 64:65], 1.0, w=[t_res])
            k.memset("pool", vw[:, :, :, 64:65], 1.0, w=[t_res])

            def evac(pov, h, b, first, gat, t_gat, clamp, g):
                rd, t_rd = rd_.next()
                if clamp:
                    k.ts("dve", rd[:, 0:4], pov[:, :, 64], 1e-30, None, ALU.max, None, r=[tps_of[0]], w=[t_rd])
                else:
                    k.cp("dve", rd[:, 0:4], pov[:, :, 64], r=[tps_of[0]], w=[t_rd])
                k.recip(rd[:, 4:8], rd[:, 0:4], r=[t_rd], w=[t_rd])
                k.tt("dve", rd[:, 8:12], rd[:, 4:8], gat[:, :, 3 * h + b], ALU.mult, r=[t_rd, t_gat], w=[t_rd])
                if first:
                    k.tt("dve", oacc[:, :, h, :], pov[:, :, 0:64], bcast_last(rd[:, 8:12], 64), ALU.mult,
                         r=[tps_of[0], t_rd], w=[t_oacc[g]])
                else:
                    tmp, t_tmp = tmp_.next()
                    k.tt("dve", tmp, pov[:, :, 0:64], bcast_last(rd[:, 8:12], 64), ALU.mult, r=[tps_of[0], t_rd], w=[t_tmp])
                    k.tt("pool", oacc[:, :, h, :], oacc[:, :, h, :], tmp, ALU.add, r=[t_tmp, t_oacc[g]], w=[t_oacc[g]])
                return rd, t_rd

            tps_of = [None]
            for s in range(k.nseq):
                for hf in range(2):
                    k.ld(ks2[hf * 64:(hf + 1) * 64], T["ksT"][s].rearrange("(g d) t -> d g t", d=64), w=[t_res])
                    k.ld(kw2[hf * 64:(hf + 1) * 64], T["kwT"][s].rearrange("(g d) t -> d g t", d=64), w=[t_res])
                vsw_v = T["vsw"][s].rearrange("(kt p) (b g d) -> p kt b g d", p=128, b=2, g=4)
                for kt4 in range(0, 16, 4):
                    for g in range(4):
                        k.ld(vs[:, kt4:kt4 + 4, g, 0:64], vsw_v[:, kt4:kt4 + 4, 0, g], w=[t_res])
                        k.ld(vw[:, kt4:kt4 + 4, g, 0:64], vsw_v[:, kt4:kt4 + 4, 1, g], w=[t_res])
                for qt in range(4):
                    t0 = qt * 512
                    q, t_q = q_.next()
                    qr, t_qr = qr_.next()
                    gat, t_gat = gat_.next()
                    k.ld(q, T["qT"][s, :, t0:t0 + 512].rearrange("(c p) t -> p c t", p=128), w=[t_q])
                    k.ld(qr, T["qrT"][s, :, t0:t0 + 512].rearrange("(c p) t -> p c t", p=128), w=[t_qr])
                    k.ld(gat, T["gate"][s, t0:t0 + 512, :].rearrange("(a p) n -> p a n", p=128), w=[t_gat])
                    need_sel = qt >= 2
                    for g in range(4):
                        for hh in range(4):
                            h = 4 * g + hh
                            ch, base = h // 2, (h % 2) * 64
                            sb = h % 2
                            k.mm(ps[sb][0:127, :], kcmpT[base:base + 64, s, g, 0:127], q[base:base + 64, ch, :], True, False,
                                 r=[t_cmp, t_q], w=[tps[sb]])
                            k.mm(ps[sb][0:127, :], k.ident_bf[0:127, 0:127], cmpn[0:127, t0:t0 + 512], False, True,
                                 r=[t_c], w=[tps[sb]])
                            ex, t_ex = ex_.next()
                            k.act(ex[0:127, :], ps[sb][0:127, :], AF.Exp, r=[tps[sb]], w=[t_ex], scale=SCALE)
                            ob = 2 + (h % 2)
                            pov = ps[ob][:, 0:388].rearrange("p (a n) -> p a n", a=4)
                            for sub in range(4):
                                k.mm(pov[:, sub, :], ex[0:127, sub * 128:(sub + 1) * 128], vcmp[0:127, s, g, 0:97], True, True,
                                     r=[t_ex, t_cmp], w=[tps[ob]])
                            tps_of[0] = tps[ob]
                            rd, t_rd = evac(pov, h, 0, True, gat, t_gat, True, g)
                            if need_sel:
                                k.cp("dve", ucmp[:, :, hh, :], pov[:, :, 64:97], r=[tps[ob]], w=[t_ucmp])
                                k.cp("dve", rdc[:, :, hh], rd[:, 4:8], r=[t_rd], w=[t_ucmp])
                        if need_sel:
                            for sub in range(4):
                                imp, t_imp = imp_.next()
                                k.ts("dve", imp, ucmp[:, sub, 0, 1:33], rdc[:, sub, 0:1], None, ALU.mult, None,
                                     r=[t_ucmp], w=[t_imp])
                                for hh in range(1, 4):
                                    k.stt("dve", imp, ucmp[:, sub, hh, 1:33], rdc[:, sub, hh:hh + 1], imp, ALU.mult, ALU.add,
                                          r=[t_ucmp, t_imp], w=[t_imp])
                                tt_ = 4 * qt + sub
                                k.tt("dve", imp, imp, fmul[:, tt_, :], ALU.mult, r=[t_imp, t_c], w=[t_imp])
                                k.tt("dve", imp, imp, fadd[:, tt_, :], ALU.add, r=[t_imp], w=[t_imp])
                                rk, t_rk = rk_.next()
                                in0 = imp.unsqueeze(1).to_broadcast([128, 32, 32])
                                in1 = imp.unsqueeze(2).to_broadcast([128, 32, 32])
                                k.tt("dve", rk, in0, in1, ALU.is_gt, r=[t_imp], w=[t_rk])
                                rs, t_rs = rs_.next()
                                k.P.op("dve", (lambda o, i: (lambda e: e.reduce_sum(o, i, AX.X)))(rs, rk), r=[t_rk], w=[t_rs])
                                k.ts("dve", negm[:, sub, g, :], rs, 15.5, NEG, ALU.is_gt, ALU.mult, r=[t_rs], w=[t_negm[sub]])
                    if need_sel:
                        pv = ps[4].bitcast(BF16)[:, 0:512].rearrange("p (a t) -> p a t", a=4)
                        for sub in range(4):
                            k.tr(pv[:, sub, :], negm[:, sub, :, :].rearrange("p g n -> p (g n)"), k.ident_bf,
                                 r=[t_negm[sub]], w=[tps[4]])
                        k.cp("dve", negT, ps[4].bitcast(BF16)[:, 0:512], r=[tps[4]], w=[t_negT])
                    for br in range(2):
                        kk = ks2 if br == 0 else kw2
                        vv = vs if br == 0 else vw
                        kt_lo = 0 if br == 0 else max(0, 4 * qt - 4)
                        kt_hi = 4 * qt + 3
                        for g in range(4):
                            for hh in range(4):
                                h = 4 * g + hh
                                ch, base = h // 2, (h % 2) * 64
                                ob = 2 + (h % 2)
                                pov = ps[ob][:, 0:260].rearrange("p (a n) -> p a n", a=4)
                                first_pv = True
                                for kt in range(kt_lo, kt_hi + 1):
                                    sb = kt % 2
                                    r_ = kt - 4 * qt
                                    extra = []
                                    if br == 0 and need_sel:
                                        extra.append((Ef[:, g, kt * 128:(kt + 1) * 128], negT, [t_c, t_negT]))
                                    if r_ >= 0:
                                        extra.append((k.ident_bf, causn[:, r_, :], [t_c]))
                                    elif br == 1:
                                        extra.append((k.ident_bf, lown[:, r_ + 4, :], [t_c]))
                                    k.mm(ps[sb], kk[base:base + 64, g, kt * 128:(kt + 1) * 128], qr[base:base + 64, ch, :],
                                         True, len(extra) == 0, r=[t_res, t_qr], w=[tps[sb]])
                                    for ei, (lt, rh, rt) in enumerate(extra):
                                        k.mm(ps[sb], lt, rh, False, ei == len(extra) - 1, r=rt, w=[tps[sb]])
                                    ex, t_ex = ex_.next()
                                    k.act(ex, ps[sb], AF.Exp, r=[tps[sb]], w=[t_ex], scale=SCALE)
                                    for sub in range(4):
                                        hi_s = 4 * qt + sub
                                        lo_s = 0 if br == 0 else max(0, hi_s - 4)
                                        if kt < lo_s or kt > hi_s:
                                            continue
                                        k.mm(pov[:, sub, :], ex[:, sub * 128:(sub + 1) * 128], vv[:, kt, g, :],
                                             first_pv, kt == hi_s, r=[t_ex, t_res], w=[tps[ob]])
                                        first_pv = False
                                tps_of[0] = tps[ob]
                                evac(pov, h, 1 + br, False, gat, t_gat, False, g)
                    for sub in range(4):
                        r0 = t0 + sub * 128
                        szt, t_szt = szt_.next()
                        xr, t_xr = xr_.next()
                        k.ld(szt, T["szt"][s, r0:r0 + 128, :], w=[t_szt])
                        k.ld(xr, x_src[s, r0:r0 + 128, :], w=[t_xr])
                        og, t_og = og_.next()
                        k.tt("pool", og, oacc[:, sub, :, :].rearrange("p h d -> p (h d)"), szt, ALU.mult,
                             r=t_oacc + [t_szt], w=[t_og])
                        pv = ps[4].bitcast(BF16).rearrange("p (a t) -> p a t", a=8)
                        for c8 in range(8):
                            k.tr(pv[:, c8, :], og[:, c8 * 128:(c8 + 1) * 128], k.ident_bf, r=[t_og], w=[tps[4]])
                        ogT, t_ogT = ogT_.next()
                        k.cp("act", ogT, pv, r=[tps[4]], w=[t_ogT])
                        yo, t_yo = yo_.next()
                        for half in range(2):
                            pb = 5 + half
                            for c8 in range(8):
                                k.mm(ps[pb], ogT[:, c8, :], w_out[:, c8, half * 512:(half + 1) * 512], c8 == 0, c8 == 7,
                                     r=[t_ogT, t_wout], w=[tps[pb]])
                            k.tt("dve", yo[:, half * 512:(half + 1) * 512], ps[pb], xr[:, half * 512:(half + 1) * 512], ALU.add,
                                 r=[tps[pb], t_xr], w=[t_yo])
                        k.st(x_dst[s, r0:r0 + 128, :], yo, r=[t_yo])
    P.barrier()


def build_program(nseq=NSEQ, layers=(0, 1, 2, 3), final_norm=True, limit=None):
    nc = bass.Bass("TRN2", target_bir_lowering=False)
    k = K(nc, nseq)
    k.P.limit = limit

    def din(name, shape, dt=F32):
        return nc.dram_tensor(name, list(shape), dt, kind="ExternalInput").ap()

    def dscr(name, shape, dt):
        return nc.dram_tensor(name, list(shape), dt, kind="Internal").ap()

    x = din("x", [nseq, S, D])
    k.ml_norm = din("ml_norm", [2, D])
    k.ml_w_in = din("ml_w_in", [2, D, 4096])
    k.ml_bd = din("ml_bd", [2, 48, 128, 128])
    k.ml_vec = din("ml_vec", [2, 128, 16, 8])
    k.ml_wif = din("ml_wif", [2, 128, 48, 8])
    k.ml_bif = din("ml_bif", [2, 8, 1])
    k.ml_w_out = din("ml_w_out", [2, ML_INNER, D])
    k.final_norm = din("final_norm", [1, D])
    k.nsa_norm = din("nsa_norm", [2, D])
    k.nsa_w_in = din("nsa_w_in", [2, D, 3632])
    k.nsa_b_gate = din("nsa_b_gate", [2, 48])
    k.nsa_peT = din("nsa_peT", [2, 2, 64, 32])
    k.nsa_cmp_w1 = din("nsa_cmp_w1", [2, 2, 2048, 128])
    k.nsa_cmp_w2 = din("nsa_cmp_w2", [2, 2, 128, 64])
    k.nsa_w_out = din("nsa_w_out", [2, D, D])
    k.c_rot = din("c_rot", [128, 128])
    k.c_cos = din("c_cos", [128, S])
    k.c_sin = din("c_sin", [128, S])
    k.c_E = din("c_E", [128, 4 * S], BF16)
    k.c_causn = din("c_causn", [128, 4 * 512], BF16)
    k.c_lown = din("c_lown", [128, 4 * 512], BF16)
    k.c_cmpn = din("c_cmpn", [128, S], BF16)
    k.c_ovl = din("c_ovl", [128, 4 * 33], BF16)
    k.c_fmul = din("c_fmul", [128, 16, 32])
    k.c_fadd = din("c_fadd", [128, 16, 32])
    c_ident = din("c_ident", [128, 128])
    c_caus = din("c_caus", [128, 128])
    y = nc.dram_tensor("y", [nseq, S, D], F32, kind="ExternalOutput").ap()
    xres = dscr("xres", [nseq, S, D], F32)
    T = {
        "qT": dscr("s_qT", [nseq, ML_INNER, S], BF16),
        "kT": dscr("s_kT", [nseq, ML_INNER, S], BF16),
        "sxT": dscr("s_sxT", [nseq, ML_INNER, S], BF16),
        "szT": dscr("s_szT", [nseq, ML_INNER, S], BF16),
        "ktok": dscr("s_ktok", [nseq, S, ML_INNER], BF16),
        "vtok": dscr("s_vtok", [nseq, S, ML_INNER], BF16),
        "gtok": dscr("s_gtok", [nseq, S, 16], F32),
    }
    TN = {
        "qT": dscr("n_qT", [nseq, 1024, S], BF16),
        "qrT": dscr("n_qrT", [nseq, 1024, S], BF16),
        "kcT": dscr("n_kcT", [nseq, 256, S], BF16),
        "vcT": dscr("n_vcT", [nseq, 256, S], BF16),
        "ksT": dscr("n_ksT", [nseq, 256, S], BF16),
        "kwT": dscr("n_kwT", [nseq, 256, S], BF16),
        "vsw": dscr("n_vsw", [nseq, S, 512], BF16),
        "szt": dscr("n_szt", [nseq, S, 1024], BF16),
        "gate": dscr("n_gate", [nseq, S, 48], F32),
    }

    def C(name, shape, dt):
        return nc.alloc_sbuf_tensor(name, shape, dt).ap()
    k.ident_f = C("ident_f", [128, 128], F32)
    k.ident_bf = C("ident_bf", [128, 128], BF16)
    k.caus_f = C("caus_f", [128, 128], F32)
    k.ones_f = C("ones_f", [128, 128], F32)
    k.c_eps = C("c_eps", [128, 1], F32)
    k.c_one = C("c_one", [128, 1], F32)
    k.ps = [nc.alloc_psum_tensor(f"ps{i}", [128, 512], F32).ap() for i in range(8)]
    k.tps = [Tok(f"ps{i}", x=True) for i in range(8)]
    tc = Tok()
    k.ld(k.ident_f, c_ident, w=[tc])
    k.ld(k.caus_f, c_caus, w=[tc])
    k.cp("dve", k.ident_bf, k.ident_f, r=[tc], w=[tc])
    k.memset("dve", k.ones_f, 1.0, w=[tc])
    k.memset("dve", k.c_eps, RMS_EPS, w=[tc])
    k.memset("dve", k.c_one, 1.0, w=[tc])
    k.P.barrier()

    cur = x
    for L in layers:
        li = L // 2
        if L % 2 == 0:
            ml_phase_a(k, li, cur, T)
            ml_phase_b(k, li, cur, xres, T)
        else:
            nsa_phase_a(k, li, cur, TN)
            nsa_phase_c(k, li, cur, xres, TN)
        cur = xres

    print("n_ins before final", k.P.n_ins, flush=True)
    k.P.limit = None
    with ExitStack() as es:
        def A(name, shape, dt):
            return es.enter_context(nc.sbuf_tensor(f"fin_{name}", shape, dt)).ap()
        gb = A("gb", [128, D], F32)
        t_gb = Tok()
        k.ld(gb, k.final_norm[0].partition_broadcast(128), w=[t_gb])
        xr_ = Ring([A(f"x{i}", [128, D], F32) for i in range(3)])
        junk = A("junk", [128, D], F32)
        t_junk = Tok()
        st_ = Ring([A(f"st{i}", [128, 4], F32) for i in range(3)])
        t_y = Tok()
        for s in range(nseq):
            for c in range(NT):
                r0 = c * 128
                xt, t_xt = xr_.next()
                k.ld(xt, cur[s, r0:r0 + 128, :], w=[t_xt])
                if final_norm:
                    sv, t_sv = st_.next()
                    k.act(junk, xt, AF.Square, r=[t_xt], w=[t_junk, t_sv], accum=sv[:, 0:1])
                    k.act(sv[:, 1:2], sv[:, 0:1], AF.Ln, r=[t_sv], w=[t_sv], scale=1.0 / D, bias=k.c_eps[:, 0:1])
                    k.act(sv[:, 2:3], sv[:, 1:2], AF.Exp, r=[t_sv], w=[t_sv], scale=-0.5)
                    k.stt("dve", xt, xt, sv[:, 2:3], gb, ALU.mult, ALU.mult, r=[t_xt, t_sv, t_gb], w=[t_xt])
                k.P.dma("sp", y[s, r0:r0 + 128, :], xt, r=[t_xt], w=[t_y])
        k.P._emit_waits("sp", k.P._deps([t_y], [t_y]))
        deps = {kk: v for kk, v in k.P.cnt.items() if kk.startswith("d_sp") and v > 0}
        k.P._emit_waits("sp", deps)
    k.P.emit()
    return nc


def _block_diag(w):
    out = np.zeros((16, 128, 128), np.float32)
    w4 = w.reshape(16, 32, 4, 4)
    for n in range(32):
        out[:, 4 * n:4 * n + 4, 4 * n:4 * n + 4] = w4[:, n].transpose(0, 2, 1)
    return out


_CONSTS = {}


def _nsa_consts():
    if _CONSTS:
        return _CONSTS
    import ml_dtypes
    bf = ml_dtypes.bfloat16
    c = {}
    rot = np.zeros((128, 128), np.float32)
    for hb in (0, 64):
        for d in range(8):
            rot[hb + d + 8, hb + d] = -1.0
            rot[hb + d, hb + d + 8] = 1.0
    c["c_rot"] = rot
    pos = np.arange(S, dtype=np.float32)
    inv_freq = (np.float32(500000.0) ** (-np.arange(0, 16, 2, dtype=np.float32) / np.float32(16))).astype(np.float32)
    ang = (pos[:, None] * inv_freq[None, :]).astype(np.float32)
    cosf = np.ones((128, S), np.float32)
    sinf = np.zeros((128, S), np.float32)
    for hb in (0, 64):
        for d in range(16):
            cosf[hb + d] = np.cos(ang[:, d % 8])
            sinf[hb + d] = np.sin(ang[:, d % 8])
    c["c_cos"], c["c_sin"] = cosf, sinf
    E = np.zeros((128, 4, S), np.float32)
    key = np.arange(S)
    for g in range(4):
        E[g * 32 + key // 64, g, key] = 1.0
    c["c_E"] = E.reshape(128, 4 * S).astype(bf)
    kk = np.arange(128)[:, None]
    tt = np.arange(512)[None, :]
    causn = np.zeros((128, 4, 512), np.float32)
    lown = np.zeros((128, 4, 512), np.float32)
    for r in range(4):
        valid = kk <= tt - 128 * r
        causn[:, r, :] = np.where(valid, 0.0, NEG)
        lown[:, r, :] = np.where(valid, NEG, 0.0)
    c["c_causn"] = causn.reshape(128, 2048).astype(bf)
    c["c_lown"] = lown.reshape(128, 2048).astype(bf)
    cc = np.arange(128)[:, None]
    tok = np.arange(S)[None, :]
    c["c_cmpn"] = np.where(16 * cc + 31 <= tok, 0.0, NEG).astype(np.float32).astype(bf)
    cs = np.arange(127) * 16
    ss = np.arange(32) * 64
    ov = np.clip(np.minimum(cs[:, None] + 32, ss[None, :] + 64) - np.maximum(cs[:, None], ss[None, :]), 0, None) / 16.0
    ovl = np.zeros((128, 4, 33), np.float32)
    ovl[:, :, 0] = 1.0
    ovl[:127, :, 1:] = ov[:, None, :]
    c["c_ovl"] = ovl.reshape(128, 4 * 33).astype(bf)
    p = np.arange(S)
    blk = np.arange(32)
    dist = (p // 64)[:, None] - blk[None, :]
    forced = (blk[None, :] == 0) | ((dist >= 0) & (dist < 2))
    fmul = np.where(forced | (dist < 0), 0.0, 1.0).astype(np.float32)
    fadd = np.where(forced, 1e9, np.where(dist >= 0, 0.0, -1.0)).astype(np.float32)
    c["c_fmul"] = np.ascontiguousarray(fmul.reshape(16, 128, 32).transpose(1, 0, 2))
    c["c_fadd"] = np.ascontiguousarray(fadd.reshape(16, 128, 32).transpose(1, 0, 2))
    _CONSTS.update(c)
    return _CONSTS


def host_layout(inp):
    f = lambda a: np.ascontiguousarray(np.asarray(a, dtype=np.float32))
    d = {}
    d["ml_norm"] = f(inp["ml_norm"])
    d["ml_w_in"] = f(inp["ml_w_in"])
    bd = np.zeros((2, 48, 128, 128), np.float32)
    vec = np.zeros((2, 128, 16, 8), np.float32)
    for li in range(2):
        for g3, nm in enumerate(("ml_w_q", "ml_w_k", "ml_w_v")):
            bd[li, g3 * 16:(g3 + 1) * 16] = _block_diag(np.asarray(inp[nm][li], np.float32))
        cw = np.asarray(inp["ml_conv_w"][li], np.float32)
        for tap in range(4):
            vec[li, :, :, tap] = cw[tap].reshape(16, 128).T
        vec[li, :, :, 4] = np.asarray(inp["ml_conv_b"][li], np.float32).reshape(16, 128).T
        vec[li, :, :, 5] = np.asarray(inp["ml_ln_w"][li], np.float32).reshape(16, 128).T
        vec[li, :, :, 6] = np.asarray(inp["ml_skip"][li], np.float32).reshape(16, 128).T
    d["ml_bd"] = bd
    d["ml_vec"] = vec
    d["ml_wif"] = f(np.asarray(inp["ml_w_if"], np.float32).reshape(2, 48, 128, 8).transpose(0, 2, 1, 3))
    d["ml_bif"] = f(np.asarray(inp["ml_b_if"], np.float32).reshape(2, 8, 1))
    d["ml_w_out"] = f(inp["ml_w_out"])
    d["final_norm"] = f(np.asarray(inp["final_norm"], np.float32).reshape(1, D))
    d["nsa_norm"] = f(inp["nsa_norm"])
    d["nsa_w_in"] = f(inp["nsa_w_in"])
    d["nsa_b_gate"] = f(inp["nsa_b_gate"])
    d["nsa_peT"] = f(np.asarray(inp["nsa_cmp_pe"], np.float32).transpose(0, 1, 3, 2))
    d["nsa_cmp_w1"] = f(inp["nsa_cmp_w1"])
    d["nsa_cmp_w2"] = f(inp["nsa_cmp_w2"])
    d["nsa_w_out"] = f(inp["nsa_w_out"])
    d.update(_nsa_consts())
    d["c_ident"] = np.eye(128, dtype=np.float32)
    d["c_caus"] = np.triu(np.ones((128, 128), np.float32))
    return d


_NC_CACHE = {}


def kernel(**inputs):
    x = np.asarray(inputs["x"], np.float32)
    shared = host_layout(inputs)
    if "nc" not in _NC_CACHE:
        _NC_CACHE["nc"] = build_program()
    nc = _NC_CACHE["nc"]
    in_maps = []
    for c in range(NCORES):
        m = dict(shared)
        m["x"] = np.ascontiguousarray(x[c * NSEQ:(c + 1) * NSEQ])
        in_maps.append(m)
    res = run_bass_kernel_spmd(nc, in_maps, core_ids=list(range(NCORES)))
    return np.concatenate([r["y"] for r in res.results], axis=0)
```

```python
from contextlib import ExitStack
import numpy as np
import concourse.bass as bass
import concourse.mybir as mybir
from concourse.bass_utils import run_bass_kernel_spmd

F32 = mybir.dt.float32
BF16 = mybir.dt.bfloat16
AF = mybir.ActivationFunctionType
ALU = mybir.AluOpType
AX = mybir.AxisListType

S = 2048
D = 1024
NT = S // 128
NSEQ = 2
NCORES = 8
ML_INNER = 2048
NH = 4
DH = 512
RMS_EPS = 1e-6
LN_EPS = 1e-6
NEG = -30000.0


class Tok:
    __slots__ = ("name", "w", "r", "x")

    def __init__(self, name="", x=False):
        self.name = name
        self.w = None
        self.r = {}
        self.x = x


class Prog:
    ENG = ("pe", "dve", "act", "pool", "sp")

    def __init__(self, nc, n_dma_ring=12):
        self.nc = nc
        self.q = {e: [] for e in self.ENG}
        self.sems = {}
        self.cnt = {}
        for e in self.ENG:
            self.sems[e] = nc.alloc_semaphore(name=f"c_{e}")
            self.cnt[e] = 0
        self.ring = {}
        self.ring_pos = {}
        for qn in ("sp", "act", "pool"):
            ks = []
            for i in range(n_dma_ring):
                k = f"d_{qn}{i}"
                self.sems[k] = nc.alloc_semaphore(name=k)
                self.cnt[k] = 0
                ks.append(k)
            self.ring[qn] = ks
            self.ring_pos[qn] = 0
        self.seen = {e: {} for e in self.ENG}
        self.n_ins = 0

    def _deps(self, r, w):
        deps = {}

        def add(k, v):
            if deps.get(k, 0) < v:
                deps[k] = v
        for t in r:
            if t.w is not None:
                add(*t.w)
            if t.x:
                for k, v in t.r.items():
                    add(k, v)
        for t in w:
            if t.w is not None:
                add(*t.w)
            for k, v in t.r.items():
                add(k, v)
        return deps

    def _emit_waits(self, e, deps):
        seen = self.seen[e]
        for k, v in deps.items():
            if k == e and e == "pe":
                continue
            if seen.get(k, 0) >= v:
                continue
            seen[k] = v
            self.q[e].append(("w", self.sems[k], v))

    limit = None

    def op(self, e, fn, r=(), w=()):
        if self.limit is not None and self.n_ins >= self.limit:
            return
        self._emit_waits(e, self._deps(r, w))
        self.cnt[e] += 1
        c = self.cnt[e]
        self.q[e].append(("i", fn, self.sems[e]))
        self.n_ins += 1
        for t in w:
            t.w = (e, c)
            t.r = {}
        for t in r:
            if t.r.get(e, 0) < c:
                t.r[e] = c

    def dma(self, qn, out, in_, r=(), w=()):
        if self.limit is not None and self.n_ins >= self.limit:
            return
        deps = self._deps(r, w)
        k = self.ring[qn][self.ring_pos[qn]]
        self.ring_pos[qn] = (self.ring_pos[qn] + 1) % len(self.ring[qn])
        if self.cnt[k] > 0 and deps.get(k, 0) < self.cnt[k]:
            deps[k] = self.cnt[k]
        self._emit_waits(qn, deps)
        self.cnt[k] += 16
        c = self.cnt[k]
        self.q[qn].append(("d", out, in_, self.sems[k]))
        self.n_ins += 1
        for t in w:
            t.w = (k, c)
            t.r = {}
        for t in r:
            if t.r.get(k, 0) < c:
                t.r[k] = c

    def barrier(self):
        deps = {k: v for k, v in self.cnt.items() if v > 0}
        for e in self.ENG:
            self._emit_waits(e, dict(deps))

    def emit(self):
        nc = self.nc
        with nc.Block() as block:
            def mk(e):
                def body(engine):
                    for it in self.q[e]:
                        if it[0] == "w":
                            engine.wait_ge(it[1], it[2])
                        elif it[0] == "i":
                            it[1](engine).then_inc(it[2], 1)
                        else:
                            engine.dma_start(out=it[1], in_=it[2]).then_inc(it[3], 16)
                return body
            block.tensor(mk("pe"))
            block.vector(mk("dve"))
            block.scalar(mk("act"))
            block.gpsimd(mk("pool"))
            block.sync(mk("sp"))


class Ring:
    def __init__(self, aps):
        self.items = [(a, Tok()) for a in aps]
        self.i = 0

    def next(self):
        it = self.items[self.i]
        self.i = (self.i + 1) % len(self.items)
        return it


class K:
    def __init__(self, nc, nseq):
        self.nc = nc
        self.P = Prog(nc)
        self.nseq = nseq

    def mm(self, out, lhsT, rhs, start, stop, r, w):
        self.P.op("pe", lambda e: e.matmul(out, lhsT=lhsT, rhs=rhs, start=start, stop=stop), r, w)

    def tr(self, out, in_, ident, r, w):
        self.P.op("pe", lambda e: e.transpose(out, in_, ident), r, w)

    def cp(self, eng, out, in_, r, w):
        if eng == "act":
            self.P.op("act", lambda e: e.activation(out, in_, AF.Copy), r, w)
        else:
            self.P.op(eng, lambda e: e.tensor_copy(out, in_), r, w)

    def act(self, out, in_, func, r, w, bias=None, scale=None, accum=None):
        kw = {}
        if bias is not None:
            kw["bias"] = bias
        if scale is not None:
            kw["scale"] = scale
        if accum is not None:
            kw["accum_out"] = accum
        self.P.op("act", lambda e: e.activation(out, in_, func, **kw), r, w)

    def tt(self, eng, out, in0, in1, op, r, w):
        self.P.op(eng, lambda e: e.tensor_tensor(out, in0, in1, op), r, w)

    def ts(self, eng, out, in0, s1, s2, op0, op1, r, w):
        if s2 is None:
            self.P.op(eng, lambda e: e.tensor_scalar(out, in0, s1, None, op0), r, w)
        else:
            self.P.op(eng, lambda e: e.tensor_scalar(out, in0, s1, s2, op0, op1), r, w)

    def stt(self, eng, out, in0, scalar, in1, op0, op1, r, w):
        self.P.op(eng, lambda e: e.scalar_tensor_tensor(out, in0, scalar, in1, op0, op1), r, w)

    def memset(self, eng, out, val, w):
        self.P.op(eng, lambda e: e.memset(out, val), (), w)

    def recip(self, out, in_, r, w):
        self.P.op("dve", lambda e: e.reciprocal(out, in_), r, w)

    def ld(self, out, in_, w, r=()):
        self.P.dma("sp", out, in_, r, w)

    def st(self, out, in_, r, w=()):
        self.P.dma("sp", out, in_, r, w)


def bcast_last(ap, n):
    shp = list(ap.shape)
    return ap.unsqueeze(len(shp)).to_broadcast(shp + [n])


def ml_phase_a(k, li, x_src, T):
    nc, P = k.nc, k.P
    ps, tps = k.ps, k.tps
    with ExitStack() as es:
        def A(name, shape, dt):
            return es.enter_context(nc.sbuf_tensor(f"mA{li}_{name}", shape, dt)).ap()
        w_in = A("win", [128, 8, 4096], BF16)
        t_win = [Tok() for _ in range(8)]
        bd = A("bd", [128, 48, 128], BF16)
        t_bd = Tok()
        vec = A("vec", [128, 16, 8], F32)
        t_vec = Tok()
        wif32 = A("wif32", [128, 48, 8], F32)
        wif = A("wif", [128, 48, 8], BF16)
        t_wif = Tok()
        bif = A("bif", [8, 1], F32)
        t_bif = Tok()
        gb = A("gb", [128, 1024], F32)
        t_gb = Tok()
        stg = Ring([A(f"stg{i}", [128, 2048], F32) for i in range(2)])
        xr_ = Ring([A(f"xt{i}", [128, 1024], F32) for i in range(2)])
        hb_ = Ring([A(f"hb{i}", [128, 1024], BF16) for i in range(2)])
        junk = A("junk", [128, 1024], BF16)
        t_junk = Tok()
        st_ = Ring([A(f"st{i}", [128, 4], F32) for i in range(2)])
        hT = A("hT", [128, 8, 512], BF16)
        t_hT = Tok()
        xin = A("xin", [128, 16, 515], BF16)
        t_xin = [Tok() for _ in range(16)]
        acc_ = Ring([A(f"acc{i}", [128, 512], F32) for i in range(2)])
        xc = A("xc", [128, 16, 512], BF16)
        t_xc = [Tok() for _ in range(16)]
        sx_ = Ring([A(f"sx{i}", [128, 512], BF16) for i in range(2)])
        sz_ = Ring([A(f"sz{i}", [128, 512], BF16) for i in range(2)])
        qkv_ = Ring([A(f"qkv{i}", [128, 512], BF16) for i in range(4)])
        tok_ = Ring([A(f"tokm{i}", [128, 2048], BF16) for i in range(3)])
        gsb = A("gsb", [8, 2, 512], F32)
        t_gsb = Tok()
        gt_ = Ring([A(f"gt{i}", [128, 16], F32) for i in range(2)])

        for kc in range(8):
            for hf in range(2):
                s_ap, s_t = stg.next()
                k.ld(s_ap, k.ml_w_in[li, kc * 128:(kc + 1) * 128, hf * 2048:(hf + 1) * 2048], w=[s_t])
                k.cp(("dve", "pool")[hf], w_in[:, kc, hf * 2048:(hf + 1) * 2048], s_ap, r=[s_t], w=[t_win[kc]])
        for g3 in range(3):
            s_ap, s_t = stg.next()
            sv = s_ap.rearrange("p (c n) -> p c n", c=16)
            k.ld(sv, k.ml_bd[li, g3 * 16:(g3 + 1) * 16].rearrange("c p n -> p c n"), w=[s_t])
            k.cp("dve", bd[:, g3 * 16:(g3 + 1) * 16, :], sv, r=[s_t], w=[t_bd])
        k.ld(vec, k.ml_vec[li], w=[t_vec])
        k.ld(wif32, k.ml_wif[li], w=[t_wif])
        k.ts("dve", wif32[:, 0:16, :], wif32[:, 0:16, :], float(DH ** 0.5), None, ALU.mult, None, r=[t_wif], w=[t_wif])
        k.cp("dve", wif, wif32, r=[t_wif], w=[t_wif])
        k.ld(bif, k.ml_bif[li], w=[t_bif])
        k.ld(gb, k.ml_norm[li].partition_broadcast(128), w=[t_gb])

        for s in range(k.nseq):
            for st in range(4):
                t0 = st * 512
                for sub in range(4):
                    r0 = t0 + sub * 128
                    xt, t_xt = xr_.next()
                    k.ld(xt, x_src[s, r0:r0 + 128, :], w=[t_xt])
                    sv, t_sv = st_.next()
                    k.act(junk, xt, AF.Square, r=[t_xt], w=[t_junk, t_sv], accum=sv[:, 0:1])
                    k.act(sv[:, 1:2], sv[:, 0:1], AF.Ln, r=[t_sv], w=[t_sv], scale=1.0 / D, bias=k.c_eps[:, 0:1])
                    k.act(sv[:, 2:3], sv[:, 1:2], AF.Exp, r=[t_sv], w=[t_sv], scale=-0.5)
                    hb, t_hb = hb_.next()
                    k.stt("dve", hb, xt, sv[:, 2:3], gb, ALU.mult, ALU.mult, r=[t_xt, t_sv, t_gb], w=[t_hb])
                    pv = ps[5].bitcast(BF16).rearrange("p (a t) -> p a t", a=8)
                    for kc in range(8):
                        k.tr(pv[:, kc, :], hb[:, kc * 128:(kc + 1) * 128], k.ident_bf, r=[t_hb], w=[tps[5]])
                    k.cp("dve", hT[:, :, sub * 128:(sub + 1) * 128], pv, r=[tps[5]], w=[t_hT])
                if st == 0:
                    k.memset("pool", xin[:, :, 0:3], 0.0, w=t_xin)
                else:
                    k.cp("pool", xin[:, :, 0:3], xin[:, :, 512:515], r=t_xin, w=t_xin)
                def proj_x(fc):
                    bi = fc % 2
                    for kc in range(8):
                        k.mm(ps[bi], w_in[:, kc, fc * 128:(fc + 1) * 128], hT[:, kc, :], kc == 0, kc == 7,
                             r=[t_win[kc], t_hT], w=[tps[bi]])
                    k.cp("act", xin[:, fc, 3:515], ps[bi], r=[tps[bi]], w=[t_xin[fc]])
                    acc, t_acc = acc_.next()
                    k.ts("dve", acc, xin[:, fc, 0:512], vec[:, fc, 0:1], None, ALU.mult, None,
                         r=[t_xin[fc], t_vec], w=[t_acc])
                    for tap in range(1, 4):
                        k.stt("dve", acc, xin[:, fc, tap:tap + 512], vec[:, fc, tap:tap + 1], acc, ALU.mult, ALU.add,
                              r=[t_xin[fc], t_acc], w=[t_acc])
                    k.act(xc[:, fc, :], acc, AF.Silu, r=[t_acc], w=[t_xc[fc]], bias=vec[:, fc, 4:5])
                    sx, t_sx = sx_.next()
                    k.ts("pool", sx, xc[:, fc, :], vec[:, fc, 6:7], None, ALU.mult, None, r=[t_xc[fc]], w=[t_sx])
                    k.st(T["sxT"][s, fc * 128:(fc + 1) * 128, t0:t0 + 512], sx, r=[t_sx])

                def qkv(fc):
                    for which in range(3):
                        pb = 2 + (which % 2)
                        src = xc[:, fc, :] if which < 2 else xin[:, fc, 3:515]
                        tsrc = t_xc[fc] if which < 2 else t_xin[fc]
                        k.mm(ps[pb], bd[:, which * 16 + fc, :], src, True, True, r=[t_bd, tsrc], w=[tps[pb]])
                        qt, t_qt = qkv_.next()
                        if which == 0:
                            k.act(qt, ps[pb], AF.Copy, r=[tps[pb]], w=[t_qt], scale=float(DH ** -0.5))
                            k.st(T["qT"][s, fc * 128:(fc + 1) * 128, t0:t0 + 512], qt, r=[t_qt])
                        elif which == 1:
                            k.cp("dve", qt, ps[pb], r=[tps[pb]], w=[t_qt])
                            k.st(T["kT"][s, fc * 128:(fc + 1) * 128, t0:t0 + 512], qt, r=[t_qt])
                        else:
                            k.cp("dve", qt, ps[pb], r=[tps[pb]], w=[t_qt])
                        first = (fc == 0 and which == 0)
                        last = (fc == 15 and which == 2)
                        k.mm(ps[4][0:8, :], wif[:, which * 16 + fc, :], qt, first, last, r=[t_wif, t_qt], w=[tps[4]])

                def proj_z(fc):
                    bi = fc % 2
                    for kc in range(8):
                        k.mm(ps[bi], w_in[:, kc, fc * 128:(fc + 1) * 128], hT[:, kc, :], kc == 0, kc == 7,
                             r=[t_win[kc], t_hT], w=[tps[bi]])
                    sz, t_sz = sz_.next()
                    k.act(sz, ps[bi], AF.Silu, r=[tps[bi]], w=[t_sz])
                    k.st(T["szT"][s, (fc - 16) * 128:(fc - 15) * 128, t0:t0 + 512], sz, r=[t_sz])

                for fc in range(32):
                    if fc < 16:
                        proj_x(fc)
                    else:
                        proj_z(fc)
                    if 2 <= fc < 18:
                        qkv(fc - 2)
                for which in (1, 2):
                    for sub in range(4):
                        tm, t_tm = tok_.next()
                        for fg in range(4):
                            pb = 6 + (fg % 2)
                            for f4 in range(4):
                                fc = fg * 4 + f4
                                if which == 1:
                                    lhsT = xc[:, fc, sub * 128:(sub + 1) * 128]
                                    tsrc = t_xc[fc]
                                else:
                                    lhsT = xin[:, fc, 3 + sub * 128:3 + (sub + 1) * 128]
                                    tsrc = t_xin[fc]
                                k.mm(ps[pb][:, f4 * 128:(f4 + 1) * 128], lhsT, bd[:, which * 16 + fc, :], True, True,
                                     r=[t_bd, tsrc], w=[tps[pb]])
                            k.cp(("act", "dve")[fg % 2], tm[:, fg * 512:(fg + 1) * 512], ps[pb], r=[tps[pb]], w=[t_tm])
                        dst = T["ktok"] if which == 1 else T["vtok"]
                        r0 = t0 + sub * 128
                        k.st(dst[s, r0:r0 + 128, :], tm, r=[t_tm])
                k.act(gsb[:, 0, :], ps[4][0:8, :], AF.Identity, r=[tps[4], t_bif], w=[t_gsb], bias=bif[:, 0:1])
                k.act(gsb[:, 1, :], gsb[:, 0, :], AF.Exp, r=[t_gsb], w=[t_gsb], scale=-1.0)
                k.act(gsb[:, 1, :], gsb[:, 1, :], AF.Ln, r=[t_gsb], w=[t_gsb], bias=k.c_one[0:8, 0:1])
                for sub in range(4):
                    for a in range(2):
                        k.tr(ps[5][:, a * 8:(a + 1) * 8], gsb[:, a, sub * 128:(sub + 1) * 128], k.ident_f[0:8, 0:8],
                             r=[t_gsb], w=[tps[5]])
                    gt, t_gt = gt_.next()
                    k.cp("dve", gt, ps[5][:, 0:16], r=[tps[5]], w=[t_gt])
                    r0 = t0 + sub * 128
                    k.st(T["gtok"][s, r0:r0 + 128, :], gt, r=[t_gt])
    P.barrier()


def ml_phase_b(k, li, x_src, x_dst, T):
    nc, P = k.nc, k.P
    ps, tps = k.ps, k.tps
    with ExitStack() as es:
        def A(name, shape, dt):
            return es.enter_context(nc.sbuf_tensor(f"mB{li}_{name}", shape, dt)).ap()
        w_out = A("wout", [128, 16, 1024], BF16)
        t_wout = Tok()
        vec = A("vec", [128, 16, 8], F32)
        t_vec = Tok()
        stg = Ring([A(f"stg{i}", [128, 2048], F32) for i in range(2)])
        X = A("X", [128, 16, 513], F32)
        t_X = [Tok() for _ in range(4)]
        STb = A("STb", [128, 16, 513], BF16)
        t_STb = [Tok() for _ in range(4)]
        qT_ = Ring([A(f"qT{i}", [128, 16, 128], BF16) for i in range(2)])
        kT_ = Ring([A(f"kT{i}", [128, 16, 128], BF16) for i in range(2)])
        kt_ = Ring([A(f"kt{i}", [128, 2048], BF16) for i in range(2)])
        vt_ = Ring([A(f"vt{i}", [128, 2048], BF16) for i in range(2)])
        gt_ = Ring([A(f"gt{i}", [128, 16], F32) for i in range(2)])
        xr_ = Ring([A(f"xr{i}", [128, 1024], F32) for i in range(2)])
        sx_ = Ring([A(f"sx{i}", [128, 16, 128], BF16) for i in range(2)])
        sz_ = Ring([A(f"sz{i}", [128, 16, 128], BF16) for i in range(2)])
        ve_ = Ring([A(f"ve{i}", [128, 4, 520], BF16) for i in range(2)])
        PT_ = Ring([A(f"PT{i}", [128, 128], BF16) for i in range(4)])
        gm_ = Ring([A(f"gm{i}", [128, 4, 4], F32) for i in range(3)])
        sm_ = Ring([A(f"sm{i}", [128, 8, 4], F32) for i in range(2)])
        mv_ = Ring([A(f"mv{i}", [128, 4, 2], F32) for i in range(2)])
        bs_ = Ring([A(f"bs{i}", [128, 6], F32) for i in range(4)])
        hn_ = Ring([A(f"hn{i}", [128, 2048], BF16) for i in range(2)])
        t1 = A("t1", [128, 16, 128], F32)
        t_t1 = Tok()
        gT_ = Ring([A(f"gT{i}", [128, 16, 128], BF16) for i in range(2)])
        yo_ = Ring([A(f"yo{i}", [128, 1024], F32) for i in range(2)])

        for fc in range(0, 16, 2):
            s_ap, s_t = stg.next()
            sv = s_ap.rearrange("p (c n) -> p c n", c=2)
            k.ld(sv, k.ml_w_out[li, fc * 128:(fc + 2) * 128, :].rearrange("(c p) n -> p c n", p=128), w=[s_t])
            k.cp("dve", w_out[:, fc:fc + 2, :], sv, r=[s_t], w=[t_wout])
        k.ld(vec, k.ml_vec[li], w=[t_vec])

        def issue_loads(s, c):
            r0 = c * 128
            d = {}
            for nm, ring in (("gt", gt_), ("kT", kT_), ("qT", qT_), ("vt", vt_), ("kt", kt_), ("sx", sx_), ("sz", sz_),
                             ("xr", xr_)):
                d[nm] = ring.next()
            k.ld(d["gt"][0], T["gtok"][s, r0:r0 + 128, :], w=[d["gt"][1]])
            k.ld(d["kT"][0], T["kT"][s, :, r0:r0 + 128].rearrange("(a p) t -> p a t", p=128), w=[d["kT"][1]])
            k.ld(d["qT"][0], T["qT"][s, :, r0:r0 + 128].rearrange("(a p) t -> p a t", p=128), w=[d["qT"][1]])
            k.ld(d["vt"][0], T["vtok"][s, r0:r0 + 128, :], w=[d["vt"][1]])
            k.ld(d["kt"][0], T["ktok"][s, r0:r0 + 128, :], w=[d["kt"][1]])
            k.ld(d["sx"][0], T["sxT"][s, :, r0:r0 + 128].rearrange("(a p) t -> p a t", p=128), w=[d["sx"][1]])
            k.ld(d["sz"][0], T["szT"][s, :, r0:r0 + 128].rearrange("(a p) t -> p a t", p=128), w=[d["sz"][1]])
            k.ld(d["xr"][0], x_src[s, r0:r0 + 128, :], w=[d["xr"][1]])
            return d

        work = [(s, c) for s in range(k.nseq) for c in range(NT)]
        nxt = issue_loads(*work[0])
        prev_gm = None
        for wi, (s, c) in enumerate(work):
            r0 = c * 128
            L = nxt
            if wi + 1 < len(work):
                nxt = issue_loads(*work[wi + 1])
            (gt, t_gt), (kT, t_kT), (qT, t_qT), (vt, t_vt) = L["gt"], L["kT"], L["qT"], L["vt"]
            (kt, t_kt), (sx, t_sx), (sz, t_sz), (xr, t_xr) = L["kt"], L["sx"], L["sz"], L["xr"]
            if c == 0:
                prev_gm = None
            gm, t_gm = gm_.next()
            k.mm(ps[0][:, 0:4], k.caus_f, gt[:, 12:16], True, True, r=[t_gt], w=[tps[0]])
            k.mm(ps[0][:, 4:8], k.ones_f, gt[:, 12:16], True, True, r=[t_gt], w=[tps[0]])
            k.tt("dve", gm[:, 3, :], ps[0][:, 0:4], gt[:, 0:4], ALU.add, r=[tps[0], t_gt], w=[t_gm])
            k.act(gm[:, 0, :], gm[:, 3, :], AF.Exp, r=[t_gm], w=[t_gm])
            k.act(gm[:, 1:3, :], ps[0][:, 0:8].rearrange("p (a b) -> p a b", a=2), AF.Exp, r=[tps[0]], w=[t_gm],
                  scale=-1.0)
            ve, t_ve = ve_.next()
            for h in range(NH):
                k.act(ve[:, h, 0:512], vt[:, h * 512:(h + 1) * 512], AF.Copy, r=[t_vt, t_gm], w=[t_ve],
                      scale=gm[:, 0, h:h + 1])
            k.cp("dve", ve[:, :, 512], gm[:, 0, :], r=[t_gm], w=[t_ve])
            for h in range(NH):
                for dc in range(4):
                    k.mm(ps[1][:, 0:128], kT[:, h * 4 + dc, :], qT[:, h * 4 + dc, :], dc == 0, dc == 3,
                         r=[t_kT, t_qT], w=[tps[1]])
                PT, t_PT = PT_.next()
                k.tt("dve", PT, ps[1][:, 0:128], k.caus_f, ALU.mult, r=[tps[1]], w=[t_PT])
                nb = 2 + h
                k.mm(ps[nb], PT, ve[:, h, 0:512], True, c == 0, r=[t_PT, t_ve], w=[tps[nb]])
                if c > 0:
                    for dc in range(4):
                        k.mm(ps[nb], qT[:, h * 4 + dc, :], STb[:, h * 4 + dc, 0:512], False, dc == 3,
                             r=[t_qT, t_STb[h]], w=[tps[nb]])
                k.mm(ps[0][:, 8 + h:9 + h], PT, ve[:, h, 512:513], True, c == 0, r=[t_PT, t_ve], w=[tps[0]])
                if c > 0:
                    for dc in range(4):
                        k.mm(ps[0][:, 8 + h:9 + h], qT[:, h * 4 + dc, :], STb[:, h * 4 + dc, 512:513], False, dc == 3,
                             r=[t_qT, t_STb[h]], w=[tps[0]])
            for h in range(NH):
                for dc in range(4):
                    ub = 6 + (dc % 2)
                    lhsT = kt[:, h * 512 + dc * 128:h * 512 + (dc + 1) * 128]
                    k.mm(ps[ub], lhsT, ve[:, h, 0:512], True, True, r=[t_kt, t_ve], w=[tps[ub]])
                    k.mm(ps[0][:, 16 + h * 4 + dc:17 + h * 4 + dc], lhsT, ve[:, h, 512:513], True, True,
                         r=[t_kt, t_ve], w=[tps[0]])
                    if c == 0:
                        k.cp("dve", X[:, h * 4 + dc, 0:512], ps[ub], r=[tps[ub]], w=[t_X[h]])
                    else:
                        k.stt("dve", X[:, h * 4 + dc, 0:512], X[:, h * 4 + dc, 0:512], prev_gm[0][:, 2, h:h + 1], ps[ub],
                              ALU.mult, ALU.add, r=[tps[ub], prev_gm[1], t_X[h]], w=[t_X[h]])
                xn = X[:, h * 4:(h + 1) * 4, 512:513].rearrange("p a b -> p (a b)")
                if c == 0:
                    k.cp("dve", xn, ps[0][:, 16 + h * 4:20 + h * 4], r=[tps[0]], w=[t_X[h]])
                else:
                    k.stt("dve", xn, xn, prev_gm[0][:, 2, h:h + 1], ps[0][:, 16 + h * 4:20 + h * 4],
                          ALU.mult, ALU.add, r=[tps[0], prev_gm[1], t_X[h]], w=[t_X[h]])
                if c < NT - 1:
                    for dc in range(4):
                        k.act(STb[:, h * 4 + dc, :], X[:, h * 4 + dc, :], AF.Copy, r=[t_X[h], t_gm], w=[t_STb[h]],
                              scale=gm[:, 2, h:h + 1])
            prev_gm = (gm, t_gm)
            sm, t_sm = sm_.next()
            mv, t_mv = mv_.next()
            for h in range(NH):
                bs, t_bs = bs_.next()
                k.P.op("dve", (lambda o, i: (lambda e: e.bn_stats(o, i)))(bs, ps[2 + h]), r=[tps[2 + h]], w=[t_bs])
                k.P.op("dve", (lambda o, i: (lambda e: e.bn_aggr(o, i)))(mv[:, h, :], bs), r=[t_bs], w=[t_mv])
            k.tt("dve", sm[:, 0, :], ps[0][:, 8:12], gm[:, 1, :], ALU.mult, r=[tps[0], t_gm], w=[t_sm])
            k.stt("dve", sm[:, 1, :], sm[:, 0, :], -1.0, sm[:, 0, :], ALU.mult, ALU.max, r=[t_sm], w=[t_sm])
            k.ts("dve", sm[:, 1, :], sm[:, 1, :], 1.0, None, ALU.max, None, r=[t_sm], w=[t_sm])
            k.recip(sm[:, 2, :], sm[:, 1, :], r=[t_sm], w=[t_sm])
            k.tt("dve", sm[:, 3, :], gm[:, 1, :], sm[:, 2, :], ALU.mult, r=[t_sm, t_gm], w=[t_sm])
            k.tt("dve", sm[:, 4, :], sm[:, 3, :], sm[:, 3, :], ALU.mult, r=[t_sm], w=[t_sm])
            k.tt("dve", sm[:, 4, :], sm[:, 4, :], mv[:, :, 1], ALU.mult, r=[t_sm, t_mv], w=[t_sm])
            k.act(sm[:, 5, :], sm[:, 4, :], AF.Ln, r=[t_sm], w=[t_sm], bias=k.c_eps[:, 0:1])
            k.act(sm[:, 5, :], sm[:, 5, :], AF.Exp, r=[t_sm], w=[t_sm], scale=-0.5)
            k.tt("dve", sm[:, 6, :], sm[:, 5, :], sm[:, 3, :], ALU.mult, r=[t_sm], w=[t_sm])
            k.stt("dve", sm[:, 7, :], mv[:, :, 0], -1.0, sm[:, 6, :], ALU.mult, ALU.mult, r=[t_sm, t_mv], w=[t_sm])
            hn, t_hn = hn_.next()
            for h in range(NH):
                k.act(hn[:, h * 512:(h + 1) * 512], ps[2 + h], AF.Identity, r=[tps[2 + h], t_sm], w=[t_hn],
                      scale=sm[:, 6, h:h + 1], bias=sm[:, 7, h:h + 1])
            for half in range(2):
                pb = 2 + half
                pv = ps[pb].bitcast(BF16).rearrange("p (a t) -> p a t", a=8)
                for a in range(8):
                    fc = half * 8 + a
                    k.tr(pv[:, a, :], hn[:, fc * 128:(fc + 1) * 128], k.ident_bf, r=[t_hn], w=[tps[pb]])
                k.tt("dve", t1[:, half * 8:(half + 1) * 8, :], pv, bcast_last(vec[:, half * 8:(half + 1) * 8, 5], 128),
                     ALU.mult, r=[tps[pb], t_vec], w=[t_t1])
            gT, t_gT = gT_.next()
            k.tt("pool", t1, t1, sx, ALU.add, r=[t_sx, t_t1], w=[t_t1])
            k.tt("pool", gT, t1, sz, ALU.mult, r=[t_sz, t_t1], w=[t_gT])
            yo, t_yo = yo_.next()
            for half in range(2):
                pb = 4 + half
                for fc in range(16):
                    k.mm(ps[pb], gT[:, fc, :], w_out[:, fc, half * 512:(half + 1) * 512], fc == 0, fc == 15,
                         r=[t_gT, t_wout], w=[tps[pb]])
                k.tt("dve", yo[:, half * 512:(half + 1) * 512], ps[pb], xr[:, half * 512:(half + 1) * 512], ALU.add,
                     r=[tps[pb], t_xr], w=[t_yo])
            k.st(x_dst[s, r0:r0 + 128, :], yo, r=[t_yo])
    P.barrier()


NSA_FM = [(0, 1024), (1024, 1280), (1280, 1536), (1536, 1792), (2048, 2304)]
SCALE = 0.125


def nsa_phase_a(k, li, x_src, T):
    nc, P = k.nc, k.P
    ps, tps = k.ps, k.tps
    with ExitStack() as es:
        def A(name, shape, dt):
            return es.enter_context(nc.sbuf_tensor(f"nA{li}_{name}", shape, dt)).ap()
        w_in = A("win", [128, 8, 3648], BF16)
        t_win = [Tok() for _ in range(8)]
        gb = A("gb", [128, 1024], F32)
        t_gb = Tok()
        bg = A("bg", [128, 48], F32)
        t_bg = Tok()
        rot = A("rot", [128, 128], BF16)
        cosf = A("cosf", [128, S], F32)
        sinf = A("sinf", [128, S], F32)
        t_c = Tok()
        stg = Ring([A(f"stg{i}", [128, 1816], F32) for i in range(2)])
        xr_ = Ring([A(f"xt{i}", [128, 1024], F32) for i in range(2)])
        hb_ = Ring([A(f"hb{i}", [128, 1024], BF16) for i in range(2)])
        junk = A("junk", [128, 1024], BF16)
        t_junk = Tok()
        st_ = Ring([A(f"st{i}", [128, 4], F32) for i in range(2)])
        hT = A("hT", [128, 8, 512], BF16)
        t_hT = Tok()
        xb_ = Ring([A(f"xb{i}", [128, 512], BF16) for i in range(3)])
        ta_ = Ring([A(f"ta{i}", [128, 512], F32) for i in range(2)])
        tb_ = Ring([A(f"tb{i}", [128, 512], F32) for i in range(2)])
        ro_ = Ring([A(f"ro{i}", [128, 512], BF16) for i in range(3)])
        tm_ = Ring([A(f"tm{i}", [128, 1536], BF16) for i in range(2)])
        gl_ = Ring([A(f"gl{i}", [128, 48], F32) for i in range(2)])

        for kc in range(8):
            for hf in range(2):
                s_ap, s_t = stg.next()
                k.ld(s_ap, k.nsa_w_in[li, kc * 128:(kc + 1) * 128, hf * 1816:(hf + 1) * 1816], w=[s_t])
                k.cp(("dve", "pool")[hf], w_in[:, kc, hf * 1816:(hf + 1) * 1816], s_ap, r=[s_t], w=[t_win[kc]])
        k.ld(gb, k.nsa_norm[li].partition_broadcast(128), w=[t_gb])
        k.ld(bg, k.nsa_b_gate[li].partition_broadcast(128), w=[t_bg])
        s_ap, s_t = stg.next()
        k.ld(s_ap[:, 0:128], k.c_rot, w=[s_t])
        k.cp("dve", rot, s_ap[:, 0:128], r=[s_t], w=[t_c])
        k.ld(cosf, k.c_cos, w=[t_c])
        k.ld(sinf, k.c_sin, w=[t_c])

        fm = []
        for c8 in range(8):
            fm.append((c8 * 128, "qT", c8 * 128, True))
        for c2 in range(2):
            fm.append((1024 + c2 * 128, "kcT", c2 * 128, False))
            fm.append((1280 + c2 * 128, "vcT", c2 * 128, False))
            fm.append((1536 + c2 * 128, "ksT", c2 * 128, True))
            fm.append((2048 + c2 * 128, "kwT", c2 * 128, True))

        for s in range(k.nseq):
            for st in range(4):
                t0 = st * 512
                for sub in range(4):
                    r0 = t0 + sub * 128
                    xt, t_xt = xr_.next()
                    k.ld(xt, x_src[s, r0:r0 + 128, :], w=[t_xt])
                    sv, t_sv = st_.next()
                    k.act(junk, xt, AF.Square, r=[t_xt], w=[t_junk, t_sv], accum=sv[:, 0:1])
                    k.act(sv[:, 1:2], sv[:, 0:1], AF.Ln, r=[t_sv], w=[t_sv], scale=1.0 / D, bias=k.c_eps[:, 0:1])
                    k.act(sv[:, 2:3], sv[:, 1:2], AF.Exp, r=[t_sv], w=[t_sv], scale=-0.5)
                    hb, t_hb = hb_.next()
                    k.stt("dve", hb, xt, sv[:, 2:3], gb, ALU.mult, ALU.mult, r=[t_xt, t_sv, t_gb], w=[t_hb])
                    pv = ps[5].bitcast(BF16).rearrange("p (a t) -> p a t", a=8)
                    for kc in range(8):
                        k.tr(pv[:, kc, :], hb[:, kc * 128:(kc + 1) * 128], k.ident_bf, r=[t_hb], w=[tps[5]])
                    k.cp("dve", hT[:, :, sub * 128:(sub + 1) * 128], pv, r=[tps[5]], w=[t_hT])
                for i, (c0, dst, d0, rope) in enumerate(fm):
                    bi = i % 2
                    for kc in range(8):
                        k.mm(ps[bi], w_in[:, kc, c0:c0 + 128], hT[:, kc, :], kc == 0, kc == 7,
                             r=[t_win[kc], t_hT], w=[tps[bi]])
                    xb, t_xb = xb_.next()
                    k.cp("act", xb, ps[bi], r=[tps[bi]], w=[t_xb])
                    if dst == "qT" or not rope:
                        k.st(T[dst][s, d0:d0 + 128, t0:t0 + 512], xb, r=[t_xb])
                    if rope:
                        pb = 2 + (i % 2)
                        k.mm(ps[pb], rot, xb, True, True, r=[t_c, t_xb], w=[tps[pb]])
                        ta, t_ta = ta_.next()
                        tb, t_tb = tb_.next()
                        k.tt("dve", ta, ps[bi], cosf[:, t0:t0 + 512], ALU.mult, r=[tps[bi], t_c, t_xb], w=[t_ta])
                        k.tt("dve", tb, ps[pb], sinf[:, t0:t0 + 512], ALU.mult, r=[tps[pb], t_c], w=[t_tb])
                        ro, t_ro = ro_.next()
                        k.tt("pool", ro, ta, tb, ALU.add, r=[t_ta, t_tb], w=[t_ro])
                        rd = "qrT" if dst == "qT" else dst
                        k.st(T[rd][s, d0:d0 + 128, t0:t0 + 512], ro, r=[t_ro])
                for sub in range(4):
                    r0 = t0 + sub * 128
                    tm, t_tm = tm_.next()
                    lh = [hT[:, kc, sub * 128:(sub + 1) * 128] for kc in range(8)]
                    for half, c0 in enumerate((1792, 2304)):
                        for kc in range(8):
                            k.mm(ps[6][:, half * 256:(half + 1) * 256], lh[kc], w_in[:, kc, c0:c0 + 256], kc == 0, kc == 7,
                                 r=[t_win[kc], t_hT], w=[tps[6]])
                    k.cp("dve", tm[:, 0:512], ps[6], r=[tps[6]], w=[t_tm])
                    for zi in range(2):
                        pb = 7 if zi == 0 else 6
                        c0 = 2560 + zi * 512
                        for kc in range(8):
                            k.mm(ps[pb], lh[kc], w_in[:, kc, c0:c0 + 512], kc == 0, kc == 7,
                                 r=[t_win[kc], t_hT], w=[tps[pb]])
                        k.act(tm[:, 512 + zi * 512:1024 + zi * 512], ps[pb], AF.Silu, r=[tps[pb]], w=[t_tm])
                    for kc in range(8):
                        k.mm(ps[7][:, 0:48], lh[kc], w_in[:, kc, 3584:3632], kc == 0, kc == 7,
                             r=[t_win[kc], t_hT], w=[tps[7]])
                    gl, t_gl = gl_.next()
                    k.tt("dve", gl, ps[7][:, 0:48], bg, ALU.add, r=[tps[7], t_bg], w=[t_gl])
                    k.act(gl, gl, AF.Sigmoid, r=[t_gl], w=[t_gl])
                    k.st(T["vsw"][s, r0:r0 + 128, :], tm[:, 0:512], r=[t_tm])
                    k.st(T["szt"][s, r0:r0 + 128, :], tm[:, 512:1536], r=[t_tm])
                    k.st(T["gate"][s, r0:r0 + 128, :], gl, r=[t_gl])
    P.barrier()


def nsa_phase_c(k, li, x_src, x_dst, T):
    nc, P = k.nc, k.P
    ps, tps = k.ps, k.tps
    with ExitStack() as es0:
        def A0(name, shape, dt):
            return es0.enter_context(nc.sbuf_tensor(f"nC{li}_{name}", shape, dt)).ap()
        w_out = A0("wout", [128, 8, 1024], BF16)
        t_wout = Tok()
        kcmpT = A0("kcmpT", [128, k.nseq, 4, 128], BF16)
        vcmp = A0("vcmp", [128, k.nseq, 4, 128], BF16)
        t_cmp = Tok()
        with ExitStack() as es:
            def A(name, shape, dt):
                return es.enter_context(nc.sbuf_tensor(f"nB{li}_{name}", shape, dt)).ap()
            stg = Ring([A(f"stg{i}", [128, 2048], F32) for i in range(2)])
            w1 = A("w1", [128, 2, 32, 128], BF16)
            w2k = A("w2k", [128, 128], BF16)
            w2v = A("w2v", [128, 64], BF16)
            peT = A("peT", [128, 2, 32], BF16)
            bh = A("bh", [128, 2], F32)
            t_w = Tok()
            kc2 = A("kc2", [128, 2, S], BF16)
            vc2 = A("vc2", [128, 2, S], BF16)
            t_kv = Tok()
            hTs_ = Ring([A(f"hTs{i}", [128, 128], BF16) for i in range(2)])
            for c8 in range(0, 8, 2):
                s_ap, s_t = stg.next()
                sv = s_ap.rearrange("p (c n) -> p c n", c=2)
                k.ld(sv, k.nsa_w_out[li, c8 * 128:(c8 + 2) * 128, :].rearrange("(c p) n -> p c n", p=128), w=[s_t])
                k.cp("dve", w_out[:, c8:c8 + 2, :], sv, r=[s_t], w=[t_wout])
            for kv in range(2):
                for hf in range(2):
                    s_ap, s_t = stg.next()
                    sv = s_ap.rearrange("p (l n) -> p l n", l=16)
                    src = k.nsa_cmp_w1[li, kv, hf * 1024:(hf + 1) * 1024, :].rearrange("(l d) n -> d l n", d=64)
                    k.ld(sv[0:64], src, w=[s_t])
                    k.ld(sv[64:128], src, w=[s_t])
                    k.cp("dve", w1[:, kv, hf * 16:(hf + 1) * 16, :], sv, r=[s_t], w=[t_w])
            s_ap, s_t = stg.next()
            k.ld(s_ap[:, 0:64], k.nsa_cmp_w2[li, 0], w=[s_t])
            k.ld(s_ap[:, 64:128], k.nsa_cmp_w2[li, 1], w=[s_t])
            pe_v = s_ap[:, 128:192].rearrange("p (a l) -> p a l", a=2)
            k.ld(pe_v[0:64], k.nsa_peT[li].rearrange("a d l -> d a l"), w=[s_t])
            k.ld(pe_v[64:128], k.nsa_peT[li].rearrange("a d l -> d a l"), w=[s_t])
            k.cp("dve", w2k[:, 0:64], s_ap[:, 0:64], r=[s_t], w=[t_w])
            k.cp("dve", w2k[:, 64:128], s_ap[:, 0:64], r=[s_t], w=[t_w])
            k.cp("dve", w2v, s_ap[:, 64:128], r=[s_t], w=[t_w])
            k.cp("dve", peT, pe_v, r=[s_t], w=[t_w])
            for kv in range(2):
                for l in range(32):
                    k.mm(ps[7][:, kv:kv + 1], w1[0:64, kv, l, :], peT[0:64, kv, l:l + 1], l == 0, l == 31, r=[t_w], w=[tps[7]])
            k.cp("dve", bh, ps[7][:, 0:2], r=[tps[7]], w=[t_w])
            for s in range(k.nseq):
                k.ld(vcmp[:, s, :, 64:97], k.c_ovl.rearrange("p (g n) -> p g n", g=4), w=[t_cmp])
                k.ld(kc2, T["kcT"][s].rearrange("(c p) t -> p c t", p=128), w=[t_kv])
                k.ld(vc2, T["vcT"][s].rearrange("(c p) t -> p c t", p=128), w=[t_kv])
                for kv in range(2):
                    src = kc2 if kv == 0 else vc2
                    for g in range(4):
                        base = (g % 2) * 64
                        for l in range(32):
                            k.mm(ps[6][:, 0:127], w1[base:base + 64, kv, l, :], src[base:base + 64, g // 2, l:l + 16 * 126 + 1:16],
                                 l == 0, l == 31, r=[t_w, t_kv], w=[tps[6]])
                        hTs, t_hTs = hTs_.next()
                        k.act(hTs[:, 0:127], ps[6][:, 0:127], AF.Silu, r=[tps[6], t_w], w=[t_hTs], bias=bh[:, kv:kv + 1])
                        if kv == 0:
                            k.mm(ps[7][:, 0:127], w2k, hTs[:, 0:127], True, True, r=[t_w, t_hTs], w=[tps[7]])
                            k.cp("dve", kcmpT[:, s, g, 0:127], ps[7][:, 0:127], r=[tps[7]], w=[t_cmp])
                        else:
                            k.mm(ps[7][0:127, 0:64], hTs[:, 0:127], w2v, True, True, r=[t_w, t_hTs], w=[tps[7]])
                            k.cp("dve", vcmp[0:127, s, g, 0:64], ps[7][0:127, 0:64], r=[tps[7]], w=[t_cmp])
        P.barrier()
        with ExitStack() as es:
            def A(name, shape, dt):
                return es.enter_context(nc.sbuf_tensor(f"nC{li}_{name}", shape, dt)).ap()
            t_c = Tok()
            Ef = A("Ef", [128, 4, S], BF16)
            causn = A("causn", [128, 4, 512], BF16)
            lown = A("lown", [128, 4, 512], BF16)
            cmpn = A("cmpn", [128, S], BF16)
            fmul = A("fmul", [128, 16, 32], F32)
            fadd = A("fadd", [128, 16, 32], F32)
            ks2 = A("ks2", [128, 4, S], BF16)
            kw2 = A("kw2", [128, 4, S], BF16)
            vs = A("vs", [128, 16, 4, 65], BF16)
            vw = A("vw", [128, 16, 4, 65], BF16)
            t_res = Tok()
            q_ = Ring([A(f"q{i}", [128, 8, 512], BF16) for i in range(1)])
            qr_ = Ring([A(f"qr{i}", [128, 8, 512], BF16) for i in range(1)])
            ex_ = Ring([A(f"ex{i}", [128, 512], BF16) for i in range(3)])
            oacc = A("oacc", [128, 4, 16, 64], F32)
            t_oacc = [Tok() for _ in range(4)]
            ucmp = A("ucmp", [128, 4, 4, 33], F32)
            rdc = A("rdc", [128, 4, 4], F32)
            t_ucmp = Tok()
            tmp_ = Ring([A(f"tmp{i}", [128, 4, 64], F32) for i in range(3)])
            imp_ = Ring([A(f"imp{i}", [128, 32], F32) for i in range(2)])
            rk_ = Ring([A(f"rk{i}", [128, 32, 32], F32) for i in range(2)])
            rs_ = Ring([A(f"rs{i}", [128, 32], F32) for i in range(2)])
            rd_ = Ring([A(f"rd{i}", [128, 16], F32) for i in range(4)])
            negm = A("negm", [128, 4, 4, 32], BF16)
            t_negm = [Tok() for _ in range(4)]
            negT = A("negT", [128, 512], BF16)
            t_negT = Tok()
            gat_ = Ring([A(f"gat{i}", [128, 4, 48], F32) for i in range(2)])
            szt_ = Ring([A(f"szt{i}", [128, 1024], BF16) for i in range(2)])
            xr_ = Ring([A(f"xr{i}", [128, 1024], F32) for i in range(2)])
            og_ = Ring([A(f"og{i}", [128, 1024], BF16) for i in range(2)])
            ogT_ = Ring([A(f"ogT{i}", [128, 8, 128], BF16) for i in range(2)])
            yo_ = Ring([A(f"yo{i}", [128, 1024], F32) for i in range(2)])

            k.ld(Ef, k.c_E.rearrange("p (g n) -> p g n", g=4), w=[t_c])
            k.ld(causn, k.c_causn.rearrange("p (a n) -> p a n", a=4), w=[t_c])
            k.ld(lown, k.c_lown.rearrange("p (a n) -> p a n", a=4), w=[t_c])
            k.ld(cmpn, k.c_cmpn, w=[t_c])
            k.ld(fmul, k.c_fmul, w=[t_c])
            k.ld(fadd, k.c_fadd, w=[t_c])
            k.memset("pool", vs[:, :, :, 64:65], 1.0, w=[t_res])
            k.memset("pool", vw[:, :, :, 64:65], 1.0, w=[t_res])

            def evac(pov, h, b, first, gat, t_gat, clamp, g):
                rd, t_rd = rd_.next()
                if clamp:
                    k.ts("dve", rd[:, 0:4], pov[:, :, 64], 1e-30, None, ALU.max, None, r=[tps_of[0]], w=[t_rd])
                else:
                    k.cp("dve", rd[:, 0:4], pov[:, :, 64], r=[tps_of[0]], w=[t_rd])
                k.recip(rd[:, 4:8], rd[:, 0:4], r=[t_rd], w=[t_rd])
                k.tt("dve", rd[:, 8:12], rd[:, 4:8], gat[:, :, 3 * h + b], ALU.mult, r=[t_rd, t_gat], w=[t_rd])
                if first:
                    k.tt("dve", oacc[:, :, h, :], pov[:, :, 0:64], bcast_last(rd[:, 8:12], 64), ALU.mult,
                         r=[tps_of[0], t_rd], w=[t_oacc[g]])
                else:
                    tmp, t_tmp = tmp_.next()
                    k.tt("dve", tmp, pov[:, :, 0:64], bcast_last(rd[:, 8:12], 64), ALU.mult, r=[tps_of[0], t_rd], w=[t_tmp])
                    k.tt("pool", oacc[:, :, h, :], oacc[:, :, h, :], tmp, ALU.add, r=[t_tmp, t_oacc[g]], w=[t_oacc[g]])
                return rd, t_rd

            tps_of = [None]
            for s in range(k.nseq):
                for hf in range(2):
                    k.ld(ks2[hf * 64:(hf + 1) * 64], T["ksT"][s].rearrange("(g d) t -> d g t", d=64), w=[t_res])
                    k.ld(kw2[hf * 64:(hf + 1) * 64], T["kwT"][s].rearrange("(g d) t -> d g t", d=64), w=[t_res])
                vsw_v = T["vsw"][s].rearrange("(kt p) (b g d) -> p kt b g d", p=128, b=2, g=4)
                for kt4 in range(0, 16, 4):
                    for g in range(4):
                        k.ld(vs[:, kt4:kt4 + 4, g, 0:64], vsw_v[:, kt4:kt4 + 4, 0, g], w=[t_res])
                        k.ld(vw[:, kt4:kt4 + 4, g, 0:64], vsw_v[:, kt4:kt4 + 4, 1, g], w=[t_res])
                for qt in range(4):
                    t0 = qt * 512
                    q, t_q = q_.next()
                    qr, t_qr = qr_.next()
                    gat, t_gat = gat_.next()
                    k.ld(q, T["qT"][s, :, t0:t0 + 512].rearrange("(c p) t -> p c t", p=128), w=[t_q])
                    k.ld(qr, T["qrT"][s, :, t0:t0 + 512].rearrange("(c p) t -> p c t", p=128), w=[t_qr])
                    k.ld(gat, T["gate"][s, t0:t0 + 512, :].rearrange("(a p) n -> p a n", p=128), w=[t_gat])
                    need_sel = qt >= 2
                    for g in range(4):
                        for hh in range(4):
                            h = 4 * g + hh
                            ch, base = h // 2, (h % 2) * 64
                            sb = h % 2
                            k.mm(ps[sb][0:127, :], kcmpT[base:base + 64, s, g, 0:127], q[base:base + 64, ch, :], True, False,
                                 r=[t_cmp, t_q], w=[tps[sb]])
                            k.mm(ps[sb][0:127, :], k.ident_bf[0:127, 0:127], cmpn[0:127, t0:t0 + 512], False, True,
                                 r=[t_c], w=[tps[sb]])
                            ex, t_ex = ex_.next()
                            k.act(ex[0:127, :], ps[sb][0:127, :], AF.Exp, r=[tps[sb]], w=[t_ex], scale=SCALE)
                            ob = 2 + (h % 2)
                            pov = ps[ob][:, 0:388].rearrange("p (a n) -> p a n", a=4)
                            for sub in range(4):
                                k.mm(pov[:, sub, :], ex[0:127, sub * 128:(sub + 1) * 128], vcmp[0:127, s, g, 0:97], True, True,
                                     r=[t_ex, t_cmp], w=[tps[ob]])
                            tps_of[0] = tps[ob]
                            rd, t_rd = evac(pov, h, 0, True, gat, t_gat, True, g)
                            if need_sel:
                                k.cp("dve", ucmp[:, :, hh, :], pov[:, :, 64:97], r=[tps[ob]], w=[t_ucmp])
                                k.cp("dve", rdc[:, :, hh], rd[:, 4:8], r=[t_rd], w=[t_ucmp])
                        if need_sel:
                            for sub in range(4):
                                imp, t_imp = imp_.next()
                                k.ts("dve", imp, ucmp[:, sub, 0, 1:33], rdc[:, sub, 0:1], None, ALU.mult, None,
                                     r=[t_ucmp], w=[t_imp])
                                for hh in range(1, 4):
                                    k.stt("dve", imp, ucmp[:, sub, hh, 1:33], rdc[:, sub, hh:hh + 1], imp, ALU.mult, ALU.add,
                                          r=[t_ucmp, t_imp], w=[t_imp])
                                tt_ = 4 * qt + sub
                                k.tt("dve", imp, imp, fmul[:, tt_, :], ALU.mult, r=[t_imp, t_c], w=[t_imp])
                                k.tt("dve", imp, imp, fadd[:, tt_, :], ALU.add, r=[t_imp], w=[t_imp])
                                rk, t_rk = rk_.next()
                                in0 = imp.unsqueeze(1).to_broadcast([128, 32, 32])
                                in1 = imp.unsqueeze(2).to_broadcast([128, 32, 32])
                                k.tt("dve", rk, in0, in1, ALU.is_gt, r=[t_imp], w=[t_rk])
                                rs, t_rs = rs_.next()
                                k.P.op("dve", (lambda o, i: (lambda e: e.reduce_sum(o, i, AX.X)))(rs, rk), r=[t_rk], w=[t_rs])
                                k.ts("dve", negm[:, sub, g, :], rs, 15.5, NEG, ALU.is_gt, ALU.mult, r=[t_rs], w=[t_negm[sub]])
                    if need_sel:
                        pv = ps[4].bitcast(BF16)[:, 0:512].rearrange("p (a t) -> p a t", a=4)
                        for sub in range(4):
                            k.tr(pv[:, sub, :], negm[:, sub, :, :].rearrange("p g n -> p (g n)"), k.ident_bf,
                                 r=[t_negm[sub]], w=[tps[4]])
                        k.cp("dve", negT, ps[4].bitcast(BF16)[:, 0:512], r=[tps[4]], w=[t_negT])
                    for br in range(2):
                        kk = ks2 if br == 0 else kw2
                        vv = vs if br == 0 else vw
                        kt_lo = 0 if br == 0 else max(0, 4 * qt - 4)
                        kt_hi = 4 * qt + 3
                        for g in range(4):
                            for hh in range(4):
                                h = 4 * g + hh
                                ch, base = h // 2, (h % 2) * 64
                                ob = 2 + (h % 2)
                                pov = ps[ob][:, 0:260].rearrange("p (a n) -> p a n", a=4)
                                first_pv = True

                                def qk(kt):
                                    sb = kt % 2
                                    r_ = kt - 4 * qt
                                    extra = []
                                    if br == 0 and need_sel:
                                        extra.append((Ef[:, g, kt * 128:(kt + 1) * 128], negT, [t_c, t_negT]))
                                    if r_ >= 0:
                                        extra.append((k.ident_bf, causn[:, r_, :], [t_c]))
                                    elif br == 1:
                                        extra.append((k.ident_bf, lown[:, r_ + 4, :], [t_c]))
                                    k.mm(ps[sb], kk[base:base + 64, g, kt * 128:(kt + 1) * 128], qr[base:base + 64, ch, :],
                                         True, len(extra) == 0, r=[t_res, t_qr], w=[tps[sb]])
                                    for ei, (lt, rh, rt) in enumerate(extra):
                                        k.mm(ps[sb], lt, rh, False, ei == len(extra) - 1, r=rt, w=[tps[sb]])
                                    ex, t_ex = ex_.next()
                                    k.act(ex, ps[sb], AF.Exp, r=[tps[sb]], w=[t_ex], scale=SCALE)
                                    return ex, t_ex

                                pend = qk(kt_lo)
                                for kt in range(kt_lo, kt_hi + 1):
                                    ex, t_ex = pend
                                    if kt + 1 <= kt_hi:
                                        pend = qk(kt + 1)
                                    for sub in range(4):
                                        hi_s = 4 * qt + sub
                                        lo_s = 0 if br == 0 else max(0, hi_s - 4)
                                        if kt < lo_s or kt > hi_s:
                                            continue
                                        k.mm(pov[:, sub, :], ex[:, sub * 128:(sub + 1) * 128], vv[:, kt, g, :],
                                             first_pv, kt == hi_s, r=[t_ex, t_res], w=[tps[ob]])
                                        first_pv = False
                                tps_of[0] = tps[ob]
                                evac(pov, h, 1 + br, False, gat, t_gat, False, g)
                    for sub in range(4):
                        r0 = t0 + sub * 128
                        szt, t_szt = szt_.next()
                        xr, t_xr = xr_.next()
                        k.ld(szt, T["szt"][s, r0:r0 + 128, :], w=[t_szt])
                        k.ld(xr, x_src[s, r0:r0 + 128, :], w=[t_xr])
                        og, t_og = og_.next()
                        k.tt("pool", og, oacc[:, sub, :, :].rearrange("p h d -> p (h d)"), szt, ALU.mult,
                             r=t_oacc + [t_szt], w=[t_og])
                        pv = ps[4].bitcast(BF16).rearrange("p (a t) -> p a t", a=8)
                        for c8 in range(8):
                            k.tr(pv[:, c8, :], og[:, c8 * 128:(c8 + 1) * 128], k.ident_bf, r=[t_og], w=[tps[4]])
                        ogT, t_ogT = ogT_.next()
                        k.cp("act", ogT, pv, r=[tps[4]], w=[t_ogT])
                        yo, t_yo = yo_.next()
                        for half in range(2):
                            pb = 5 + half
                            for c8 in range(8):
                                k.mm(ps[pb], ogT[:, c8, :], w_out[:, c8, half * 512:(half + 1) * 512], c8 == 0, c8 == 7,
                                     r=[t_ogT, t_wout], w=[tps[pb]])
                            k.tt("dve", yo[:, half * 512:(half + 1) * 512], ps[pb], xr[:, half * 512:(half + 1) * 512], ALU.add,
                                 r=[tps[pb], t_xr], w=[t_yo])
                        k.st(x_dst[s, r0:r0 + 128, :], yo, r=[t_yo])
    P.barrier()


def build_program(nseq=NSEQ, layers=(0, 1, 2, 3), final_norm=True, limit=None):
    nc = bass.Bass("TRN2", target_bir_lowering=False)
    k = K(nc, nseq)
    k.P.limit = limit

    def din(name, shape, dt=F32):
        return nc.dram_tensor(name, list(shape), dt, kind="ExternalInput").ap()

    def dscr(name, shape, dt):
        return nc.dram_tensor(name, list(shape), dt, kind="Internal").ap()

    x = din("x", [nseq, S, D])
    k.ml_norm = din("ml_norm", [2, D])
    k.ml_w_in = din("ml_w_in", [2, D, 4096])
    k.ml_bd = din("ml_bd", [2, 48, 128, 128])
    k.ml_vec = din("ml_vec", [2, 128, 16, 8])
    k.ml_wif = din("ml_wif", [2, 128, 48, 8])
    k.ml_bif = din("ml_bif", [2, 8, 1])
    k.ml_w_out = din("ml_w_out", [2, ML_INNER, D])
    k.final_norm = din("final_norm", [1, D])
    k.nsa_norm = din("nsa_norm", [2, D])
    k.nsa_w_in = din("nsa_w_in", [2, D, 3632])
    k.nsa_b_gate = din("nsa_b_gate", [2, 48])
    k.nsa_peT = din("nsa_peT", [2, 2, 64, 32])
    k.nsa_cmp_w1 = din("nsa_cmp_w1", [2, 2, 2048, 128])
    k.nsa_cmp_w2 = din("nsa_cmp_w2", [2, 2, 128, 64])
    k.nsa_w_out = din("nsa_w_out", [2, D, D])
    k.c_rot = din("c_rot", [128, 128])
    k.c_cos = din("c_cos", [128, S])
    k.c_sin = din("c_sin", [128, S])
    k.c_E = din("c_E", [128, 4 * S], BF16)
    k.c_causn = din("c_causn", [128, 4 * 512], BF16)
    k.c_lown = din("c_lown", [128, 4 * 512], BF16)
    k.c_cmpn = din("c_cmpn", [128, S], BF16)
    k.c_ovl = din("c_ovl", [128, 4 * 33], BF16)
    k.c_fmul = din("c_fmul", [128, 16, 32])
    k.c_fadd = din("c_fadd", [128, 16, 32])
    c_ident = din("c_ident", [128, 128])
    c_caus = din("c_caus", [128, 128])
    y = nc.dram_tensor("y", [nseq, S, D], F32, kind="ExternalOutput").ap()
    xres = dscr("xres", [nseq, S, D], F32)
    T = {
        "qT": dscr("s_qT", [nseq, ML_INNER, S], BF16),
        "kT": dscr("s_kT", [nseq, ML_INNER, S], BF16),
        "sxT": dscr("s_sxT", [nseq, ML_INNER, S], BF16),
        "szT": dscr("s_szT", [nseq, ML_INNER, S], BF16),
        "ktok": dscr("s_ktok", [nseq, S, ML_INNER], BF16),
        "vtok": dscr("s_vtok", [nseq, S, ML_INNER], BF16),
        "gtok": dscr("s_gtok", [nseq, S, 16], F32),
    }
    TN = {
        "qT": dscr("n_qT", [nseq, 1024, S], BF16),
        "qrT": dscr("n_qrT", [nseq, 1024, S], BF16),
        "kcT": dscr("n_kcT", [nseq, 256, S], BF16),
        "vcT": dscr("n_vcT", [nseq, 256, S], BF16),
        "ksT": dscr("n_ksT", [nseq, 256, S], BF16),
        "kwT": dscr("n_kwT", [nseq, 256, S], BF16),
        "vsw": dscr("n_vsw", [nseq, S, 512], BF16),
        "szt": dscr("n_szt", [nseq, S, 1024], BF16),
        "gate": dscr("n_gate", [nseq, S, 48], F32),
    }

    def C(name, shape, dt):
        return nc.alloc_sbuf_tensor(name, shape, dt).ap()
    k.ident_f = C("ident_f", [128, 128], F32)
    k.ident_bf = C("ident_bf", [128, 128], BF16)
    k.caus_f = C("caus_f", [128, 128], F32)
    k.ones_f = C("ones_f", [128, 128], F32)
    k.c_eps = C("c_eps", [128, 1], F32)
    k.c_one = C("c_one", [128, 1], F32)
    k.ps = [nc.alloc_psum_tensor(f"ps{i}", [128, 512], F32).ap() for i in range(8)]
    k.tps = [Tok(f"ps{i}", x=True) for i in range(8)]
    tc = Tok()
    k.ld(k.ident_f, c_ident, w=[tc])
    k.ld(k.caus_f, c_caus, w=[tc])
    k.cp("dve", k.ident_bf, k.ident_f, r=[tc], w=[tc])
    k.memset("dve", k.ones_f, 1.0, w=[tc])
    k.memset("dve", k.c_eps, RMS_EPS, w=[tc])
    k.memset("dve", k.c_one, 1.0, w=[tc])
    k.P.barrier()

    cur = x
    for L in layers:
        li = L // 2
        if L % 2 == 0:
            ml_phase_a(k, li, cur, T)
            ml_phase_b(k, li, cur, xres, T)
        else:
            nsa_phase_a(k, li, cur, TN)
            nsa_phase_c(k, li, cur, xres, TN)
        cur = xres

    print("n_ins before final", k.P.n_ins, flush=True)
    k.P.limit = None
    with ExitStack() as es:
        def A(name, shape, dt):
            return es.enter_context(nc.sbuf_tensor(f"fin_{name}", shape, dt)).ap()
        gb = A("gb", [128, D], F32)
        t_gb = Tok()
        k.ld(gb, k.final_norm[0].partition_broadcast(128), w=[t_gb])
        xr_ = Ring([A(f"x{i}", [128, D], F32) for i in range(3)])
        junk = A("junk", [128, D], F32)
        t_junk = Tok()
        st_ = Ring([A(f"st{i}", [128, 4], F32) for i in range(3)])
        t_y = Tok()
        for s in range(nseq):
            for c in range(NT):
                r0 = c * 128
                xt, t_xt = xr_.next()
                k.ld(xt, cur[s, r0:r0 + 128, :], w=[t_xt])
                if final_norm:
                    sv, t_sv = st_.next()
                    k.act(junk, xt, AF.Square, r=[t_xt], w=[t_junk, t_sv], accum=sv[:, 0:1])
                    k.act(sv[:, 1:2], sv[:, 0:1], AF.Ln, r=[t_sv], w=[t_sv], scale=1.0 / D, bias=k.c_eps[:, 0:1])
                    k.act(sv[:, 2:3], sv[:, 1:2], AF.Exp, r=[t_sv], w=[t_sv], scale=-0.5)
                    k.stt("dve", xt, xt, sv[:, 2:3], gb, ALU.mult, ALU.mult, r=[t_xt, t_sv, t_gb], w=[t_xt])
                k.P.dma("sp", y[s, r0:r0 + 128, :], xt, r=[t_xt], w=[t_y])
        k.P._emit_waits("sp", k.P._deps([t_y], [t_y]))
        deps = {kk: v for kk, v in k.P.cnt.items() if kk.startswith("d_sp") and v > 0}
        k.P._emit_waits("sp", deps)
    k.P.emit()
    return nc


def _block_diag(w):
    out = np.zeros((16, 128, 128), np.float32)
    w4 = w.reshape(16, 32, 4, 4)
    for n in range(32):
        out[:, 4 * n:4 * n + 4, 4 * n:4 * n + 4] = w4[:, n].transpose(0, 2, 1)
    return out


_CONSTS = {}


def _nsa_consts():
    if _CONSTS:
        return _CONSTS
    import ml_dtypes
    bf = ml_dtypes.bfloat16
    c = {}
    rot = np.zeros((128, 128), np.float32)
    for hb in (0, 64):
        for d in range(8):
            rot[hb + d + 8, hb + d] = -1.0
            rot[hb + d, hb + d + 8] = 1.0
    c["c_rot"] = rot
    pos = np.arange(S, dtype=np.float32)
    inv_freq = (np.float32(500000.0) ** (-np.arange(0, 16, 2, dtype=np.float32) / np.float32(16))).astype(np.float32)
    ang = (pos[:, None] * inv_freq[None, :]).astype(np.float32)
    cosf = np.ones((128, S), np.float32)
    sinf = np.zeros((128, S), np.float32)
    for hb in (0, 64):
        for d in range(16):
            cosf[hb + d] = np.cos(ang[:, d % 8])
            sinf[hb + d] = np.sin(ang[:, d % 8])
    c["c_cos"], c["c_sin"] = cosf, sinf
    E = np.zeros((128, 4, S), np.float32)
    key = np.arange(S)
    for g in range(4):
        E[g * 32 + key // 64, g, key] = 1.0
    c["c_E"] = E.reshape(128, 4 * S).astype(bf)
    kk = np.arange(128)[:, None]
    tt = np.arange(512)[None, :]
    causn = np.zeros((128, 4, 512), np.float32)
    lown = np.zeros((128, 4, 512), np.float32)
    for r in range(4):
        valid = kk <= tt - 128 * r
        causn[:, r, :] = np.where(valid, 0.0, NEG)
        lown[:, r, :] = np.where(valid, NEG, 0.0)
    c["c_causn"] = causn.reshape(128, 2048).astype(bf)
    c["c_lown"] = lown.reshape(128, 2048).astype(bf)
    cc = np.arange(128)[:, None]
    tok = np.arange(S)[None, :]
    c["c_cmpn"] = np.where(16 * cc + 31 <= tok, 0.0, NEG).astype(np.float32).astype(bf)
    cs = np.arange(127) * 16
    ss = np.arange(32) * 64
    ov = np.clip(np.minimum(cs[:, None] + 32, ss[None, :] + 64) - np.maximum(cs[:, None], ss[None, :]), 0, None) / 16.0
    ovl = np.zeros((128, 4, 33), np.float32)
    ovl[:, :, 0] = 1.0
    ovl[:127, :, 1:] = ov[:, None, :]
    c["c_ovl"] = ovl.reshape(128, 4 * 33).astype(bf)
    p = np.arange(S)
    blk = np.arange(32)
    dist = (p // 64)[:, None] - blk[None, :]
    forced = (blk[None, :] == 0) | ((dist >= 0) & (dist < 2))
    fmul = np.where(forced | (dist < 0), 0.0, 1.0).astype(np.float32)
    fadd = np.where(forced, 1e9, np.where(dist >= 0, 0.0, -1.0)).astype(np.float32)
    c["c_fmul"] = np.ascontiguousarray(fmul.reshape(16, 128, 32).transpose(1, 0, 2))
    c["c_fadd"] = np.ascontiguousarray(fadd.reshape(16, 128, 32).transpose(1, 0, 2))
    _CONSTS.update(c)
    return _CONSTS


def host_layout(inp):
    f = lambda a: np.ascontiguousarray(np.asarray(a, dtype=np.float32))
    d = {}
    d["ml_norm"] = f(inp["ml_norm"])
    d["ml_w_in"] = f(inp["ml_w_in"])
    bd = np.zeros((2, 48, 128, 128), np.float32)
    vec = np.zeros((2, 128, 16, 8), np.float32)
    for li in range(2):
        for g3, nm in enumerate(("ml_w_q", "ml_w_k", "ml_w_v")):
            bd[li, g3 * 16:(g3 + 1) * 16] = _block_diag(np.asarray(inp[nm][li], np.float32))
        cw = np.asarray(inp["ml_conv_w"][li], np.float32)
        for tap in range(4):
            vec[li, :, :, tap] = cw[tap].reshape(16, 128).T
        vec[li, :, :, 4] = np.asarray(inp["ml_conv_b"][li], np.float32).reshape(16, 128).T
        vec[li, :, :, 5] = np.asarray(inp["ml_ln_w"][li], np.float32).reshape(16, 128).T
        vec[li, :, :, 6] = np.asarray(inp["ml_skip"][li], np.float32).reshape(16, 128).T
    d["ml_bd"] = bd
    d["ml_vec"] = vec
    d["ml_wif"] = f(np.asarray(inp["ml_w_if"], np.float32).reshape(2, 48, 128, 8).transpose(0, 2, 1, 3))
    d["ml_bif"] = f(np.asarray(inp["ml_b_if"], np.float32).reshape(2, 8, 1))
    d["ml_w_out"] = f(inp["ml_w_out"])
    d["final_norm"] = f(np.asarray(inp["final_norm"], np.float32).reshape(1, D))
    d["nsa_norm"] = f(inp["nsa_norm"])
    d["nsa_w_in"] = f(inp["nsa_w_in"])
    d["nsa_b_gate"] = f(inp["nsa_b_gate"])
    d["nsa_peT"] = f(np.asarray(inp["nsa_cmp_pe"], np.float32).transpose(0, 1, 3, 2))
    d["nsa_cmp_w1"] = f(inp["nsa_cmp_w1"])
    d["nsa_cmp_w2"] = f(inp["nsa_cmp_w2"])
    d["nsa_w_out"] = f(inp["nsa_w_out"])
    d.update(_nsa_consts())
    d["c_ident"] = np.eye(128, dtype=np.float32)
    d["c_caus"] = np.triu(np.ones((128, 128), np.float32))
    return d


_NC_CACHE = {}


def kernel(**inputs):
    x = np.asarray(inputs["x"], np.float32)
    shared = host_layout(inputs)
    if "nc" not in _NC_CACHE:
        _NC_CACHE["nc"] = build_program()
    nc = _NC_CACHE["nc"]
    in_maps = []
    for c in range(NCORES):
        m = dict(shared)
        m["x"] = np.ascontiguousarray(x[c * NSEQ:(c + 1) * NSEQ])
        in_maps.append(m)
    res = run_bass_kernel_spmd(nc, in_maps, core_ids=list(range(NCORES)))
    return np.concatenate([r["y"] for r in res.results], axis=0)
```

```python
from contextlib import ExitStack
import numpy as np
import concourse.bass as bass
import concourse.mybir as mybir
from concourse.bass_utils import run_bass_kernel_spmd

F32 = mybir.dt.float32
BF16 = mybir.dt.bfloat16
AF = mybir.ActivationFunctionType
ALU = mybir.AluOpType
AX = mybir.AxisListType

S = 2048
D = 1024
NT = S // 128
NSEQ = 2
NCORES = 8
ML_INNER = 2048
NH = 4
DH = 512
RMS_EPS = 1e-6
LN_EPS = 1e-6
NEG = -30000.0


class Tok:
    __slots__ = ("name", "w", "r", "x")

    def __init__(self, name="", x=False):
        self.name = name
        self.w = None
        self.r = {}
        self.x = x


class Prog:
    ENG = ("pe", "dve", "act", "pool", "sp")

    def __init__(self, nc, n_dma_ring=12):
        self.nc = nc
        self.q = {e: [] for e in self.ENG}
        self.sems = {}
        self.cnt = {}
        for e in self.ENG:
            self.sems[e] = nc.alloc_semaphore(name=f"c_{e}")
            self.cnt[e] = 0
        self.ring = {}
        self.ring_pos = {}
        for qn in ("sp", "act", "pool"):
            ks = []
            for i in range(n_dma_ring):
                k = f"d_{qn}{i}"
                self.sems[k] = nc.alloc_semaphore(name=k)
                self.cnt[k] = 0
                ks.append(k)
            self.ring[qn] = ks
            self.ring_pos[qn] = 0
        self.seen = {e: {} for e in self.ENG}
        self.n_ins = 0

    def _deps(self, r, w):
        deps = {}

        def add(k, v):
            if deps.get(k, 0) < v:
                deps[k] = v
        for t in r:
            if t.w is not None:
                add(*t.w)
            if t.x:
                for k, v in t.r.items():
                    add(k, v)
        for t in w:
            if t.w is not None:
                add(*t.w)
            for k, v in t.r.items():
                add(k, v)
        return deps

    def _emit_waits(self, e, deps):
        seen = self.seen[e]
        for k, v in deps.items():
            if k == e and e == "pe":
                continue
            if seen.get(k, 0) >= v:
                continue
            seen[k] = v
            self.q[e].append(("w", self.sems[k], v))

    limit = None

    def op(self, e, fn, r=(), w=()):
        if self.limit is not None and self.n_ins >= self.limit:
            return
        self._emit_waits(e, self._deps(r, w))
        self.cnt[e] += 1
        c = self.cnt[e]
        self.q[e].append(("i", fn, self.sems[e]))
        self.n_ins += 1
        for t in w:
            t.w = (e, c)
            t.r = {}
        for t in r:
            if t.r.get(e, 0) < c:
                t.r[e] = c

    def dma(self, qn, out, in_, r=(), w=()):
        if self.limit is not None and self.n_ins >= self.limit:
            return
        deps = self._deps(r, w)
        k = self.ring[qn][self.ring_pos[qn]]
        self.ring_pos[qn] = (self.ring_pos[qn] + 1) % len(self.ring[qn])
        if self.cnt[k] > 0 and deps.get(k, 0) < self.cnt[k]:
            deps[k] = self.cnt[k]
        self._emit_waits(qn, deps)
        self.cnt[k] += 16
        c = self.cnt[k]
        self.q[qn].append(("d", out, in_, self.sems[k]))
        self.n_ins += 1
        for t in w:
            t.w = (k, c)
            t.r = {}
        for t in r:
            if t.r.get(k, 0) < c:
                t.r[k] = c

    def barrier(self):
        deps = {k: v for k, v in self.cnt.items() if v > 0}
        for e in self.ENG:
            self._emit_waits(e, dict(deps))

    def emit(self):
        nc = self.nc
        with nc.Block() as block:
            def mk(e):
                def body(engine):
                    for it in self.q[e]:
                        if it[0] == "w":
                            engine.wait_ge(it[1], it[2])
                        elif it[0] == "i":
                            it[1](engine).then_inc(it[2], 1)
                        else:
                            engine.dma_start(out=it[1], in_=it[2]).then_inc(it[3], 16)
                return body
            block.tensor(mk("pe"))
            block.vector(mk("dve"))
            block.scalar(mk("act"))
            block.gpsimd(mk("pool"))
            block.sync(mk("sp"))


class Ring:
    def __init__(self, aps):
        self.items = [(a, Tok()) for a in aps]
        self.i = 0

    def next(self):
        it = self.items[self.i]
        self.i = (self.i + 1) % len(self.items)
        return it


class K:
    def __init__(self, nc, nseq):
        self.nc = nc
        self.P = Prog(nc)
        self.nseq = nseq

    def mm(self, out, lhsT, rhs, start, stop, r, w):
        self.P.op("pe", lambda e: e.matmul(out, lhsT=lhsT, rhs=rhs, start=start, stop=stop), r, w)

    def tr(self, out, in_, ident, r, w):
        self.P.op("pe", lambda e: e.transpose(out, in_, ident), r, w)

    def cp(self, eng, out, in_, r, w):
        if eng == "act":
            self.P.op("act", lambda e: e.activation(out, in_, AF.Copy), r, w)
        else:
            self.P.op(eng, lambda e: e.tensor_copy(out, in_), r, w)

    def act(self, out, in_, func, r, w, bias=None, scale=None, accum=None):
        kw = {}
        if bias is not None:
            kw["bias"] = bias
        if scale is not None:
            kw["scale"] = scale
        if accum is not None:
            kw["accum_out"] = accum
        self.P.op("act", lambda e: e.activation(out, in_, func, **kw), r, w)

    def tt(self, eng, out, in0, in1, op, r, w):
        self.P.op(eng, lambda e: e.tensor_tensor(out, in0, in1, op), r, w)

    def ts(self, eng, out, in0, s1, s2, op0, op1, r, w):
        if s2 is None:
            self.P.op(eng, lambda e: e.tensor_scalar(out, in0, s1, None, op0), r, w)
        else:
            self.P.op(eng, lambda e: e.tensor_scalar(out, in0, s1, s2, op0, op1), r, w)

    def stt(self, eng, out, in0, scalar, in1, op0, op1, r, w):
        self.P.op(eng, lambda e: e.scalar_tensor_tensor(out, in0, scalar, in1, op0, op1), r, w)

    def memset(self, eng, out, val, w):
        self.P.op(eng, lambda e: e.memset(out, val), (), w)

    def recip(self, out, in_, r, w):
        self.P.op("dve", lambda e: e.reciprocal(out, in_), r, w)

    def ld(self, out, in_, w, r=()):
        self.P.dma("sp", out, in_, r, w)

    def st(self, out, in_, r, w=()):
        self.P.dma("sp", out, in_, r, w)


def bcast_last(ap, n):
    shp = list(ap.shape)
    return ap.unsqueeze(len(shp)).to_broadcast(shp + [n])


def ml_phase_a(k, li, x_src, T):
    nc, P = k.nc, k.P
    ps, tps = k.ps, k.tps
    with ExitStack() as es:
        def A(name, shape, dt):
            return es.enter_context(nc.sbuf_tensor(f"mA{li}_{name}", shape, dt)).ap()
        w_in = A("win", [128, 8, 4096], BF16)
        t_win = [Tok() for _ in range(8)]
        bd = A("bd", [128, 48, 128], BF16)
        t_bd = Tok()
        vec = A("vec", [128, 16, 8], F32)
        t_vec = Tok()
        wif32 = A("wif32", [128, 48, 8], F32)
        wif = A("wif", [128, 48, 8], BF16)
        t_wif = Tok()
        bif = A("bif", [8, 1], F32)
        t_bif = Tok()
        gb = A("gb", [128, 1024], F32)
        t_gb = Tok()
        stg = Ring([A(f"stg{i}", [128, 2048], F32) for i in range(2)])
        xr_ = Ring([A(f"xt{i}", [128, 1024], F32) for i in range(2)])
        hb_ = Ring([A(f"hb{i}", [128, 1024], BF16) for i in range(2)])
        junk = A("junk", [128, 1024], BF16)
        t_junk = Tok()
        st_ = Ring([A(f"st{i}", [128, 4], F32) for i in range(2)])
        hT = A("hT", [128, 8, 512], BF16)
        t_hT = Tok()
        xin = A("xin", [128, 16, 515], BF16)
        t_xin = [Tok() for _ in range(16)]
        acc_ = Ring([A(f"acc{i}", [128, 512], F32) for i in range(3)])
        xc = A("xc", [128, 16, 512], BF16)
        t_xc = [Tok() for _ in range(16)]
        sx_ = Ring([A(f"sx{i}", [128, 512], BF16) for i in range(2)])
        sz_ = Ring([A(f"sz{i}", [128, 512], BF16) for i in range(2)])
        qkv_ = Ring([A(f"qkv{i}", [128, 512], BF16) for i in range(8)])
        tok_ = Ring([A(f"tokm{i}", [128, 2048], BF16) for i in range(3)])
        gsb = A("gsb", [8, 2, 512], F32)
        t_gsb = Tok()
        gt_ = Ring([A(f"gt{i}", [128, 16], F32) for i in range(2)])

        for kc in range(8):
            for hf in range(2):
                s_ap, s_t = stg.next()
                k.ld(s_ap, k.ml_w_in[li, kc * 128:(kc + 1) * 128, hf * 2048:(hf + 1) * 2048], w=[s_t])
                k.cp(("dve", "act")[hf], w_in[:, kc, hf * 2048:(hf + 1) * 2048], s_ap, r=[s_t], w=[t_win[kc]])
        for g3 in range(3):
            s_ap, s_t = stg.next()
            sv = s_ap.rearrange("p (c n) -> p c n", c=16)
            k.ld(sv, k.ml_bd[li, g3 * 16:(g3 + 1) * 16].rearrange("c p n -> p c n"), w=[s_t])
            k.cp("dve", bd[:, g3 * 16:(g3 + 1) * 16, :], sv, r=[s_t], w=[t_bd])
        k.ld(vec, k.ml_vec[li], w=[t_vec])
        k.ld(wif32, k.ml_wif[li], w=[t_wif])
        k.ts("dve", wif32[:, 0:16, :], wif32[:, 0:16, :], float(DH ** 0.5), None, ALU.mult, None, r=[t_wif], w=[t_wif])
        k.cp("dve", wif, wif32, r=[t_wif], w=[t_wif])
        k.ld(bif, k.ml_bif[li], w=[t_bif])
        k.ld(gb, k.ml_norm[li].partition_broadcast(128), w=[t_gb])

        for s in range(k.nseq):
            for st in range(4):
                t0 = st * 512
                for sub in range(4):
                    r0 = t0 + sub * 128
                    xt, t_xt = xr_.next()
                    k.ld(xt, x_src[s, r0:r0 + 128, :], w=[t_xt])
                    sv, t_sv = st_.next()
                    k.act(junk, xt, AF.Square, r=[t_xt], w=[t_junk, t_sv], accum=sv[:, 0:1])
                    k.act(sv[:, 1:2], sv[:, 0:1], AF.Ln, r=[t_sv], w=[t_sv], scale=1.0 / D, bias=k.c_eps[:, 0:1])
                    k.act(sv[:, 2:3], sv[:, 1:2], AF.Exp, r=[t_sv], w=[t_sv], scale=-0.5)
                    hb, t_hb = hb_.next()
                    k.stt("dve", hb, xt, sv[:, 2:3], gb, ALU.mult, ALU.mult, r=[t_xt, t_sv, t_gb], w=[t_hb])
                    pv = ps[5].bitcast(BF16).rearrange("p (a t) -> p a t", a=8)
                    for kc in range(8):
                        k.tr(pv[:, kc, :], hb[:, kc * 128:(kc + 1) * 128], k.ident_bf, r=[t_hb], w=[tps[5]])
                    k.cp("dve", hT[:, :, sub * 128:(sub + 1) * 128], pv, r=[tps[5]], w=[t_hT])
                if st == 0:
                    k.memset("dve", xin[:, :, 0:3], 0.0, w=t_xin)
                else:
                    k.cp("dve", xin[:, :, 0:3], xin[:, :, 512:515], r=t_xin, w=t_xin)
                def proj_x1(fc):
                    bi = fc % 2
                    for kc in range(8):
                        k.mm(ps[bi], w_in[:, kc, fc * 128:(fc + 1) * 128], hT[:, kc, :], kc == 0, kc == 7,
                             r=[t_win[kc], t_hT], w=[tps[bi]])
                    k.cp("act", xin[:, fc, 3:515], ps[bi], r=[tps[bi]], w=[t_xin[fc]])

                def proj_x2a(fc):
                    acc, t_acc = acc_.next()
                    k.act(acc, xin[:, fc, 0:512], AF.Copy, r=[t_xin[fc], t_vec], w=[t_acc], scale=vec[:, fc, 0:1])
                    for tap in range(1, 4):
                        k.stt("dve", acc, xin[:, fc, tap:tap + 512], vec[:, fc, tap:tap + 1], acc, ALU.mult, ALU.add,
                              r=[t_xin[fc], t_acc], w=[t_acc])
                    pend_acc[fc] = (acc, t_acc)

                def proj_x2b(fc):
                    acc, t_acc = pend_acc.pop(fc)
                    k.act(xc[:, fc, :], acc, AF.Silu, r=[t_acc], w=[t_xc[fc]], bias=vec[:, fc, 4:5])
                    sx, t_sx = sx_.next()
                    k.act(sx, xc[:, fc, :], AF.Copy, r=[t_xc[fc]], w=[t_sx], scale=vec[:, fc, 6:7])
                    k.st(T["sxT"][s, fc * 128:(fc + 1) * 128, t0:t0 + 512], sx, r=[t_sx])

                def bdmm(fc):
                    tiles = []
                    for which in range(3):
                        pb = (2, 3, 6)[which]
                        src = xc[:, fc, :] if which < 2 else xin[:, fc, 3:515]
                        tsrc = t_xc[fc] if which < 2 else t_xin[fc]
                        k.mm(ps[pb], bd[:, which * 16 + fc, :], src, True, True, r=[t_bd, tsrc], w=[tps[pb]])
                    for which in range(3):
                        pb = (2, 3, 6)[which]
                        qt, t_qt = qkv_.next()
                        if which == 0:
                            k.act(qt, ps[pb], AF.Copy, r=[tps[pb]], w=[t_qt], scale=float(DH ** -0.5))
                            k.st(T["qT"][s, fc * 128:(fc + 1) * 128, t0:t0 + 512], qt, r=[t_qt])
                        elif which == 1:
                            k.cp("dve", qt, ps[pb], r=[tps[pb]], w=[t_qt])
                            k.st(T["kT"][s, fc * 128:(fc + 1) * 128, t0:t0 + 512], qt, r=[t_qt])
                        else:
                            k.cp("dve", qt, ps[pb], r=[tps[pb]], w=[t_qt])
                        tiles.append((qt, t_qt))
                    pend_gates[fc] = tiles

                def gates(fc):
                    for which, (qt, t_qt) in enumerate(pend_gates.pop(fc)):
                        first = (fc == 0 and which == 0)
                        last = (fc == 15 and which == 2)
                        k.mm(ps[4][0:8, :], wif[:, which * 16 + fc, :], qt, first, last, r=[t_wif, t_qt], w=[tps[4]])

                def proj_z(fc):
                    bi = fc % 2
                    for kc in range(8):
                        k.mm(ps[bi], w_in[:, kc, fc * 128:(fc + 1) * 128], hT[:, kc, :], kc == 0, kc == 7,
                             r=[t_win[kc], t_hT], w=[tps[bi]])
                    sz, t_sz = sz_.next()
                    k.act(sz, ps[bi], AF.Silu, r=[tps[bi]], w=[t_sz])
                    k.st(T["szT"][s, (fc - 16) * 128:(fc - 15) * 128, t0:t0 + 512], sz, r=[t_sz])

                pend_gates = {}
                pend_acc = {}
                for fc in range(32):
                    if fc < 16:
                        proj_x1(fc)
                    else:
                        proj_z(fc)
                    if 3 <= fc < 19:
                        bdmm(fc - 3)
                    if 4 <= fc < 20:
                        gates(fc - 4)
                    if fc < 16:
                        proj_x2a(fc)
                    if 1 <= fc < 17:
                        proj_x2b(fc - 1)
                for which in (1, 2):
                    for sub in range(4):
                        tm, t_tm = tok_.next()
                        for fg in range(4):
                            pb = 6 + (fg % 2)
                            for f4 in range(4):
                                fc = fg * 4 + f4
                                if which == 1:
                                    lhsT = xc[:, fc, sub * 128:(sub + 1) * 128]
                                    tsrc = t_xc[fc]
                                else:
                                    lhsT = xin[:, fc, 3 + sub * 128:3 + (sub + 1) * 128]
                                    tsrc = t_xin[fc]
                                k.mm(ps[pb][:, f4 * 128:(f4 + 1) * 128], lhsT, bd[:, which * 16 + fc, :], True, True,
                                     r=[t_bd, tsrc], w=[tps[pb]])
                            k.cp(("act", "dve")[fg % 2], tm[:, fg * 512:(fg + 1) * 512], ps[pb], r=[tps[pb]], w=[t_tm])
                        dst = T["ktok"] if which == 1 else T["vtok"]
                        r0 = t0 + sub * 128
                        k.st(dst[s, r0:r0 + 128, :], tm, r=[t_tm])
                k.act(gsb[:, 0, :], ps[4][0:8, :], AF.Identity, r=[tps[4], t_bif], w=[t_gsb], bias=bif[:, 0:1])
                k.act(gsb[:, 1, :], gsb[:, 0, :], AF.Exp, r=[t_gsb], w=[t_gsb], scale=-1.0)
                k.act(gsb[:, 1, :], gsb[:, 1, :], AF.Ln, r=[t_gsb], w=[t_gsb], bias=k.c_one[0:8, 0:1])
                for sub in range(4):
                    for a in range(2):
                        k.tr(ps[5][:, a * 8:(a + 1) * 8], gsb[:, a, sub * 128:(sub + 1) * 128], k.ident_f[0:8, 0:8],
                             r=[t_gsb], w=[tps[5]])
                    gt, t_gt = gt_.next()
                    k.cp("dve", gt, ps[5][:, 0:16], r=[tps[5]], w=[t_gt])
                    r0 = t0 + sub * 128
                    k.st(T["gtok"][s, r0:r0 + 128, :], gt, r=[t_gt])
    P.barrier()


def ml_phase_b(k, li, x_src, x_dst, T):
    nc, P = k.nc, k.P
    ps, tps = k.ps, k.tps
    with ExitStack() as es:
        def A(name, shape, dt):
            return es.enter_context(nc.sbuf_tensor(f"mB{li}_{name}", shape, dt)).ap()
        w_out = A("wout", [128, 16, 1024], BF16)
        t_wout = Tok()
        vec = A("vec", [128, 16, 8], F32)
        t_vec = Tok()
        stg = Ring([A(f"stg{i}", [128, 2048], F32) for i in range(1)])
        X = A("X", [128, 16, 513], F32)
        t_X = [Tok() for _ in range(4)]
        STb = A("STb", [128, 16, 513], BF16)
        t_STb = [Tok() for _ in range(4)]
        qT_ = Ring([A(f"qT{i}", [128, 16, 128], BF16) for i in range(2)])
        kT_ = Ring([A(f"kT{i}", [128, 16, 128], BF16) for i in range(2)])
        kt_ = Ring([A(f"kt{i}", [128, 2048], BF16) for i in range(2)])
        vt_ = Ring([A(f"vt{i}", [128, 2048], BF16) for i in range(2)])
        gt_ = Ring([A(f"gt{i}", [128, 16], F32) for i in range(2)])
        xr_ = Ring([A(f"xr{i}", [128, 1024], F32) for i in range(3)])
        sx_ = Ring([A(f"sx{i}", [128, 16, 128], BF16) for i in range(3)])
        sz_ = Ring([A(f"sz{i}", [128, 16, 128], BF16) for i in range(3)])
        ve_ = Ring([A(f"ve{i}", [128, 4, 520], BF16) for i in range(2)])
        PT_ = Ring([A(f"PT{i}", [128, 128], BF16) for i in range(4)])
        gm_ = Ring([A(f"gm{i}", [128, 4, 4], F32) for i in range(3)])
        sm_ = Ring([A(f"sm{i}", [128, 8, 4], F32) for i in range(2)])
        mv_ = Ring([A(f"mv{i}", [128, 4, 2], F32) for i in range(2)])
        bs_ = Ring([A(f"bs{i}", [128, 6], F32) for i in range(4)])
        hn_ = Ring([A(f"hn{i}", [128, 2048], BF16) for i in range(2)])
        t1 = A("t1", [128, 16, 128], F32)
        t_t1 = Tok()
        gT_ = Ring([A(f"gT{i}", [128, 16, 128], BF16) for i in range(2)])
        yo_ = Ring([A(f"yo{i}", [128, 1024], F32) for i in range(2)])

        for fc in range(0, 16, 2):
            s_ap, s_t = stg.next()
            sv = s_ap.rearrange("p (c n) -> p c n", c=2)
            k.ld(sv, k.ml_w_out[li, fc * 128:(fc + 2) * 128, :].rearrange("(c p) n -> p c n", p=128), w=[s_t])
            k.cp("dve", w_out[:, fc:fc + 2, :], sv, r=[s_t], w=[t_wout])
        k.ld(vec, k.ml_vec[li], w=[t_vec])

        def issue_loads(s, c):
            r0 = c * 128
            d = {}
            for nm, ring in (("gt", gt_), ("kT", kT_), ("qT", qT_), ("vt", vt_), ("kt", kt_), ("sx", sx_), ("sz", sz_),
                             ("xr", xr_)):
                d[nm] = ring.next()
            k.ld(d["gt"][0], T["gtok"][s, r0:r0 + 128, :], w=[d["gt"][1]])
            k.ld(d["kT"][0], T["kT"][s, :, r0:r0 + 128].rearrange("(a p) t -> p a t", p=128), w=[d["kT"][1]])
            k.ld(d["qT"][0], T["qT"][s, :, r0:r0 + 128].rearrange("(a p) t -> p a t", p=128), w=[d["qT"][1]])
            k.ld(d["vt"][0], T["vtok"][s, r0:r0 + 128, :], w=[d["vt"][1]])
            k.ld(d["kt"][0], T["ktok"][s, r0:r0 + 128, :], w=[d["kt"][1]])
            k.ld(d["sx"][0], T["sxT"][s, :, r0:r0 + 128].rearrange("(a p) t -> p a t", p=128), w=[d["sx"][1]])
            k.ld(d["sz"][0], T["szT"][s, :, r0:r0 + 128].rearrange("(a p) t -> p a t", p=128), w=[d["sz"][1]])
            k.ld(d["xr"][0], x_src[s, r0:r0 + 128, :], w=[d["xr"][1]])
            return d

        def epilogue(pe_, fillers=()):
            fillers = list(fillers)
            hn, t_hn, sx, t_sx, sz, t_sz, xr, t_xr, s, r0 = pe_
            for half in range(2):
                pb = 6 + half
                pv = ps[pb].bitcast(BF16).rearrange("p (a t) -> p a t", a=8)
                for a in range(8):
                    fc = half * 8 + a
                    k.tr(pv[:, a, :], hn[:, fc * 128:(fc + 1) * 128], k.ident_bf, r=[t_hn], w=[tps[pb]])
                k.tt("dve", t1[:, half * 8:(half + 1) * 8, :], pv, bcast_last(vec[:, half * 8:(half + 1) * 8, 5], 128),
                     ALU.mult, r=[tps[pb], t_vec], w=[t_t1])
            gT, t_gT = gT_.next()
            k.tt("dve", t1, t1, sx, ALU.add, r=[t_sx, t_t1], w=[t_t1])
            k.tt("dve", gT, t1, sz, ALU.mult, r=[t_sz, t_t1], w=[t_gT])
            yo, t_yo = yo_.next()
            for half in range(2):
                pb = 6 + half
                for fc in range(16):
                    k.mm(ps[pb], gT[:, fc, :], w_out[:, fc, half * 512:(half + 1) * 512], fc == 0, fc == 15,
                         r=[t_gT, t_wout], w=[tps[pb]])
                    if fc % 2 == 1 and fillers:
                        fillers.pop(0)()
                k.tt("dve", yo[:, half * 512:(half + 1) * 512], ps[pb], xr[:, half * 512:(half + 1) * 512], ALU.add,
                     r=[tps[pb], t_xr], w=[t_yo])
            k.st(x_dst[s, r0:r0 + 128, :], yo, r=[t_yo])
            for f in fillers:
                f()


        work = [(s, c) for s in range(k.nseq) for c in range(NT)]
        nxt = issue_loads(*work[0])
        prev_gm = None
        pend_epi = None
        for wi, (s, c) in enumerate(work):
            r0 = c * 128
            L = nxt
            if wi + 1 < len(work):
                nxt = issue_loads(*work[wi + 1])
            (gt, t_gt), (kT, t_kT), (qT, t_qT), (vt, t_vt) = L["gt"], L["kT"], L["qT"], L["vt"]
            (kt, t_kt), (sx, t_sx), (sz, t_sz), (xr, t_xr) = L["kt"], L["sx"], L["sz"], L["xr"]
            if c == 0:
                prev_gm = None
            gm, t_gm = gm_.next()
            k.mm(ps[0][:, 0:4], k.caus_f, gt[:, 12:16], True, True, r=[t_gt], w=[tps[0]])
            k.mm(ps[0][:, 4:8], k.ones_f, gt[:, 12:16], True, True, r=[t_gt], w=[tps[0]])
            k.tt("dve", gm[:, 3, :], ps[0][:, 0:4], gt[:, 0:4], ALU.add, r=[tps[0], t_gt], w=[t_gm])
            k.act(gm[:, 0, :], gm[:, 3, :], AF.Exp, r=[t_gm], w=[t_gm])
            k.act(gm[:, 1:3, :], ps[0][:, 0:8].rearrange("p (a b) -> p a b", a=2), AF.Exp, r=[tps[0]], w=[t_gm],
                  scale=-1.0)
            ve, t_ve = ve_.next()
            for h in range(NH):
                k.act(ve[:, h, 0:512], vt[:, h * 512:(h + 1) * 512], AF.Copy, r=[t_vt, t_gm], w=[t_ve],
                      scale=gm[:, 0, h:h + 1])
            k.cp("dve", ve[:, :, 512], gm[:, 0, :], r=[t_gm], w=[t_ve])
            for h in range(NH):
                for dc in range(4):
                    k.mm(ps[1][:, 0:128], kT[:, h * 4 + dc, :], qT[:, h * 4 + dc, :], dc == 0, dc == 3,
                         r=[t_kT, t_qT], w=[tps[1]])
                PT, t_PT = PT_.next()
                k.tt("dve", PT, ps[1][:, 0:128], k.caus_f, ALU.mult, r=[tps[1]], w=[t_PT])
                nb = 2 + h
                k.mm(ps[nb], PT, ve[:, h, 0:512], True, c == 0, r=[t_PT, t_ve], w=[tps[nb]])
                if c > 0:
                    for dc in range(4):
                        k.mm(ps[nb], qT[:, h * 4 + dc, :], STb[:, h * 4 + dc, 0:512], False, dc == 3,
                             r=[t_qT, t_STb[h]], w=[tps[nb]])
                k.mm(ps[0][:, 8 + h:9 + h], PT, ve[:, h, 512:513], True, c == 0, r=[t_PT, t_ve], w=[tps[0]])
                if c > 0:
                    for dc in range(4):
                        k.mm(ps[0][:, 8 + h:9 + h], qT[:, h * 4 + dc, :], STb[:, h * 4 + dc, 512:513], False, dc == 3,
                             r=[t_qT, t_STb[h]], w=[tps[0]])
            def mk_filler(h, dc, kt, t_kt, ve, t_ve, gm, t_gm, pg, c):
                def f():
                    ub = 1
                    lhsT = kt[:, h * 512 + dc * 128:h * 512 + (dc + 1) * 128]
                    k.mm(ps[ub], lhsT, ve[:, h, 0:512], True, True, r=[t_kt, t_ve], w=[tps[ub]])
                    k.mm(ps[0][:, 16 + h * 4 + dc:17 + h * 4 + dc], lhsT, ve[:, h, 512:513], True, True,
                         r=[t_kt, t_ve], w=[tps[0]])
                    if c == 0:
                        k.cp("dve", X[:, h * 4 + dc, 0:512], ps[ub], r=[tps[ub]], w=[t_X[h]])
                    else:
                        k.stt("dve", X[:, h * 4 + dc, 0:512], X[:, h * 4 + dc, 0:512], pg[0][:, 2, h:h + 1], ps[ub],
                              ALU.mult, ALU.add, r=[tps[ub], pg[1], t_X[h]], w=[t_X[h]])
                    if c < NT - 1:
                        k.act(STb[:, h * 4 + dc, 0:512], X[:, h * 4 + dc, 0:512], AF.Copy, r=[t_X[h], t_gm], w=[t_STb[h]],
                              scale=gm[:, 2, h:h + 1])
                    if dc == 3:
                        xn = X[:, h * 4:(h + 1) * 4, 512:513].rearrange("p a b -> p (a b)")
                        if c == 0:
                            k.cp("dve", xn, ps[0][:, 16 + h * 4:20 + h * 4], r=[tps[0]], w=[t_X[h]])
                        else:
                            k.stt("dve", xn, xn, pg[0][:, 2, h:h + 1], ps[0][:, 16 + h * 4:20 + h * 4],
                                  ALU.mult, ALU.add, r=[tps[0], pg[1], t_X[h]], w=[t_X[h]])
                        if c < NT - 1:
                            sn = STb[:, h * 4:(h + 1) * 4, 512:513].rearrange("p a b -> p (a b)")
                            k.act(sn, xn, AF.Copy, r=[t_X[h], t_gm], w=[t_STb[h]], scale=gm[:, 2, h:h + 1])
                return f

            fillers = [mk_filler(h, dc, kt, t_kt, ve, t_ve, gm, t_gm, prev_gm, c) for h in range(NH) for dc in range(4)]
            if pend_epi is not None:
                epilogue(pend_epi, fillers)
                pend_epi = None
            else:
                for f in fillers:
                    f()
            prev_gm = (gm, t_gm)
            sm, t_sm = sm_.next()
            mv, t_mv = mv_.next()
            for h in range(NH):
                bs, t_bs = bs_.next()
                k.P.op("dve", (lambda o, i: (lambda e: e.bn_stats(o, i)))(bs, ps[2 + h]), r=[tps[2 + h]], w=[t_bs])
                k.P.op("dve", (lambda o, i: (lambda e: e.bn_aggr(o, i)))(mv[:, h, :], bs), r=[t_bs], w=[t_mv])
            k.tt("dve", sm[:, 0, :], ps[0][:, 8:12], gm[:, 1, :], ALU.mult, r=[tps[0], t_gm], w=[t_sm])
            k.stt("dve", sm[:, 1, :], sm[:, 0, :], -1.0, sm[:, 0, :], ALU.mult, ALU.max, r=[t_sm], w=[t_sm])
            k.ts("dve", sm[:, 1, :], sm[:, 1, :], 1.0, None, ALU.max, None, r=[t_sm], w=[t_sm])
            k.recip(sm[:, 2, :], sm[:, 1, :], r=[t_sm], w=[t_sm])
            k.tt("dve", sm[:, 3, :], gm[:, 1, :], sm[:, 2, :], ALU.mult, r=[t_sm, t_gm], w=[t_sm])
            k.tt("dve", sm[:, 4, :], sm[:, 3, :], sm[:, 3, :], ALU.mult, r=[t_sm], w=[t_sm])
            k.tt("dve", sm[:, 4, :], sm[:, 4, :], mv[:, :, 1], ALU.mult, r=[t_sm, t_mv], w=[t_sm])
            k.act(sm[:, 5, :], sm[:, 4, :], AF.Ln, r=[t_sm], w=[t_sm], bias=k.c_eps[:, 0:1])
            k.act(sm[:, 5, :], sm[:, 5, :], AF.Exp, r=[t_sm], w=[t_sm], scale=-0.5)
            k.tt("dve", sm[:, 6, :], sm[:, 5, :], sm[:, 3, :], ALU.mult, r=[t_sm], w=[t_sm])
            k.stt("dve", sm[:, 7, :], mv[:, :, 0], -1.0, sm[:, 6, :], ALU.mult, ALU.mult, r=[t_sm, t_mv], w=[t_sm])
            hn, t_hn = hn_.next()
            for h in range(NH):
                k.act(hn[:, h * 512:(h + 1) * 512], ps[2 + h], AF.Identity, r=[tps[2 + h], t_sm], w=[t_hn],
                      scale=sm[:, 6, h:h + 1], bias=sm[:, 7, h:h + 1])
            pend_epi = (hn, t_hn, sx, t_sx, sz, t_sz, xr, t_xr, s, r0)
        epilogue(pend_epi)
    P.barrier()


NSA_FM = [(0, 1024), (1024, 1280), (1280, 1536), (1536, 1792), (2048, 2304)]
SCALE = 0.125


def nsa_phase_a(k, li, x_src, T):
    nc, P = k.nc, k.P
    ps, tps = k.ps, k.tps
    with ExitStack() as es:
        def A(name, shape, dt):
            return es.enter_context(nc.sbuf_tensor(f"nA{li}_{name}", shape, dt)).ap()
        w_in = A("win", [128, 8, 3648], BF16)
        t_win = [Tok() for _ in range(8)]
        gb = A("gb", [128, 1024], F32)
        t_gb = Tok()
        bg = A("bg", [128, 48], F32)
        t_bg = Tok()
        rot = A("rot", [128, 128], BF16)
        cosf = A("cosf", [128, S], F32)
        sinf = A("sinf", [128, S], F32)
        t_c = Tok()
        stg = Ring([A(f"stg{i}", [128, 1816], F32) for i in range(2)])
        xr_ = Ring([A(f"xt{i}", [128, 1024], F32) for i in range(2)])
        hb_ = Ring([A(f"hb{i}", [128, 1024], BF16) for i in range(2)])
        junk = A("junk", [128, 1024], BF16)
        t_junk = Tok()
        st_ = Ring([A(f"st{i}", [128, 4], F32) for i in range(2)])
        hT = A("hT", [128, 8, 512], BF16)
        t_hT = Tok()
        xb_ = Ring([A(f"xb{i}", [128, 512], BF16) for i in range(3)])
        ta_ = Ring([A(f"ta{i}", [128, 512], F32) for i in range(2)])
        tb_ = Ring([A(f"tb{i}", [128, 512], F32) for i in range(2)])
        ro_ = Ring([A(f"ro{i}", [128, 512], BF16) for i in range(3)])
        tm_ = Ring([A(f"tm{i}", [128, 1536], BF16) for i in range(2)])
        gl_ = Ring([A(f"gl{i}", [128, 48], F32) for i in range(2)])

        for kc in range(8):
            for hf in range(2):
                s_ap, s_t = stg.next()
                k.ld(s_ap, k.nsa_w_in[li, kc * 128:(kc + 1) * 128, hf * 1816:(hf + 1) * 1816], w=[s_t])
                k.cp(("dve", "act")[hf], w_in[:, kc, hf * 1816:(hf + 1) * 1816], s_ap, r=[s_t], w=[t_win[kc]])
        k.ld(gb, k.nsa_norm[li].partition_broadcast(128), w=[t_gb])
        k.ld(bg, k.nsa_b_gate[li].partition_broadcast(128), w=[t_bg])
        s_ap, s_t = stg.next()
        k.ld(s_ap[:, 0:128], k.c_rot, w=[s_t])
        k.cp("dve", rot, s_ap[:, 0:128], r=[s_t], w=[t_c])
        k.ld(cosf, k.c_cos, w=[t_c])
        k.ld(sinf, k.c_sin, w=[t_c])

        fm = []
        for c8 in range(8):
            fm.append((c8 * 128, "qT", c8 * 128, True))
        for c2 in range(2):
            fm.append((1024 + c2 * 128, "kcT", c2 * 128, False))
            fm.append((1280 + c2 * 128, "vcT", c2 * 128, False))
            fm.append((1536 + c2 * 128, "ksT", c2 * 128, True))
            fm.append((2048 + c2 * 128, "kwT", c2 * 128, True))

        for s in range(k.nseq):
            for st in range(4):
                t0 = st * 512
                for sub in range(4):
                    r0 = t0 + sub * 128
                    xt, t_xt = xr_.next()
                    k.ld(xt, x_src[s, r0:r0 + 128, :], w=[t_xt])
                    sv, t_sv = st_.next()
                    k.act(junk, xt, AF.Square, r=[t_xt], w=[t_junk, t_sv], accum=sv[:, 0:1])
                    k.act(sv[:, 1:2], sv[:, 0:1], AF.Ln, r=[t_sv], w=[t_sv], scale=1.0 / D, bias=k.c_eps[:, 0:1])
                    k.act(sv[:, 2:3], sv[:, 1:2], AF.Exp, r=[t_sv], w=[t_sv], scale=-0.5)
                    hb, t_hb = hb_.next()
                    k.stt("dve", hb, xt, sv[:, 2:3], gb, ALU.mult, ALU.mult, r=[t_xt, t_sv, t_gb], w=[t_hb])
                    pv = ps[5].bitcast(BF16).rearrange("p (a t) -> p a t", a=8)
                    for kc in range(8):
                        k.tr(pv[:, kc, :], hb[:, kc * 128:(kc + 1) * 128], k.ident_bf, r=[t_hb], w=[tps[5]])
                    k.cp("dve", hT[:, :, sub * 128:(sub + 1) * 128], pv, r=[tps[5]], w=[t_hT])
                for i, (c0, dst, d0, rope) in enumerate(fm):
                    bi = i % 2
                    for kc in range(8):
                        k.mm(ps[bi], w_in[:, kc, c0:c0 + 128], hT[:, kc, :], kc == 0, kc == 7,
                             r=[t_win[kc], t_hT], w=[tps[bi]])
                    xb, t_xb = xb_.next()
                    k.cp("act", xb, ps[bi], r=[tps[bi]], w=[t_xb])
                    if dst == "qT" or not rope:
                        k.st(T[dst][s, d0:d0 + 128, t0:t0 + 512], xb, r=[t_xb])
                    if rope:
                        pb = 2 + (i % 2)
                        k.mm(ps[pb], rot, xb, True, True, r=[t_c, t_xb], w=[tps[pb]])
                        ta, t_ta = ta_.next()
                        tb, t_tb = tb_.next()
                        k.tt("dve", ta, ps[bi], cosf[:, t0:t0 + 512], ALU.mult, r=[tps[bi], t_c, t_xb], w=[t_ta])
                        k.tt("dve", tb, ps[pb], sinf[:, t0:t0 + 512], ALU.mult, r=[tps[pb], t_c], w=[t_tb])
                        ro, t_ro = ro_.next()
                        k.tt("dve", ro, ta, tb, ALU.add, r=[t_ta, t_tb], w=[t_ro])
                        rd = "qrT" if dst == "qT" else dst
                        k.st(T[rd][s, d0:d0 + 128, t0:t0 + 512], ro, r=[t_ro])
                for sub in range(4):
                    r0 = t0 + sub * 128
                    tm, t_tm = tm_.next()
                    lh = [hT[:, kc, sub * 128:(sub + 1) * 128] for kc in range(8)]
                    for half, c0 in enumerate((1792, 2304)):
                        for kc in range(8):
                            k.mm(ps[6][:, half * 256:(half + 1) * 256], lh[kc], w_in[:, kc, c0:c0 + 256], kc == 0, kc == 7,
                                 r=[t_win[kc], t_hT], w=[tps[6]])
                    k.cp("dve", tm[:, 0:512], ps[6], r=[tps[6]], w=[t_tm])
                    for zi in range(2):
                        pb = 7 if zi == 0 else 6
                        c0 = 2560 + zi * 512
                        for kc in range(8):
                            k.mm(ps[pb], lh[kc], w_in[:, kc, c0:c0 + 512], kc == 0, kc == 7,
                                 r=[t_win[kc], t_hT], w=[tps[pb]])
                        k.act(tm[:, 512 + zi * 512:1024 + zi * 512], ps[pb], AF.Silu, r=[tps[pb]], w=[t_tm])
                    for kc in range(8):
                        k.mm(ps[7][:, 0:48], lh[kc], w_in[:, kc, 3584:3632], kc == 0, kc == 7,
                             r=[t_win[kc], t_hT], w=[tps[7]])
                    gl, t_gl = gl_.next()
                    k.tt("dve", gl, ps[7][:, 0:48], bg, ALU.add, r=[tps[7], t_bg], w=[t_gl])
                    k.act(gl, gl, AF.Sigmoid, r=[t_gl], w=[t_gl])
                    k.st(T["vsw"][s, r0:r0 + 128, :], tm[:, 0:512], r=[t_tm])
                    k.st(T["szt"][s, r0:r0 + 128, :], tm[:, 512:1536], r=[t_tm])
                    k.st(T["gate"][s, r0:r0 + 128, :], gl, r=[t_gl])
    P.barrier()


def nsa_phase_c(k, li, x_src, x_dst, T):
    nc, P = k.nc, k.P
    ps, tps = k.ps, k.tps
    with ExitStack() as es0:
        def A0(name, shape, dt):
            return es0.enter_context(nc.sbuf_tensor(f"nC{li}_{name}", shape, dt)).ap()
        w_out = A0("wout", [128, 8, 1024], BF16)
        t_wout = Tok()
        kcmpT = A0("kcmpT", [128, k.nseq, 4, 128], BF16)
        vcmp = A0("vcmp", [128, k.nseq, 4, 128], BF16)
        t_cmp = Tok()
        with ExitStack() as es:
            def A(name, shape, dt):
                return es.enter_context(nc.sbuf_tensor(f"nB{li}_{name}", shape, dt)).ap()
            stg = Ring([A(f"stg{i}", [128, 2048], F32) for i in range(2)])
            w1 = A("w1", [128, 2, 32, 128], BF16)
            w2k = A("w2k", [128, 128], BF16)
            w2v = A("w2v", [128, 64], BF16)
            peT = A("peT", [128, 2, 32], BF16)
            bh = A("bh", [128, 2], F32)
            t_w = Tok()
            kc2 = A("kc2", [128, 2, S], BF16)
            vc2 = A("vc2", [128, 2, S], BF16)
            t_kv = Tok()
            hTs_ = Ring([A(f"hTs{i}", [128, 128], BF16) for i in range(2)])
            for c8 in range(0, 8, 2):
                s_ap, s_t = stg.next()
                sv = s_ap.rearrange("p (c n) -> p c n", c=2)
                k.ld(sv, k.nsa_w_out[li, c8 * 128:(c8 + 2) * 128, :].rearrange("(c p) n -> p c n", p=128), w=[s_t])
                k.cp("dve", w_out[:, c8:c8 + 2, :], sv, r=[s_t], w=[t_wout])
            for kv in range(2):
                for hf in range(2):
                    s_ap, s_t = stg.next()
                    sv = s_ap.rearrange("p (l n) -> p l n", l=16)
                    src = k.nsa_cmp_w1[li, kv, hf * 1024:(hf + 1) * 1024, :].rearrange("(l d) n -> d l n", d=64)
                    k.ld(sv[0:64], src, w=[s_t])
                    k.ld(sv[64:128], src, w=[s_t])
                    k.cp("dve", w1[:, kv, hf * 16:(hf + 1) * 16, :], sv, r=[s_t], w=[t_w])
            s_ap, s_t = stg.next()
            k.ld(s_ap[:, 0:64], k.nsa_cmp_w2[li, 0], w=[s_t])
            k.ld(s_ap[:, 64:128], k.nsa_cmp_w2[li, 1], w=[s_t])
            pe_v = s_ap[:, 128:192].rearrange("p (a l) -> p a l", a=2)
            k.ld(pe_v[0:64], k.nsa_peT[li].rearrange("a d l -> d a l"), w=[s_t])
            k.ld(pe_v[64:128], k.nsa_peT[li].rearrange("a d l -> d a l"), w=[s_t])
            k.cp("dve", w2k[:, 0:64], s_ap[:, 0:64], r=[s_t], w=[t_w])
            k.cp("dve", w2k[:, 64:128], s_ap[:, 0:64], r=[s_t], w=[t_w])
            k.cp("dve", w2v, s_ap[:, 64:128], r=[s_t], w=[t_w])
            k.cp("dve", peT, pe_v, r=[s_t], w=[t_w])
            for kv in range(2):
                for l in range(32):
                    k.mm(ps[7][:, kv:kv + 1], w1[0:64, kv, l, :], peT[0:64, kv, l:l + 1], l == 0, l == 31, r=[t_w], w=[tps[7]])
            k.cp("dve", bh, ps[7][:, 0:2], r=[tps[7]], w=[t_w])
            for s in range(k.nseq):
                k.ld(vcmp[:, s, :, 64:97], k.c_ovl.rearrange("p (g n) -> p g n", g=4), w=[t_cmp])
                k.ld(kc2, T["kcT"][s].rearrange("(c p) t -> p c t", p=128), w=[t_kv])
                k.ld(vc2, T["vcT"][s].rearrange("(c p) t -> p c t", p=128), w=[t_kv])
                for kv in range(2):
                    src = kc2 if kv == 0 else vc2
                    for g in range(4):
                        base = (g % 2) * 64
                        for l in range(32):
                            k.mm(ps[6][:, 0:127], w1[base:base + 64, kv, l, :], src[base:base + 64, g // 2, l:l + 16 * 126 + 1:16],
                                 l == 0, l == 31, r=[t_w, t_kv], w=[tps[6]])
                        hTs, t_hTs = hTs_.next()
                        k.act(hTs[:, 0:127], ps[6][:, 0:127], AF.Silu, r=[tps[6], t_w], w=[t_hTs], bias=bh[:, kv:kv + 1])
                        if kv == 0:
                            k.mm(ps[7][:, 0:127], w2k, hTs[:, 0:127], True, True, r=[t_w, t_hTs], w=[tps[7]])
                            k.cp("dve", kcmpT[:, s, g, 0:127], ps[7][:, 0:127], r=[tps[7]], w=[t_cmp])
                        else:
                            k.mm(ps[7][0:127, 0:64], hTs[:, 0:127], w2v, True, True, r=[t_w, t_hTs], w=[tps[7]])
                            k.cp("dve", vcmp[0:127, s, g, 0:64], ps[7][0:127, 0:64], r=[tps[7]], w=[t_cmp])
        P.barrier()
        with ExitStack() as es:
            def A(name, shape, dt):
                return es.enter_context(nc.sbuf_tensor(f"nC{li}_{name}", shape, dt)).ap()
            t_c = Tok()
            Ef = A("Ef", [128, 4, S], BF16)
            cmpn = A("cmpn", [128, S], BF16)
            fmul = A("fmul", [128, 16, 32], F32)
            fadd = A("fadd", [128, 16, 32], F32)
            ks2 = A("ks2", [128, 4, S], BF16)
            kw2 = A("kw2", [128, 4, S], BF16)
            vs = A("vs", [128, 16, 4, 65], BF16)
            vw = A("vw", [128, 16, 4, 65], BF16)
            t_res = Tok()
            q_ = Ring([A(f"q{i}", [128, 8, 512], BF16) for i in range(1)])
            qr_ = Ring([A(f"qr{i}", [128, 8, 512], BF16) for i in range(1)])
            ex_ = Ring([A(f"ex{i}", [128, 512], BF16) for i in range(3)])
            oacc = A("oacc", [128, 4, 16, 64], F32)
            t_oacc = [Tok() for _ in range(4)]
            ucmp = A("ucmp", [128, 4, 4, 33], F32)
            rdc = A("rdc", [128, 4, 4], F32)
            t_ucmp = Tok()
            tmp_ = Ring([A(f"tmp{i}", [128, 4, 64], F32) for i in range(3)])
            imp_ = Ring([A(f"imp{i}", [128, 32], F32) for i in range(2)])
            rk_ = Ring([A(f"rk{i}", [128, 32, 32], F32) for i in range(2)])
            rs_ = Ring([A(f"rs{i}", [128, 32], F32) for i in range(2)])
            rd_ = Ring([A(f"rd{i}", [128, 16], F32) for i in range(4)])
            negm = A("negm", [128, 4, 4, 32], BF16)
            t_negm = [Tok() for _ in range(4)]
            negT = A("negT", [128, 512], BF16)
            t_negT = Tok()
            gat_ = Ring([A(f"gat{i}", [128, 4, 48], F32) for i in range(2)])
            szt_ = Ring([A(f"szt{i}", [128, 1024], BF16) for i in range(2)])
            xr_ = Ring([A(f"xr{i}", [128, 1024], F32) for i in range(2)])
            og_ = Ring([A(f"og{i}", [128, 1024], BF16) for i in range(2)])
            ogT_ = Ring([A(f"ogT{i}", [128, 8, 128], BF16) for i in range(2)])
            yo_ = Ring([A(f"yo{i}", [128, 1024], F32) for i in range(2)])

            k.ld(Ef, k.c_E.rearrange("p (g n) -> p g n", g=4), w=[t_c])
            k.ld(cmpn, k.c_cmpn, w=[t_c])
            k.ld(fmul, k.c_fmul, w=[t_c])
            k.ld(fadd, k.c_fadd, w=[t_c])
            k.memset("pool", vs[:, :, :, 64:65], 1.0, w=[t_res])
            k.memset("pool", vw[:, :, :, 64:65], 1.0, w=[t_res])

            def evac(pov, h, b, first, gat, t_gat, clamp, g):
                rd, t_rd = rd_.next()
                if clamp:
                    k.ts("dve", rd[:, 0:4], pov[:, :, 64], 1e-30, None, ALU.max, None, r=[tps_of[0]], w=[t_rd])
                else:
                    k.cp("dve", rd[:, 0:4], pov[:, :, 64], r=[tps_of[0]], w=[t_rd])
                k.recip(rd[:, 4:8], rd[:, 0:4], r=[t_rd], w=[t_rd])
                k.tt("dve", rd[:, 8:12], rd[:, 4:8], gat[:, :, 3 * h + b], ALU.mult, r=[t_rd, t_gat], w=[t_rd])
                if first:
                    k.tt("dve", oacc[:, :, h, :], pov[:, :, 0:64], bcast_last(rd[:, 8:12], 64), ALU.mult,
                         r=[tps_of[0], t_rd], w=[t_oacc[g]])
                else:
                    for sub in range(4):
                        k.stt("dve", oacc[:, sub, h, :], pov[:, sub, 0:64], rd[:, 8 + sub:9 + sub], oacc[:, sub, h, :],
                              ALU.mult, ALU.add, r=[tps_of[0], t_rd, t_oacc[g]], w=[t_oacc[g]])
                return rd, t_rd

            tps_of = [None]
            for s in range(k.nseq):
                for hf in range(2):
                    k.ld(ks2[hf * 64:(hf + 1) * 64], T["ksT"][s].rearrange("(g d) t -> d g t", d=64), w=[t_res])
                    k.ld(kw2[hf * 64:(hf + 1) * 64], T["kwT"][s].rearrange("(g d) t -> d g t", d=64), w=[t_res])
                vsw_v = T["vsw"][s].rearrange("(kt p) (b g d) -> p kt b g d", p=128, b=2, g=4)
                for kt4 in range(0, 16, 4):
                    for g in range(4):
                        k.ld(vs[:, kt4:kt4 + 4, g, 0:64], vsw_v[:, kt4:kt4 + 4, 0, g], w=[t_res])
                        k.ld(vw[:, kt4:kt4 + 4, g, 0:64], vsw_v[:, kt4:kt4 + 4, 1, g], w=[t_res])
                for qt in range(4):
                    t0 = qt * 512
                    q, t_q = q_.next()
                    qr, t_qr = qr_.next()
                    gat, t_gat = gat_.next()
                    k.ld(q, T["qT"][s, :, t0:t0 + 512].rearrange("(c p) t -> p c t", p=128), w=[t_q])
                    k.ld(qr, T["qrT"][s, :, t0:t0 + 512].rearrange("(c p) t -> p c t", p=128), w=[t_qr])
                    k.ld(gat, T["gate"][s, t0:t0 + 512, :].rearrange("(a p) n -> p a n", p=128), w=[t_gat])
                    need_sel = qt >= 2
                    for g in range(4):
                        for hh in range(4):
                            h = 4 * g + hh
                            ch, base = h // 2, (h % 2) * 64
                            sb = h % 2
                            k.mm(ps[sb][0:127, :], kcmpT[base:base + 64, s, g, 0:127], q[base:base + 64, ch, :], True, False,
                                 r=[t_cmp, t_q], w=[tps[sb]])
                            k.mm(ps[sb][0:127, :], k.ident_bf[0:127, 0:127], cmpn[0:127, t0:t0 + 512], False, True,
                                 r=[t_c], w=[tps[sb]])
                            ex, t_ex = ex_.next()
                            k.act(ex[0:127, :], ps[sb][0:127, :], AF.Exp, r=[tps[sb]], w=[t_ex], scale=SCALE)
                            ob = 2 + (h % 2)
                            pov = ps[ob][:, 0:388].rearrange("p (a n) -> p a n", a=4)
                            for sub in range(4):
                                k.mm(pov[:, sub, :], ex[0:127, sub * 128:(sub + 1) * 128], vcmp[0:127, s, g, 0:97], True, True,
                                     r=[t_ex, t_cmp], w=[tps[ob]])
                            tps_of[0] = tps[ob]
                            rd, t_rd = evac(pov, h, 0, True, gat, t_gat, True, g)
                            if need_sel:
                                k.cp("dve", ucmp[:, :, hh, :], pov[:, :, 64:97], r=[tps[ob]], w=[t_ucmp])
                                k.cp("dve", rdc[:, :, hh], rd[:, 4:8], r=[t_rd], w=[t_ucmp])
                        if need_sel:
                            for sub in range(4):
                                imp, t_imp = imp_.next()
                                k.ts("dve", imp, ucmp[:, sub, 0, 1:33], rdc[:, sub, 0:1], None, ALU.mult, None,
                                     r=[t_ucmp], w=[t_imp])
                                for hh in range(1, 4):
                                    k.stt("dve", imp, ucmp[:, sub, hh, 1:33], rdc[:, sub, hh:hh + 1], imp, ALU.mult, ALU.add,
                                          r=[t_ucmp, t_imp], w=[t_imp])
                                tt_ = 4 * qt + sub
                                k.tt("dve", imp, imp, fmul[:, tt_, :], ALU.mult, r=[t_imp, t_c], w=[t_imp])
                                k.tt("dve", imp, imp, fadd[:, tt_, :], ALU.add, r=[t_imp], w=[t_imp])
                                rk, t_rk = rk_.next()
                                in0 = imp.unsqueeze(1).to_broadcast([128, 32, 32])
                                in1 = imp.unsqueeze(2).to_broadcast([128, 32, 32])
                                k.tt("dve", rk, in0, in1, ALU.is_gt, r=[t_imp], w=[t_rk])
                                rs, t_rs = rs_.next()
                                k.P.op("dve", (lambda o, i: (lambda e: e.reduce_sum(o, i, AX.X)))(rs, rk), r=[t_rk], w=[t_rs])
                                k.ts("dve", negm[:, sub, g, :], rs, 15.5, NEG, ALU.is_gt, ALU.mult, r=[t_rs], w=[t_negm[sub]])
                    if need_sel:
                        pv = ps[4].bitcast(BF16)[:, 0:512].rearrange("p (a t) -> p a t", a=4)
                        for sub in range(4):
                            k.tr(pv[:, sub, :], negm[:, sub, :, :].rearrange("p g n -> p (g n)"), k.ident_bf,
                                 r=[t_negm[sub]], w=[tps[4]])
                        k.cp("dve", negT, ps[4].bitcast(BF16)[:, 0:512], r=[tps[4]], w=[t_negT])
                    for br in range(2):
                        kk = ks2 if br == 0 else kw2
                        vv = vs if br == 0 else vw
                        kt_lo = 0 if br == 0 else max(0, 4 * qt - 4)
                        kt_hi = 4 * qt + 3
                        for g in range(4):
                            for hh in range(4):
                                h = 4 * g + hh
                                ch, base = h // 2, (h % 2) * 64
                                ob = 2 + (h % 2)
                                pov = ps[ob][:, 0:260].rearrange("p (a n) -> p a n", a=4)
                                first_pv = True

                                def qk(kt):
                                    sb = kt % 2
                                    r_ = kt - 4 * qt
                                    extra = []
                                    if br == 0 and need_sel:
                                        extra.append((Ef[:, g, kt * 128:(kt + 1) * 128], negT, [t_c, t_negT]))
                                    k.mm(ps[sb], kk[base:base + 64, g, kt * 128:(kt + 1) * 128], qr[base:base + 64, ch, :],
                                         True, len(extra) == 0, r=[t_res, t_qr], w=[tps[sb]])
                                    for ei, (lt, rh, rt) in enumerate(extra):
                                        k.mm(ps[sb], lt, rh, False, ei == len(extra) - 1, r=rt, w=[tps[sb]])
                                    ex, t_ex = ex_.next()
                                    k.act(ex, ps[sb], AF.Exp, r=[tps[sb]], w=[t_ex], scale=SCALE)
                                    for sub in range(4):
                                        blk = ex[:, sub * 128:(sub + 1) * 128]
                                        if kt == 4 * qt + sub:
                                            k.tt("dve", blk, blk, k.caus_bf, ALU.mult, r=[t_ex], w=[t_ex])
                                        elif br == 1 and kt == 4 * qt + sub - 4:
                                            k.tt("dve", blk, blk, k.low_bf, ALU.mult, r=[t_ex], w=[t_ex])
                                    return ex, t_ex

                                pend = qk(kt_lo)
                                for kt in range(kt_lo, kt_hi + 1):
                                    ex, t_ex = pend
                                    if kt + 1 <= kt_hi:
                                        pend = qk(kt + 1)
                                    for sub in range(4):
                                        hi_s = 4 * qt + sub
                                        lo_s = 0 if br == 0 else max(0, hi_s - 4)
                                        if kt < lo_s or kt > hi_s:
                                            continue
                                        k.mm(pov[:, sub, :], ex[:, sub * 128:(sub + 1) * 128], vv[:, kt, g, :],
                                             first_pv, kt == hi_s, r=[t_ex, t_res], w=[tps[ob]])
                                        first_pv = False
                                tps_of[0] = tps[ob]
                                evac(pov, h, 1 + br, False, gat, t_gat, False, g)
                    for sub in range(4):
                        r0 = t0 + sub * 128
                        szt, t_szt = szt_.next()
                        xr, t_xr = xr_.next()
                        k.ld(szt, T["szt"][s, r0:r0 + 128, :], w=[t_szt])
                        k.ld(xr, x_src[s, r0:r0 + 128, :], w=[t_xr])
                        og, t_og = og_.next()
                        k.tt("dve", og, oacc[:, sub, :, :].rearrange("p h d -> p (h d)"), szt, ALU.mult,
                             r=t_oacc + [t_szt], w=[t_og])
                        pv = ps[4].bitcast(BF16).rearrange("p (a t) -> p a t", a=8)
                        for c8 in range(8):
                            k.tr(pv[:, c8, :], og[:, c8 * 128:(c8 + 1) * 128], k.ident_bf, r=[t_og], w=[tps[4]])
                        ogT, t_ogT = ogT_.next()
                        k.cp("act", ogT, pv, r=[tps[4]], w=[t_ogT])
                        yo, t_yo = yo_.next()
                        for half in range(2):
                            pb = 5 + half
                            for c8 in range(8):
                                k.mm(ps[pb], ogT[:, c8, :], w_out[:, c8, half * 512:(half + 1) * 512], c8 == 0, c8 == 7,
                                     r=[t_ogT, t_wout], w=[tps[pb]])
                            k.tt("dve", yo[:, half * 512:(half + 1) * 512], ps[pb], xr[:, half * 512:(half + 1) * 512], ALU.add,
                                 r=[tps[pb], t_xr], w=[t_yo])
                        k.st(x_dst[s, r0:r0 + 128, :], yo, r=[t_yo])
    P.barrier()


def build_program(nseq=NSEQ, layers=(0, 1, 2, 3), final_norm=True, limit=None):
    nc = bass.Bass("TRN2", target_bir_lowering=False)
    k = K(nc, nseq)
    k.P.limit = limit

    def din(name, shape, dt=F32):
        return nc.dram_tensor(name, list(shape), dt, kind="ExternalInput").ap()

    def dscr(name, shape, dt):
        return nc.dram_tensor(name, list(shape), dt, kind="Internal").ap()

    x = din("x", [nseq, S, D])
    k.ml_norm = din("ml_norm", [2, D])
    k.ml_w_in = din("ml_w_in", [2, D, 4096])
    k.ml_bd = din("ml_bd", [2, 48, 128, 128])
    k.ml_vec = din("ml_vec", [2, 128, 16, 8])
    k.ml_wif = din("ml_wif", [2, 128, 48, 8])
    k.ml_bif = din("ml_bif", [2, 8, 1])
    k.ml_w_out = din("ml_w_out", [2, ML_INNER, D])
    k.final_norm = din("final_norm", [1, D])
    k.nsa_norm = din("nsa_norm", [2, D])
    k.nsa_w_in = din("nsa_w_in", [2, D, 3632])
    k.nsa_b_gate = din("nsa_b_gate", [2, 48])
    k.nsa_peT = din("nsa_peT", [2, 2, 64, 32])
    k.nsa_cmp_w1 = din("nsa_cmp_w1", [2, 2, 2048, 128])
    k.nsa_cmp_w2 = din("nsa_cmp_w2", [2, 2, 128, 64])
    k.nsa_w_out = din("nsa_w_out", [2, D, D])
    k.c_rot = din("c_rot", [128, 128])
    k.c_cos = din("c_cos", [128, S])
    k.c_sin = din("c_sin", [128, S])
    k.c_E = din("c_E", [128, 4 * S], BF16)
    k.c_causn = din("c_causn", [128, 4 * 512], BF16)
    k.c_lown = din("c_lown", [128, 4 * 512], BF16)
    k.c_cmpn = din("c_cmpn", [128, S], BF16)
    k.c_ovl = din("c_ovl", [128, 4 * 33], BF16)
    k.c_fmul = din("c_fmul", [128, 16, 32])
    k.c_fadd = din("c_fadd", [128, 16, 32])
    c_ident = din("c_ident", [128, 128])
    c_caus = din("c_caus", [128, 128])
    y = nc.dram_tensor("y", [nseq, S, D], F32, kind="ExternalOutput").ap()
    xres = dscr("xres", [nseq, S, D], F32)
    T = {
        "qT": dscr("s_qT", [nseq, ML_INNER, S], BF16),
        "kT": dscr("s_kT", [nseq, ML_INNER, S], BF16),
        "sxT": dscr("s_sxT", [nseq, ML_INNER, S], BF16),
        "szT": dscr("s_szT", [nseq, ML_INNER, S], BF16),
        "ktok": dscr("s_ktok", [nseq, S, ML_INNER], BF16),
        "vtok": dscr("s_vtok", [nseq, S, ML_INNER], BF16),
        "gtok": dscr("s_gtok", [nseq, S, 16], F32),
    }
    TN = {
        "qT": dscr("n_qT", [nseq, 1024, S], BF16),
        "qrT": dscr("n_qrT", [nseq, 1024, S], BF16),
        "kcT": dscr("n_kcT", [nseq, 256, S], BF16),
        "vcT": dscr("n_vcT", [nseq, 256, S], BF16),
        "ksT": dscr("n_ksT", [nseq, 256, S], BF16),
        "kwT": dscr("n_kwT", [nseq, 256, S], BF16),
        "vsw": dscr("n_vsw", [nseq, S, 512], BF16),
        "szt": dscr("n_szt", [nseq, S, 1024], BF16),
        "gate": dscr("n_gate", [nseq, S, 48], F32),
    }

    def C(name, shape, dt):
        return nc.alloc_sbuf_tensor(name, shape, dt).ap()
    k.ident_f = C("ident_f", [128, 128], F32)
    k.ident_bf = C("ident_bf", [128, 128], BF16)
    k.caus_f = C("caus_f", [128, 128], F32)
    k.ones_f = C("ones_f", [128, 128], F32)
    k.caus_bf = C("caus_bf", [128, 128], BF16)
    k.low_bf = C("low_bf", [128, 128], BF16)
    k.c_eps = C("c_eps", [128, 1], F32)
    k.c_one = C("c_one", [128, 1], F32)
    k.ps = [nc.alloc_psum_tensor(f"ps{i}", [128, 512], F32).ap() for i in range(8)]
    k.tps = [Tok(f"ps{i}", x=True) for i in range(8)]
    tc = Tok()
    k.ld(k.ident_f, c_ident, w=[tc])
    k.ld(k.caus_f, c_caus, w=[tc])
    k.cp("dve", k.ident_bf, k.ident_f, r=[tc], w=[tc])
    k.memset("dve", k.ones_f, 1.0, w=[tc])
    k.cp("dve", k.caus_bf, k.caus_f, r=[tc], w=[tc])
    k.ts("dve", k.low_bf, k.caus_f, -1.0, 1.0, ALU.mult, ALU.add, r=[tc], w=[tc])
    k.memset("dve", k.c_eps, RMS_EPS, w=[tc])
    k.memset("dve", k.c_one, 1.0, w=[tc])
    k.P.barrier()

    cur = x
    for L in layers:
        li = L // 2
        if L % 2 == 0:
            ml_phase_a(k, li, cur, T)
            ml_phase_b(k, li, cur, xres, T)
        else:
            nsa_phase_a(k, li, cur, TN)
            nsa_phase_c(k, li, cur, xres, TN)
        cur = xres

    print("n_ins before final", k.P.n_ins, flush=True)
    k.P.limit = None
    with ExitStack() as es:
        def A(name, shape, dt):
            return es.enter_context(nc.sbuf_tensor(f"fin_{name}", shape, dt)).ap()
        gb = A("gb", [128, D], F32)
        t_gb = Tok()
        k.ld(gb, k.final_norm[0].partition_broadcast(128), w=[t_gb])
        xr_ = Ring([A(f"x{i}", [128, D], F32) for i in range(3)])
        junk = A("junk", [128, D], F32)
        t_junk = Tok()
        st_ = Ring([A(f"st{i}", [128, 4], F32) for i in range(3)])
        t_y = Tok()
        for s in range(nseq):
            for c in range(NT):
                r0 = c * 128
                xt, t_xt = xr_.next()
                k.ld(xt, cur[s, r0:r0 + 128, :], w=[t_xt])
                if final_norm:
                    sv, t_sv = st_.next()
                    k.act(junk, xt, AF.Square, r=[t_xt], w=[t_junk, t_sv], accum=sv[:, 0:1])
                    k.act(sv[:, 1:2], sv[:, 0:1], AF.Ln, r=[t_sv], w=[t_sv], scale=1.0 / D, bias=k.c_eps[:, 0:1])
                    k.act(sv[:, 2:3], sv[:, 1:2], AF.Exp, r=[t_sv], w=[t_sv], scale=-0.5)
                    k.stt("dve", xt, xt, sv[:, 2:3], gb, ALU.mult, ALU.mult, r=[t_xt, t_sv, t_gb], w=[t_xt])
                k.P.dma("sp", y[s, r0:r0 + 128, :], xt, r=[t_xt], w=[t_y])
        k.P._emit_waits("sp", k.P._deps([t_y], [t_y]))
        deps = {kk: v for kk, v in k.P.cnt.items() if kk.startswith("d_sp") and v > 0}
        k.P._emit_waits("sp", deps)
    k.P.emit()
    return nc


def _block_diag(w):
    out = np.zeros((16, 128, 128), np.float32)
    w4 = w.reshape(16, 32, 4, 4)
    for n in range(32):
        out[:, 4 * n:4 * n + 4, 4 * n:4 * n + 4] = w4[:, n].transpose(0, 2, 1)
    return out


_CONSTS = {}


def _nsa_consts():
    if _CONSTS:
        return _CONSTS
    import ml_dtypes
    bf = ml_dtypes.bfloat16
    c = {}
    rot = np.zeros((128, 128), np.float32)
    for hb in (0, 64):
        for d in range(8):
            rot[hb + d + 8, hb + d] = -1.0
            rot[hb + d, hb + d + 8] = 1.0
    c["c_rot"] = rot
    pos = np.arange(S, dtype=np.float32)
    inv_freq = (np.float32(500000.0) ** (-np.arange(0, 16, 2, dtype=np.float32) / np.float32(16))).astype(np.float32)
    ang = (pos[:, None] * inv_freq[None, :]).astype(np.float32)
    cosf = np.ones((128, S), np.float32)
    sinf = np.zeros((128, S), np.float32)
    for hb in (0, 64):
        for d in range(16):
            cosf[hb + d] = np.cos(ang[:, d % 8])
            sinf[hb + d] = np.sin(ang[:, d % 8])
    c["c_cos"], c["c_sin"] = cosf, sinf
    E = np.zeros((128, 4, S), np.float32)
    key = np.arange(S)
    for g in range(4):
        E[g * 32 + key // 64, g, key] = 1.0
    c["c_E"] = E.reshape(128, 4 * S).astype(bf)
    kk = np.arange(128)[:, None]
    tt = np.arange(512)[None, :]
    causn = np.zeros((128, 4, 512), np.float32)
    lown = np.zeros((128, 4, 512), np.float32)
    for r in range(4):
        valid = kk <= tt - 128 * r
        causn[:, r, :] = np.where(valid, 0.0, NEG)
        lown[:, r, :] = np.where(valid, NEG, 0.0)
    c["c_causn"] = causn.reshape(128, 2048).astype(bf)
    c["c_lown"] = lown.reshape(128, 2048).astype(bf)
    cc = np.arange(128)[:, None]
    tok = np.arange(S)[None, :]
    c["c_cmpn"] = np.where(16 * cc + 31 <= tok, 0.0, NEG).astype(np.float32).astype(bf)
    cs = np.arange(127) * 16
    ss = np.arange(32) * 64
    ov = np.clip(np.minimum(cs[:, None] + 32, ss[None, :] + 64) - np.maximum(cs[:, None], ss[None, :]), 0, None) / 16.0
    ovl = np.zeros((128, 4, 33), np.float32)
    ovl[:, :, 0] = 1.0
    ovl[:127, :, 1:] = ov[:, None, :]
    c["c_ovl"] = ovl.reshape(128, 4 * 33).astype(bf)
    p = np.arange(S)
    blk = np.arange(32)
    dist = (p // 64)[:, None] - blk[None, :]
    forced = (blk[None, :] == 0) | ((dist >= 0) & (dist < 2))
    fmul = np.where(forced | (dist < 0), 0.0, 1.0).astype(np.float32)
    fadd = np.where(forced, 1e9, np.where(dist >= 0, 0.0, -1.0)).astype(np.float32)
    c["c_fmul"] = np.ascontiguousarray(fmul.reshape(16, 128, 32).transpose(1, 0, 2))
    c["c_fadd"] = np.ascontiguousarray(fadd.reshape(16, 128, 32).transpose(1, 0, 2))
    _CONSTS.update(c)
    return _CONSTS


def host_layout(inp):
    f = lambda a: np.ascontiguousarray(np.asarray(a, dtype=np.float32))
    d = {}
    d["ml_norm"] = f(inp["ml_norm"])
    d["ml_w_in"] = f(inp["ml_w_in"])
    bd = np.zeros((2, 48, 128, 128), np.float32)
    vec = np.zeros((2, 128, 16, 8), np.float32)
    for li in range(2):
        for g3, nm in enumerate(("ml_w_q", "ml_w_k", "ml_w_v")):
            bd[li, g3 * 16:(g3 + 1) * 16] = _block_diag(np.asarray(inp[nm][li], np.float32))
        cw = np.asarray(inp["ml_conv_w"][li], np.float32)
        for tap in range(4):
            vec[li, :, :, tap] = cw[tap].reshape(16, 128).T
        vec[li, :, :, 4] = np.asarray(inp["ml_conv_b"][li], np.float32).reshape(16, 128).T
        vec[li, :, :, 5] = np.asarray(inp["ml_ln_w"][li], np.float32).reshape(16, 128).T
        vec[li, :, :, 6] = np.asarray(inp["ml_skip"][li], np.float32).reshape(16, 128).T
    d["ml_bd"] = bd
    d["ml_vec"] = vec
    d["ml_wif"] = f(np.asarray(inp["ml_w_if"], np.float32).reshape(2, 48, 128, 8).transpose(0, 2, 1, 3))
    d["ml_bif"] = f(np.asarray(inp["ml_b_if"], np.float32).reshape(2, 8, 1))
    d["ml_w_out"] = f(inp["ml_w_out"])
    d["final_norm"] = f(np.asarray(inp["final_norm"], np.float32).reshape(1, D))
    d["nsa_norm"] = f(inp["nsa_norm"])
    d["nsa_w_in"] = f(inp["nsa_w_in"])
    d["nsa_b_gate"] = f(inp["nsa_b_gate"])
    d["nsa_peT"] = f(np.asarray(inp["nsa_cmp_pe"], np.float32).transpose(0, 1, 3, 2))
    d["nsa_cmp_w1"] = f(inp["nsa_cmp_w1"])
    d["nsa_cmp_w2"] = f(inp["nsa_cmp_w2"])
    d["nsa_w_out"] = f(inp["nsa_w_out"])
    d.update(_nsa_consts())
    d["c_ident"] = np.eye(128, dtype=np.float32)
    d["c_caus"] = np.triu(np.ones((128, 128), np.float32))
    return d


_NC_CACHE = {}


def kernel(**inputs):
    x = np.asarray(inputs["x"], np.float32)
    shared = host_layout(inputs)
    if "nc" not in _NC_CACHE:
        _NC_CACHE["nc"] = build_program()
    nc = _NC_CACHE["nc"]
    in_maps = []
    for c in range(NCORES):
        m = dict(shared)
        m["x"] = np.ascontiguousarray(x[c * NSEQ:(c + 1) * NSEQ])
        in_maps.append(m)
    res = run_bass_kernel_spmd(nc, in_maps, core_ids=list(range(NCORES)))
    return np.concatenate([r["y"] for r in res.results], axis=0)
```

```python
from contextlib import ExitStack
import numpy as np
import concourse.bass as bass
import concourse.mybir as mybir
from concourse.bass_utils import run_bass_kernel_spmd

F32 = mybir.dt.float32
BF16 = mybir.dt.bfloat16
AF = mybir.ActivationFunctionType
ALU = mybir.AluOpType
AX = mybir.AxisListType

S = 2048
D = 1024
NT = S // 128
NSEQ = 2
NCORES = 8
ML_INNER = 2048
NH = 4
DH = 512
RMS_EPS = 1e-6
LN_EPS = 1e-6
NEG = -30000.0


class Tok:
    __slots__ = ("name", "w", "r", "x")

    def __init__(self, name="", x=False):
        self.name = name
        self.w = None
        self.r = {}
        self.x = x


class Prog:
    ENG = ("pe", "dve", "act", "pool", "sp")

    def __init__(self, nc, n_dma_ring=12):
        self.nc = nc
        self.q = {e: [] for e in self.ENG}
        self.sems = {}
        self.cnt = {}
        for e in self.ENG:
            self.sems[e] = nc.alloc_semaphore(name=f"c_{e}")
            self.cnt[e] = 0
        self.ring = {}
        self.ring_pos = {}
        for qn in ("sp", "act", "pool"):
            ks = []
            for i in range(n_dma_ring):
                k = f"d_{qn}{i}"
                self.sems[k] = nc.alloc_semaphore(name=k)
                self.cnt[k] = 0
                ks.append(k)
            self.ring[qn] = ks
            self.ring_pos[qn] = 0
        self.seen = {e: {} for e in self.ENG}
        self.n_ins = 0

    def _deps(self, r, w):
        deps = {}

        def add(k, v):
            if deps.get(k, 0) < v:
                deps[k] = v
        for t in r:
            if t.w is not None:
                add(*t.w)
            if t.x:
                for k, v in t.r.items():
                    add(k, v)
        for t in w:
            if t.w is not None:
                add(*t.w)
            for k, v in t.r.items():
                add(k, v)
        return deps

    def _emit_waits(self, e, deps):
        seen = self.seen[e]
        for k, v in deps.items():
            if k == e and e == "pe":
                continue
            if seen.get(k, 0) >= v:
                continue
            seen[k] = v
            self.q[e].append(("w", self.sems[k], v))

    limit = None

    def op(self, e, fn, r=(), w=()):
        if self.limit is not None and self.n_ins >= self.limit:
            return
        self._emit_waits(e, self._deps(r, w))
        self.cnt[e] += 1
        c = self.cnt[e]
        self.q[e].append(("i", fn, self.sems[e]))
        self.n_ins += 1
        for t in w:
            t.w = (e, c)
            t.r = {}
        for t in r:
            if t.r.get(e, 0) < c:
                t.r[e] = c

    def dma(self, qn, out, in_, r=(), w=()):
        if self.limit is not None and self.n_ins >= self.limit:
            return
        deps = self._deps(r, w)
        k = self.ring[qn][self.ring_pos[qn]]
        self.ring_pos[qn] = (self.ring_pos[qn] + 1) % len(self.ring[qn])
        if self.cnt[k] > 0 and deps.get(k, 0) < self.cnt[k]:
            deps[k] = self.cnt[k]
        self._emit_waits(qn, deps)
        self.cnt[k] += 16
        c = self.cnt[k]
        self.q[qn].append(("d", out, in_, self.sems[k]))
        self.n_ins += 1
        for t in w:
            t.w = (k, c)
            t.r = {}
        for t in r:
            if t.r.get(k, 0) < c:
                t.r[k] = c

    def barrier(self):
        deps = {k: v for k, v in self.cnt.items() if v > 0}
        for e in self.ENG:
            self._emit_waits(e, dict(deps))

    def emit(self):
        nc = self.nc
        with nc.Block() as block:
            def mk(e):
                def body(engine):
                    for it in self.q[e]:
                        if it[0] == "w":
                            engine.wait_ge(it[1], it[2])
                        elif it[0] == "i":
                            it[1](engine).then_inc(it[2], 1)
                        else:
                            engine.dma_start(out=it[1], in_=it[2]).then_inc(it[3], 16)
                return body
            block.tensor(mk("pe"))
            block.vector(mk("dve"))
            block.scalar(mk("act"))
            block.gpsimd(mk("pool"))
            block.sync(mk("sp"))


class Ring:
    def __init__(self, aps):
        self.items = [(a, Tok()) for a in aps]
        self.i = 0

    def next(self):
        it = self.items[self.i]
        self.i = (self.i + 1) % len(self.items)
        return it


class K:
    def __init__(self, nc, nseq):
        self.nc = nc
        self.P = Prog(nc)
        self.nseq = nseq

    def mm(self, out, lhsT, rhs, start, stop, r, w):
        self.P.op("pe", lambda e: e.matmul(out, lhsT=lhsT, rhs=rhs, start=start, stop=stop), r, w)

    def tr(self, out, in_, ident, r, w):
        self.P.op("pe", lambda e: e.transpose(out, in_, ident), r, w)

    def cp(self, eng, out, in_, r, w):
        if eng == "act":
            self.P.op("act", lambda e: e.activation(out, in_, AF.Copy), r, w)
        else:
            self.P.op(eng, lambda e: e.tensor_copy(out, in_), r, w)

    def act(self, out, in_, func, r, w, bias=None, scale=None, accum=None):
        kw = {}
        if bias is not None:
            kw["bias"] = bias
        if scale is not None:
            kw["scale"] = scale
        if accum is not None:
            kw["accum_out"] = accum
        self.P.op("act", lambda e: e.activation(out, in_, func, **kw), r, w)

    def tt(self, eng, out, in0, in1, op, r, w):
        self.P.op(eng, lambda e: e.tensor_tensor(out, in0, in1, op), r, w)

    def ts(self, eng, out, in0, s1, s2, op0, op1, r, w):
        if s2 is None:
            self.P.op(eng, lambda e: e.tensor_scalar(out, in0, s1, None, op0), r, w)
        else:
            self.P.op(eng, lambda e: e.tensor_scalar(out, in0, s1, s2, op0, op1), r, w)

    def stt(self, eng, out, in0, scalar, in1, op0, op1, r, w):
        self.P.op(eng, lambda e: e.scalar_tensor_tensor(out, in0, scalar, in1, op0, op1), r, w)

    def memset(self, eng, out, val, w):
        self.P.op(eng, lambda e: e.memset(out, val), (), w)

    def recip(self, out, in_, r, w):
        self.P.op("dve", lambda e: e.reciprocal(out, in_), r, w)

    def ld(self, out, in_, w, r=()):
        self.P.dma("sp", out, in_, r, w)

    def st(self, out, in_, r, w=()):
        self.P.dma("sp", out, in_, r, w)


def bcast_last(ap, n):
    shp = list(ap.shape)
    return ap.unsqueeze(len(shp)).to_broadcast(shp + [n])


def ml_phase_a(k, li, x_src, T):
    nc, P = k.nc, k.P
    ps, tps = k.ps, k.tps
    with ExitStack() as es:
        def A(name, shape, dt):
            return es.enter_context(nc.sbuf_tensor(f"mA{li}_{name}", shape, dt)).ap()
        w_in = A("win", [128, 8, 4096], BF16)
        t_win = [Tok() for _ in range(8)]
        bd = A("bd", [128, 48, 128], BF16)
        t_bd = Tok()
        vec = A("vec", [128, 16, 8], F32)
        t_vec = Tok()
        wif32 = A("wif32", [128, 48, 8], F32)
        wif = A("wif", [128, 48, 8], BF16)
        t_wif = Tok()
        bif = A("bif", [8, 1], F32)
        t_bif = Tok()
        gb = A("gb", [128, 1024], F32)
        t_gb = Tok()
        stg = Ring([A(f"stg{i}", [128, 2048], F32) for i in range(2)])
        xr_ = Ring([A(f"xt{i}", [128, 1024], F32) for i in range(2)])
        hb_ = Ring([A(f"hb{i}", [128, 1024], BF16) for i in range(2)])
        junk = A("junk", [128, 1024], BF16)
        t_junk = Tok()
        st_ = Ring([A(f"st{i}", [128, 4], F32) for i in range(2)])
        hT = A("hT", [128, 8, 512], BF16)
        t_hT = Tok()
        xin = A("xin", [128, 16, 515], BF16)
        t_xin = [Tok() for _ in range(16)]
        acc_ = Ring([A(f"acc{i}", [128, 512], F32) for i in range(3)])
        xc = A("xc", [128, 16, 512], BF16)
        t_xc = [Tok() for _ in range(16)]
        sx_ = Ring([A(f"sx{i}", [128, 512], BF16) for i in range(2)])
        sz_ = Ring([A(f"sz{i}", [128, 512], BF16) for i in range(2)])
        qkv_ = Ring([A(f"qkv{i}", [128, 512], BF16) for i in range(8)])
        tok_ = Ring([A(f"tokm{i}", [128, 2048], BF16) for i in range(3)])
        gsb = A("gsb", [8, 2, 512], F32)
        t_gsb = Tok()
        gt_ = Ring([A(f"gt{i}", [128, 16], F32) for i in range(2)])

        for kc in range(8):
            for hf in range(2):
                s_ap, s_t = stg.next()
                k.ld(s_ap, k.ml_w_in[li, kc * 128:(kc + 1) * 128, hf * 2048:(hf + 1) * 2048], w=[s_t])
                k.cp(("dve", "act")[hf], w_in[:, kc, hf * 2048:(hf + 1) * 2048], s_ap, r=[s_t], w=[t_win[kc]])
        for g3 in range(3):
            s_ap, s_t = stg.next()
            sv = s_ap.rearrange("p (c n) -> p c n", c=16)
            k.ld(sv, k.ml_bd[li, g3 * 16:(g3 + 1) * 16].rearrange("c p n -> p c n"), w=[s_t])
            k.cp("dve", bd[:, g3 * 16:(g3 + 1) * 16, :], sv, r=[s_t], w=[t_bd])
        k.ld(vec, k.ml_vec[li], w=[t_vec])
        k.ld(wif32, k.ml_wif[li], w=[t_wif])
        k.ts("dve", wif32[:, 0:16, :], wif32[:, 0:16, :], float(DH ** 0.5), None, ALU.mult, None, r=[t_wif], w=[t_wif])
        k.cp("dve", wif, wif32, r=[t_wif], w=[t_wif])
        k.ld(bif, k.ml_bif[li], w=[t_bif])
        k.ld(gb, k.ml_norm[li].partition_broadcast(128), w=[t_gb])

        for s in range(k.nseq):
            for st in range(4):
                t0 = st * 512
                for sub in range(4):
                    r0 = t0 + sub * 128
                    xt, t_xt = xr_.next()
                    k.ld(xt, x_src[s, r0:r0 + 128, :], w=[t_xt])
                    sv, t_sv = st_.next()
                    k.act(junk, xt, AF.Square, r=[t_xt], w=[t_junk, t_sv], accum=sv[:, 0:1])
                    k.act(sv[:, 1:2], sv[:, 0:1], AF.Ln, r=[t_sv], w=[t_sv], scale=1.0 / D, bias=k.c_eps[:, 0:1])
                    k.act(sv[:, 2:3], sv[:, 1:2], AF.Exp, r=[t_sv], w=[t_sv], scale=-0.5)
                    hb, t_hb = hb_.next()
                    k.stt("dve", hb, xt, sv[:, 2:3], gb, ALU.mult, ALU.mult, r=[t_xt, t_sv, t_gb], w=[t_hb])
                    pv = ps[5].bitcast(BF16).rearrange("p (a t) -> p a t", a=8)
                    for kc in range(8):
                        k.tr(pv[:, kc, :], hb[:, kc * 128:(kc + 1) * 128], k.ident_bf, r=[t_hb], w=[tps[5]])
                    k.cp("dve", hT[:, :, sub * 128:(sub + 1) * 128], pv, r=[tps[5]], w=[t_hT])
                if st == 0:
                    k.memset("dve", xin[:, :, 0:3], 0.0, w=t_xin)
                else:
                    k.cp("dve", xin[:, :, 0:3], xin[:, :, 512:515], r=t_xin, w=t_xin)
                def proj_x1(fc):
                    bi = fc % 2
                    for kc in range(8):
                        k.mm(ps[bi], w_in[:, kc, fc * 128:(fc + 1) * 128], hT[:, kc, :], kc == 0, kc == 7,
                             r=[t_win[kc], t_hT], w=[tps[bi]])
                    k.cp("act", xin[:, fc, 3:515], ps[bi], r=[tps[bi]], w=[t_xin[fc]])

                def proj_x2a(fc):
                    acc, t_acc = acc_.next()
                    k.act(acc, xin[:, fc, 0:512], AF.Copy, r=[t_xin[fc], t_vec], w=[t_acc], scale=vec[:, fc, 0:1])
                    for tap in range(1, 4):
                        k.stt("dve", acc, xin[:, fc, tap:tap + 512], vec[:, fc, tap:tap + 1], acc, ALU.mult, ALU.add,
                              r=[t_xin[fc], t_acc], w=[t_acc])
                    pend_acc[fc] = (acc, t_acc)

                def proj_x2b(fc):
                    acc, t_acc = pend_acc.pop(fc)
                    k.act(xc[:, fc, :], acc, AF.Silu, r=[t_acc], w=[t_xc[fc]], bias=vec[:, fc, 4:5])
                    sx, t_sx = sx_.next()
                    k.act(sx, xc[:, fc, :], AF.Copy, r=[t_xc[fc]], w=[t_sx], scale=vec[:, fc, 6:7])
                    k.st(T["sxT"][s, fc * 128:(fc + 1) * 128, t0:t0 + 512], sx, r=[t_sx])

                def bdmm(fc):
                    tiles = []
                    for which in range(3):
                        pb = (2, 3, 6)[which]
                        src = xc[:, fc, :] if which < 2 else xin[:, fc, 3:515]
                        tsrc = t_xc[fc] if which < 2 else t_xin[fc]
                        k.mm(ps[pb], bd[:, which * 16 + fc, :], src, True, True, r=[t_bd, tsrc], w=[tps[pb]])
                    for which in range(3):
                        pb = (2, 3, 6)[which]
                        qt, t_qt = qkv_.next()
                        if which == 0:
                            k.act(qt, ps[pb], AF.Copy, r=[tps[pb]], w=[t_qt], scale=float(DH ** -0.5))
                            k.st(T["qT"][s, fc * 128:(fc + 1) * 128, t0:t0 + 512], qt, r=[t_qt])
                        elif which == 1:
                            k.cp("dve", qt, ps[pb], r=[tps[pb]], w=[t_qt])
                            k.st(T["kT"][s, fc * 128:(fc + 1) * 128, t0:t0 + 512], qt, r=[t_qt])
                        else:
                            k.cp("dve", qt, ps[pb], r=[tps[pb]], w=[t_qt])
                        tiles.append((qt, t_qt))
                    pend_gates[fc] = tiles

                def gates(fc):
                    for which, (qt, t_qt) in enumerate(pend_gates.pop(fc)):
                        first = (fc == 0 and which == 0)
                        last = (fc == 15 and which == 2)
                        k.mm(ps[4][0:8, :], wif[:, which * 16 + fc, :], qt, first, last, r=[t_wif, t_qt], w=[tps[4]])

                def proj_z(fc):
                    bi = fc % 2
                    for kc in range(8):
                        k.mm(ps[bi], w_in[:, kc, fc * 128:(fc + 1) * 128], hT[:, kc, :], kc == 0, kc == 7,
                             r=[t_win[kc], t_hT], w=[tps[bi]])
                    sz, t_sz = sz_.next()
                    k.act(sz, ps[bi], AF.Silu, r=[tps[bi]], w=[t_sz])
                    k.st(T["szT"][s, (fc - 16) * 128:(fc - 15) * 128, t0:t0 + 512], sz, r=[t_sz])

                pend_gates = {}
                pend_acc = {}
                for fc in range(32):
                    if fc < 16:
                        proj_x1(fc)
                    else:
                        proj_z(fc)
                    if 3 <= fc < 19:
                        bdmm(fc - 3)
                    if 4 <= fc < 20:
                        gates(fc - 4)
                    if fc < 16:
                        proj_x2a(fc)
                    if 1 <= fc < 17:
                        proj_x2b(fc - 1)
                for which in (1, 2):
                    for sub in range(4):
                        tm, t_tm = tok_.next()
                        for fg in range(4):
                            pb = 6 + (fg % 2)
                            for f4 in range(4):
                                fc = fg * 4 + f4
                                if which == 1:
                                    lhsT = xc[:, fc, sub * 128:(sub + 1) * 128]
                                    tsrc = t_xc[fc]
                                else:
                                    lhsT = xin[:, fc, 3 + sub * 128:3 + (sub + 1) * 128]
                                    tsrc = t_xin[fc]
                                k.mm(ps[pb][:, f4 * 128:(f4 + 1) * 128], lhsT, bd[:, which * 16 + fc, :], True, True,
                                     r=[t_bd, tsrc], w=[tps[pb]])
                            k.cp(("act", "dve")[fg % 2], tm[:, fg * 512:(fg + 1) * 512], ps[pb], r=[tps[pb]], w=[t_tm])
                        dst = T["ktok"] if which == 1 else T["vtok"]
                        r0 = t0 + sub * 128
                        k.st(dst[s, r0:r0 + 128, :], tm, r=[t_tm])
                k.act(gsb[:, 0, :], ps[4][0:8, :], AF.Identity, r=[tps[4], t_bif], w=[t_gsb], bias=bif[:, 0:1])
                k.act(gsb[:, 1, :], gsb[:, 0, :], AF.Exp, r=[t_gsb], w=[t_gsb], scale=-1.0)
                k.act(gsb[:, 1, :], gsb[:, 1, :], AF.Ln, r=[t_gsb], w=[t_gsb], bias=k.c_one[0:8, 0:1])
                for sub in range(4):
                    for a in range(2):
                        k.tr(ps[5][:, a * 8:(a + 1) * 8], gsb[:, a, sub * 128:(sub + 1) * 128], k.ident_f[0:8, 0:8],
                             r=[t_gsb], w=[tps[5]])
                    gt, t_gt = gt_.next()
                    k.cp("dve", gt, ps[5][:, 0:16], r=[tps[5]], w=[t_gt])
                    r0 = t0 + sub * 128
                    k.st(T["gtok"][s, r0:r0 + 128, :], gt, r=[t_gt])
    P.barrier()


def ml_phase_b(k, li, x_src, x_dst, T):
    nc, P = k.nc, k.P
    ps, tps = k.ps, k.tps
    with ExitStack() as es:
        def A(name, shape, dt):
            return es.enter_context(nc.sbuf_tensor(f"mB{li}_{name}", shape, dt)).ap()
        w_out = A("wout", [128, 16, 1024], BF16)
        t_wout = Tok()
        vec = A("vec", [128, 16, 8], F32)
        t_vec = Tok()
        stg = Ring([A(f"stg{i}", [128, 2048], F32) for i in range(1)])
        X = A("X", [128, 16, 513], F32)
        t_X = [Tok() for _ in range(4)]
        STb = A("STb", [128, 16, 513], BF16)
        t_STb = [Tok() for _ in range(4)]
        qT_ = Ring([A(f"qT{i}", [128, 16, 128], BF16) for i in range(2)])
        kT_ = Ring([A(f"kT{i}", [128, 16, 128], BF16) for i in range(2)])
        kt_ = Ring([A(f"kt{i}", [128, 2048], BF16) for i in range(2)])
        vt_ = Ring([A(f"vt{i}", [128, 2048], BF16) for i in range(2)])
        gt_ = Ring([A(f"gt{i}", [128, 16], F32) for i in range(2)])
        xr_ = Ring([A(f"xr{i}", [128, 1024], F32) for i in range(3)])
        sx_ = Ring([A(f"sx{i}", [128, 16, 128], BF16) for i in range(3)])
        sz_ = Ring([A(f"sz{i}", [128, 16, 128], BF16) for i in range(3)])
        ve_ = Ring([A(f"ve{i}", [128, 4, 520], BF16) for i in range(2)])
        PT_ = Ring([A(f"PT{i}", [128, 128], BF16) for i in range(4)])
        gm_ = Ring([A(f"gm{i}", [128, 4, 4], F32) for i in range(3)])
        sm_ = Ring([A(f"sm{i}", [128, 8, 4], F32) for i in range(2)])
        mv_ = Ring([A(f"mv{i}", [128, 4, 2], F32) for i in range(2)])
        bs_ = Ring([A(f"bs{i}", [128, 6], F32) for i in range(4)])
        hn_ = Ring([A(f"hn{i}", [128, 2048], BF16) for i in range(2)])
        t1 = A("t1", [128, 16, 128], F32)
        t_t1 = Tok()
        gT_ = Ring([A(f"gT{i}", [128, 16, 128], BF16) for i in range(2)])
        yo_ = Ring([A(f"yo{i}", [128, 1024], F32) for i in range(2)])

        for fc in range(0, 16, 2):
            s_ap, s_t = stg.next()
            sv = s_ap.rearrange("p (c n) -> p c n", c=2)
            k.ld(sv, k.ml_w_out[li, fc * 128:(fc + 2) * 128, :].rearrange("(c p) n -> p c n", p=128), w=[s_t])
            k.cp("dve", w_out[:, fc:fc + 2, :], sv, r=[s_t], w=[t_wout])
        k.ld(vec, k.ml_vec[li], w=[t_vec])

        def issue_loads(s, c):
            r0 = c * 128
            d = {}
            for nm, ring in (("gt", gt_), ("kT", kT_), ("qT", qT_), ("vt", vt_), ("kt", kt_), ("sx", sx_), ("sz", sz_),
                             ("xr", xr_)):
                d[nm] = ring.next()
            k.ld(d["gt"][0], T["gtok"][s, r0:r0 + 128, :], w=[d["gt"][1]])
            k.ld(d["kT"][0], T["kT"][s, :, r0:r0 + 128].rearrange("(a p) t -> p a t", p=128), w=[d["kT"][1]])
            k.ld(d["qT"][0], T["qT"][s, :, r0:r0 + 128].rearrange("(a p) t -> p a t", p=128), w=[d["qT"][1]])
            k.ld(d["vt"][0], T["vtok"][s, r0:r0 + 128, :], w=[d["vt"][1]])
            k.ld(d["kt"][0], T["ktok"][s, r0:r0 + 128, :], w=[d["kt"][1]])
            k.ld(d["sx"][0], T["sxT"][s, :, r0:r0 + 128].rearrange("(a p) t -> p a t", p=128), w=[d["sx"][1]])
            k.ld(d["sz"][0], T["szT"][s, :, r0:r0 + 128].rearrange("(a p) t -> p a t", p=128), w=[d["sz"][1]])
            k.ld(d["xr"][0], x_src[s, r0:r0 + 128, :], w=[d["xr"][1]])
            return d

        def epilogue(pe_, fillers=()):
            fillers = list(fillers)
            hn, t_hn, sx, t_sx, sz, t_sz, xr, t_xr, s, r0 = pe_
            for half in range(2):
                pb = 6 + half
                pv = ps[pb].bitcast(BF16).rearrange("p (a t) -> p a t", a=8)
                for a in range(8):
                    fc = half * 8 + a
                    k.tr(pv[:, a, :], hn[:, fc * 128:(fc + 1) * 128], k.ident_bf, r=[t_hn], w=[tps[pb]])
                k.tt("dve", t1[:, half * 8:(half + 1) * 8, :], pv, bcast_last(vec[:, half * 8:(half + 1) * 8, 5], 128),
                     ALU.mult, r=[tps[pb], t_vec], w=[t_t1])
            gT, t_gT = gT_.next()
            k.tt("dve", t1, t1, sx, ALU.add, r=[t_sx, t_t1], w=[t_t1])
            k.tt("dve", gT, t1, sz, ALU.mult, r=[t_sz, t_t1], w=[t_gT])
            yo, t_yo = yo_.next()
            for half in range(2):
                pb = 6 + half
                for fc in range(16):
                    k.mm(ps[pb], gT[:, fc, :], w_out[:, fc, half * 512:(half + 1) * 512], fc == 0, fc == 15,
                         r=[t_gT, t_wout], w=[tps[pb]])
                    if fc % 2 == 1 and fillers:
                        fillers.pop(0)()
                k.tt("dve", yo[:, half * 512:(half + 1) * 512], ps[pb], xr[:, half * 512:(half + 1) * 512], ALU.add,
                     r=[tps[pb], t_xr], w=[t_yo])
            k.st(x_dst[s, r0:r0 + 128, :], yo, r=[t_yo])
            for f in fillers:
                f()


        work = [(s, c) for s in range(k.nseq) for c in range(NT)]
        nxt = issue_loads(*work[0])
        prev_gm = None
        pend_epi = None
        for wi, (s, c) in enumerate(work):
            r0 = c * 128
            L = nxt
            if wi + 1 < len(work):
                nxt = issue_loads(*work[wi + 1])
            (gt, t_gt), (kT, t_kT), (qT, t_qT), (vt, t_vt) = L["gt"], L["kT"], L["qT"], L["vt"]
            (kt, t_kt), (sx, t_sx), (sz, t_sz), (xr, t_xr) = L["kt"], L["sx"], L["sz"], L["xr"]
            if c == 0:
                prev_gm = None
            gm, t_gm = gm_.next()
            k.mm(ps[0][:, 0:4], k.caus_f, gt[:, 12:16], True, True, r=[t_gt], w=[tps[0]])
            k.mm(ps[0][:, 4:8], k.ones_f, gt[:, 12:16], True, True, r=[t_gt], w=[tps[0]])
            k.tt("dve", gm[:, 3, :], ps[0][:, 0:4], gt[:, 0:4], ALU.add, r=[tps[0], t_gt], w=[t_gm])
            k.act(gm[:, 0, :], gm[:, 3, :], AF.Exp, r=[t_gm], w=[t_gm])
            k.act(gm[:, 1:3, :], ps[0][:, 0:8].rearrange("p (a b) -> p a b", a=2), AF.Exp, r=[tps[0]], w=[t_gm],
                  scale=-1.0)
            ve, t_ve = ve_.next()
            for h in range(NH):
                k.act(ve[:, h, 0:512], vt[:, h * 512:(h + 1) * 512], AF.Copy, r=[t_vt, t_gm], w=[t_ve],
                      scale=gm[:, 0, h:h + 1])
            k.cp("dve", ve[:, :, 512], gm[:, 0, :], r=[t_gm], w=[t_ve])
            for h in range(NH):
                for dc in range(4):
                    k.mm(ps[1][:, 0:128], kT[:, h * 4 + dc, :], qT[:, h * 4 + dc, :], dc == 0, dc == 3,
                         r=[t_kT, t_qT], w=[tps[1]])
                PT, t_PT = PT_.next()
                k.tt("dve", PT, ps[1][:, 0:128], k.caus_f, ALU.mult, r=[tps[1]], w=[t_PT])
                nb = 2 + h
                k.mm(ps[nb], PT, ve[:, h, 0:512], True, c == 0, r=[t_PT, t_ve], w=[tps[nb]])
                if c > 0:
                    for dc in range(4):
                        k.mm(ps[nb], qT[:, h * 4 + dc, :], STb[:, h * 4 + dc, 0:512], False, dc == 3,
                             r=[t_qT, t_STb[h]], w=[tps[nb]])
                k.mm(ps[0][:, 8 + h:9 + h], PT, ve[:, h, 512:513], True, c == 0, r=[t_PT, t_ve], w=[tps[0]])
                if c > 0:
                    for dc in range(4):
                        k.mm(ps[0][:, 8 + h:9 + h], qT[:, h * 4 + dc, :], STb[:, h * 4 + dc, 512:513], False, dc == 3,
                             r=[t_qT, t_STb[h]], w=[tps[0]])
            def mk_filler(h, dc, kt, t_kt, ve, t_ve, gm, t_gm, pg, c):
                def f():
                    ub = 1
                    lhsT = kt[:, h * 512 + dc * 128:h * 512 + (dc + 1) * 128]
                    k.mm(ps[ub], lhsT, ve[:, h, 0:512], True, True, r=[t_kt, t_ve], w=[tps[ub]])
                    k.mm(ps[0][:, 16 + h * 4 + dc:17 + h * 4 + dc], lhsT, ve[:, h, 512:513], True, True,
                         r=[t_kt, t_ve], w=[tps[0]])
                    if c == 0:
                        k.cp("dve", X[:, h * 4 + dc, 0:512], ps[ub], r=[tps[ub]], w=[t_X[h]])
                    else:
                        k.stt("dve", X[:, h * 4 + dc, 0:512], X[:, h * 4 + dc, 0:512], pg[0][:, 2, h:h + 1], ps[ub],
                              ALU.mult, ALU.add, r=[tps[ub], pg[1], t_X[h]], w=[t_X[h]])
                    if c < NT - 1:
                        k.act(STb[:, h * 4 + dc, 0:512], X[:, h * 4 + dc, 0:512], AF.Copy, r=[t_X[h], t_gm], w=[t_STb[h]],
                              scale=gm[:, 2, h:h + 1])
                    if dc == 3:
                        xn = X[:, h * 4:(h + 1) * 4, 512:513].rearrange("p a b -> p (a b)")
                        if c == 0:
                            k.cp("dve", xn, ps[0][:, 16 + h * 4:20 + h * 4], r=[tps[0]], w=[t_X[h]])
                        else:
                            k.stt("dve", xn, xn, pg[0][:, 2, h:h + 1], ps[0][:, 16 + h * 4:20 + h * 4],
                                  ALU.mult, ALU.add, r=[tps[0], pg[1], t_X[h]], w=[t_X[h]])
                        if c < NT - 1:
                            sn = STb[:, h * 4:(h + 1) * 4, 512:513].rearrange("p a b -> p (a b)")
                            k.act(sn, xn, AF.Copy, r=[t_X[h], t_gm], w=[t_STb[h]], scale=gm[:, 2, h:h + 1])
                return f

            fillers = [mk_filler(h, dc, kt, t_kt, ve, t_ve, gm, t_gm, prev_gm, c) for h in range(NH) for dc in range(4)]
            if pend_epi is not None:
                epilogue(pend_epi, fillers)
                pend_epi = None
            else:
                for f in fillers:
                    f()
            prev_gm = (gm, t_gm)
            sm, t_sm = sm_.next()
            mv, t_mv = mv_.next()
            for h in range(NH):
                bs, t_bs = bs_.next()
                k.P.op("dve", (lambda o, i: (lambda e: e.bn_stats(o, i)))(bs, ps[2 + h]), r=[tps[2 + h]], w=[t_bs])
                k.P.op("dve", (lambda o, i: (lambda e: e.bn_aggr(o, i)))(mv[:, h, :], bs), r=[t_bs], w=[t_mv])
            k.tt("dve", sm[:, 0, :], ps[0][:, 8:12], gm[:, 1, :], ALU.mult, r=[tps[0], t_gm], w=[t_sm])
            k.stt("dve", sm[:, 1, :], sm[:, 0, :], -1.0, sm[:, 0, :], ALU.mult, ALU.max, r=[t_sm], w=[t_sm])
            k.ts("dve", sm[:, 1, :], sm[:, 1, :], 1.0, None, ALU.max, None, r=[t_sm], w=[t_sm])
            k.recip(sm[:, 2, :], sm[:, 1, :], r=[t_sm], w=[t_sm])
            k.tt("dve", sm[:, 3, :], gm[:, 1, :], sm[:, 2, :], ALU.mult, r=[t_sm, t_gm], w=[t_sm])
            k.tt("dve", sm[:, 4, :], sm[:, 3, :], sm[:, 3, :], ALU.mult, r=[t_sm], w=[t_sm])
            k.tt("dve", sm[:, 4, :], sm[:, 4, :], mv[:, :, 1], ALU.mult, r=[t_sm, t_mv], w=[t_sm])
            k.act(sm[:, 5, :], sm[:, 4, :], AF.Ln, r=[t_sm], w=[t_sm], bias=k.c_eps[:, 0:1])
            k.act(sm[:, 5, :], sm[:, 5, :], AF.Exp, r=[t_sm], w=[t_sm], scale=-0.5)
            k.tt("dve", sm[:, 6, :], sm[:, 5, :], sm[:, 3, :], ALU.mult, r=[t_sm], w=[t_sm])
            k.stt("dve", sm[:, 7, :], mv[:, :, 0], -1.0, sm[:, 6, :], ALU.mult, ALU.mult, r=[t_sm, t_mv], w=[t_sm])
            hn, t_hn = hn_.next()
            for h in range(NH):
                k.act(hn[:, h * 512:(h + 1) * 512], ps[2 + h], AF.Identity, r=[tps[2 + h], t_sm], w=[t_hn],
                      scale=sm[:, 6, h:h + 1], bias=sm[:, 7, h:h + 1])
            pend_epi = (hn, t_hn, sx, t_sx, sz, t_sz, xr, t_xr, s, r0)
        epilogue(pend_epi)
    P.barrier()


NSA_FM = [(0, 1024), (1024, 1280), (1280, 1536), (1536, 1792), (2048, 2304)]
SCALE = 0.125


def nsa_phase_a(k, li, x_src, T):
    nc, P = k.nc, k.P
    ps, tps = k.ps, k.tps
    with ExitStack() as es:
        def A(name, shape, dt):
            return es.enter_context(nc.sbuf_tensor(f"nA{li}_{name}", shape, dt)).ap()
        w_in = A("win", [128, 8, 3648], BF16)
        t_win = [Tok() for _ in range(8)]
        gb = A("gb", [128, 1024], F32)
        t_gb = Tok()
        bg = A("bg", [128, 48], F32)
        t_bg = Tok()
        rot = A("rot", [128, 128], BF16)
        cosf = A("cosf", [128, S], F32)
        sinf = A("sinf", [128, S], F32)
        t_c = Tok()
        stg = Ring([A(f"stg{i}", [128, 1816], F32) for i in range(2)])
        xr_ = Ring([A(f"xt{i}", [128, 1024], F32) for i in range(2)])
        hb_ = Ring([A(f"hb{i}", [128, 1024], BF16) for i in range(2)])
        junk = A("junk", [128, 1024], BF16)
        t_junk = Tok()
        st_ = Ring([A(f"st{i}", [128, 4], F32) for i in range(2)])
        hT = A("hT", [128, 8, 512], BF16)
        t_hT = Tok()
        xb_ = Ring([A(f"xb{i}", [128, 512], BF16) for i in range(3)])
        ta_ = Ring([A(f"ta{i}", [128, 512], F32) for i in range(2)])
        tb_ = Ring([A(f"tb{i}", [128, 512], F32) for i in range(2)])
        ro_ = Ring([A(f"ro{i}", [128, 512], BF16) for i in range(3)])
        tm_ = Ring([A(f"tm{i}", [128, 1536], BF16) for i in range(2)])
        gl_ = Ring([A(f"gl{i}", [128, 48], F32) for i in range(2)])

        for kc in range(8):
            for hf in range(2):
                s_ap, s_t = stg.next()
                k.ld(s_ap, k.nsa_w_in[li, kc * 128:(kc + 1) * 128, hf * 1816:(hf + 1) * 1816], w=[s_t])
                k.cp(("dve", "act")[hf], w_in[:, kc, hf * 1816:(hf + 1) * 1816], s_ap, r=[s_t], w=[t_win[kc]])
        k.ld(gb, k.nsa_norm[li].partition_broadcast(128), w=[t_gb])
        k.ld(bg, k.nsa_b_gate[li].partition_broadcast(128), w=[t_bg])
        s_ap, s_t = stg.next()
        k.ld(s_ap[:, 0:128], k.c_rot, w=[s_t])
        k.cp("dve", rot, s_ap[:, 0:128], r=[s_t], w=[t_c])
        k.ld(cosf, k.c_cos, w=[t_c])
        k.ld(sinf, k.c_sin, w=[t_c])

        fm = []
        for c8 in range(8):
            fm.append((c8 * 128, "qT", c8 * 128, True))
        for c2 in range(2):
            fm.append((1024 + c2 * 128, "kcT", c2 * 128, False))
            fm.append((1280 + c2 * 128, "vcT", c2 * 128, False))
            fm.append((1536 + c2 * 128, "ksT", c2 * 128, True))
            fm.append((2048 + c2 * 128, "kwT", c2 * 128, True))

        for s in range(k.nseq):
            for st in range(4):
                t0 = st * 512
                for sub in range(4):
                    r0 = t0 + sub * 128
                    xt, t_xt = xr_.next()
                    k.ld(xt, x_src[s, r0:r0 + 128, :], w=[t_xt])
                    sv, t_sv = st_.next()
                    k.act(junk, xt, AF.Square, r=[t_xt], w=[t_junk, t_sv], accum=sv[:, 0:1])
                    k.act(sv[:, 1:2], sv[:, 0:1], AF.Ln, r=[t_sv], w=[t_sv], scale=1.0 / D, bias=k.c_eps[:, 0:1])
                    k.act(sv[:, 2:3], sv[:, 1:2], AF.Exp, r=[t_sv], w=[t_sv], scale=-0.5)
                    hb, t_hb = hb_.next()
                    k.stt("dve", hb, xt, sv[:, 2:3], gb, ALU.mult, ALU.mult, r=[t_xt, t_sv, t_gb], w=[t_hb])
                    pv = ps[5].bitcast(BF16).rearrange("p (a t) -> p a t", a=8)
                    for kc in range(8):
                        k.tr(pv[:, kc, :], hb[:, kc * 128:(kc + 1) * 128], k.ident_bf, r=[t_hb], w=[tps[5]])
                    k.cp("dve", hT[:, :, sub * 128:(sub + 1) * 128], pv, r=[tps[5]], w=[t_hT])
                for i, (c0, dst, d0, rope) in enumerate(fm):
                    bi = i % 2
                    for kc in range(8):
                        k.mm(ps[bi], w_in[:, kc, c0:c0 + 128], hT[:, kc, :], kc == 0, kc == 7,
                             r=[t_win[kc], t_hT], w=[tps[bi]])
                    xb, t_xb = xb_.next()
                    k.cp("act", xb, ps[bi], r=[tps[bi]], w=[t_xb])
                    if dst == "qT" or not rope:
                        k.st(T[dst][s, d0:d0 + 128, t0:t0 + 512], xb, r=[t_xb])
                    if rope:
                        pb = 2 + (i % 2)
                        k.mm(ps[pb], rot, xb, True, True, r=[t_c, t_xb], w=[tps[pb]])
                        ta, t_ta = ta_.next()
                        tb, t_tb = tb_.next()
                        k.tt("dve", ta, ps[bi], cosf[:, t0:t0 + 512], ALU.mult, r=[tps[bi], t_c, t_xb], w=[t_ta])
                        k.tt("dve", tb, ps[pb], sinf[:, t0:t0 + 512], ALU.mult, r=[tps[pb], t_c], w=[t_tb])
                        ro, t_ro = ro_.next()
                        k.tt("dve", ro, ta, tb, ALU.add, r=[t_ta, t_tb], w=[t_ro])
                        rd = "qrT" if dst == "qT" else dst
                        k.st(T[rd][s, d0:d0 + 128, t0:t0 + 512], ro, r=[t_ro])
                for sub in range(4):
                    r0 = t0 + sub * 128
                    tm, t_tm = tm_.next()
                    lh = [hT[:, kc, sub * 128:(sub + 1) * 128] for kc in range(8)]
                    for half, c0 in enumerate((1792, 2304)):
                        for kc in range(8):
                            k.mm(ps[6][:, half * 256:(half + 1) * 256], lh[kc], w_in[:, kc, c0:c0 + 256], kc == 0, kc == 7,
                                 r=[t_win[kc], t_hT], w=[tps[6]])
                    k.cp("dve", tm[:, 0:512], ps[6], r=[tps[6]], w=[t_tm])
                    for zi in range(2):
                        pb = 7 if zi == 0 else 6
                        c0 = 2560 + zi * 512
                        for kc in range(8):
                            k.mm(ps[pb], lh[kc], w_in[:, kc, c0:c0 + 512], kc == 0, kc == 7,
                                 r=[t_win[kc], t_hT], w=[tps[pb]])
                        k.act(tm[:, 512 + zi * 512:1024 + zi * 512], ps[pb], AF.Silu, r=[tps[pb]], w=[t_tm])
                    for kc in range(8):
                        k.mm(ps[7][:, 0:48], lh[kc], w_in[:, kc, 3584:3632], kc == 0, kc == 7,
                             r=[t_win[kc], t_hT], w=[tps[7]])
                    gl, t_gl = gl_.next()
                    k.tt("dve", gl, ps[7][:, 0:48], bg, ALU.add, r=[tps[7], t_bg], w=[t_gl])
                    k.act(gl, gl, AF.Sigmoid, r=[t_gl], w=[t_gl])
                    k.st(T["vsw"][s, r0:r0 + 128, :], tm[:, 0:512], r=[t_tm])
                    k.st(T["szt"][s, r0:r0 + 128, :], tm[:, 512:1536], r=[t_tm])
                    k.st(T["gate"][s, r0:r0 + 128, :], gl, r=[t_gl])
    P.barrier()


def nsa_phase_c(k, li, x_src, x_dst, T):
    nc, P = k.nc, k.P
    ps, tps = k.ps, k.tps
    with ExitStack() as es0:
        def A0(name, shape, dt):
            return es0.enter_context(nc.sbuf_tensor(f"nC{li}_{name}", shape, dt)).ap()
        w_out = A0("wout", [128, 8, 1024], BF16)
        t_wout = Tok()
        kcmpT = A0("kcmpT", [128, k.nseq, 4, 128], BF16)
        vcmp = A0("vcmp", [128, k.nseq, 4, 128], BF16)
        t_cmp = Tok()
        with ExitStack() as es:
            def A(name, shape, dt):
                return es.enter_context(nc.sbuf_tensor(f"nB{li}_{name}", shape, dt)).ap()
            stg = Ring([A(f"stg{i}", [128, 2048], F32) for i in range(2)])
            w1 = A("w1", [128, 2, 32, 128], BF16)
            w2k = A("w2k", [128, 128], BF16)
            w2v = A("w2v", [128, 64], BF16)
            peT = A("peT", [128, 2, 32], BF16)
            bh = A("bh", [128, 2], F32)
            t_w = Tok()
            kc2 = A("kc2", [128, 2, S], BF16)
            vc2 = A("vc2", [128, 2, S], BF16)
            t_kv = Tok()
            hTs_ = Ring([A(f"hTs{i}", [128, 128], BF16) for i in range(2)])
            for c8 in range(0, 8, 2):
                s_ap, s_t = stg.next()
                sv = s_ap.rearrange("p (c n) -> p c n", c=2)
                k.ld(sv, k.nsa_w_out[li, c8 * 128:(c8 + 2) * 128, :].rearrange("(c p) n -> p c n", p=128), w=[s_t])
                k.cp("dve", w_out[:, c8:c8 + 2, :], sv, r=[s_t], w=[t_wout])
            for kv in range(2):
                for hf in range(2):
                    s_ap, s_t = stg.next()
                    sv = s_ap.rearrange("p (l n) -> p l n", l=16)
                    src = k.nsa_cmp_w1[li, kv, hf * 1024:(hf + 1) * 1024, :].rearrange("(l d) n -> d l n", d=64)
                    k.ld(sv[0:64], src, w=[s_t])
                    k.ld(sv[64:128], src, w=[s_t])
                    k.cp("dve", w1[:, kv, hf * 16:(hf + 1) * 16, :], sv, r=[s_t], w=[t_w])
            s_ap, s_t = stg.next()
            k.ld(s_ap[:, 0:64], k.nsa_cmp_w2[li, 0], w=[s_t])
            k.ld(s_ap[:, 64:128], k.nsa_cmp_w2[li, 1], w=[s_t])
            pe_v = s_ap[:, 128:192].rearrange("p (a l) -> p a l", a=2)
            k.ld(pe_v[0:64], k.nsa_peT[li].rearrange("a d l -> d a l"), w=[s_t])
            k.ld(pe_v[64:128], k.nsa_peT[li].rearrange("a d l -> d a l"), w=[s_t])
            k.cp("dve", w2k[:, 0:64], s_ap[:, 0:64], r=[s_t], w=[t_w])
            k.cp("dve", w2k[:, 64:128], s_ap[:, 0:64], r=[s_t], w=[t_w])
            k.cp("dve", w2v, s_ap[:, 64:128], r=[s_t], w=[t_w])
            k.cp("dve", peT, pe_v, r=[s_t], w=[t_w])
            for kv in range(2):
                for l in range(32):
                    k.mm(ps[7][:, kv:kv + 1], w1[0:64, kv, l, :], peT[0:64, kv, l:l + 1], l == 0, l == 31, r=[t_w], w=[tps[7]])
            k.cp("dve", bh, ps[7][:, 0:2], r=[tps[7]], w=[t_w])
            for s in range(k.nseq):
                k.ld(vcmp[:, s, :, 64:97], k.c_ovl.rearrange("p (g n) -> p g n", g=4), w=[t_cmp])
                k.ld(kc2, T["kcT"][s].rearrange("(c p) t -> p c t", p=128), w=[t_kv])
                k.ld(vc2, T["vcT"][s].rearrange("(c p) t -> p c t", p=128), w=[t_kv])
                for kv in range(2):
                    src = kc2 if kv == 0 else vc2
                    for g in range(4):
                        base = (g % 2) * 64
                        for l in range(32):
                            k.mm(ps[6][:, 0:127], w1[base:base + 64, kv, l, :], src[base:base + 64, g // 2, l:l + 16 * 126 + 1:16],
                                 l == 0, l == 31, r=[t_w, t_kv], w=[tps[6]])
                        hTs, t_hTs = hTs_.next()
                        k.act(hTs[:, 0:127], ps[6][:, 0:127], AF.Silu, r=[tps[6], t_w], w=[t_hTs], bias=bh[:, kv:kv + 1])
                        if kv == 0:
                            k.mm(ps[7][:, 0:127], w2k, hTs[:, 0:127], True, True, r=[t_w, t_hTs], w=[tps[7]])
                            k.cp("dve", kcmpT[:, s, g, 0:127], ps[7][:, 0:127], r=[tps[7]], w=[t_cmp])
                        else:
                            k.mm(ps[7][0:127, 0:64], hTs[:, 0:127], w2v, True, True, r=[t_w, t_hTs], w=[tps[7]])
                            k.cp("dve", vcmp[0:127, s, g, 0:64], ps[7][0:127, 0:64], r=[tps[7]], w=[t_cmp])
        P.barrier()
        with ExitStack() as es:
            def A(name, shape, dt):
                return es.enter_context(nc.sbuf_tensor(f"nC{li}_{name}", shape, dt)).ap()
            t_c = Tok()
            Ef = A("Ef", [128, 4, S], BF16)
            cmpn = A("cmpn", [128, S], BF16)
            fmul = A("fmul", [128, 16, 32], F32)
            fadd = A("fadd", [128, 16, 32], F32)
            ks2 = A("ks2", [128, 4, S], BF16)
            kw2 = A("kw2", [128, 4, S], BF16)
            vs = A("vs", [128, 16, 4, 65], BF16)
            vw = A("vw", [128, 16, 4, 65], BF16)
            t_res = Tok()
            q_ = Ring([A(f"q{i}", [128, 8, 512], BF16) for i in range(1)])
            qr_ = Ring([A(f"qr{i}", [128, 8, 512], BF16) for i in range(1)])
            ex_ = Ring([A(f"ex{i}", [128, 512], BF16) for i in range(18)])
            oacc = A("oacc", [128, 4, 16, 64], F32)
            t_oacc = [Tok() for _ in range(4)]
            ucmp = A("ucmp", [128, 4, 4, 33], F32)
            rdc = A("rdc", [128, 4, 4], F32)
            t_ucmp = Tok()
            tmp_ = Ring([A(f"tmp{i}", [128, 4, 64], F32) for i in range(3)])
            imp_ = Ring([A(f"imp{i}", [128, 32], F32) for i in range(2)])
            rk_ = Ring([A(f"rk{i}", [128, 32, 32], F32) for i in range(2)])
            rs_ = Ring([A(f"rs{i}", [128, 32], F32) for i in range(2)])
            rd_ = Ring([A(f"rd{i}", [128, 16], F32) for i in range(4)])
            negm = A("negm", [128, 4, 4, 32], BF16)
            t_negm = [Tok() for _ in range(4)]
            negT = A("negT", [128, 512], BF16)
            t_negT = Tok()
            gat_ = Ring([A(f"gat{i}", [128, 4, 48], F32) for i in range(2)])
            szt_ = Ring([A(f"szt{i}", [128, 1024], BF16) for i in range(2)])
            xr_ = Ring([A(f"xr{i}", [128, 1024], F32) for i in range(2)])
            og_ = Ring([A(f"og{i}", [128, 1024], BF16) for i in range(2)])
            ogT_ = Ring([A(f"ogT{i}", [128, 8, 128], BF16) for i in range(2)])
            yo_ = Ring([A(f"yo{i}", [128, 1024], F32) for i in range(2)])

            k.ld(Ef, k.c_E.rearrange("p (g n) -> p g n", g=4), w=[t_c])
            k.ld(cmpn, k.c_cmpn, w=[t_c])
            k.ld(fmul, k.c_fmul, w=[t_c])
            k.ld(fadd, k.c_fadd, w=[t_c])
            k.memset("pool", vs[:, :, :, 64:65], 1.0, w=[t_res])
            k.memset("pool", vw[:, :, :, 64:65], 1.0, w=[t_res])

            def evac(pov, h, b, first, gat, t_gat, clamp, g):
                rd, t_rd = rd_.next()
                if clamp:
                    k.ts("dve", rd[:, 0:4], pov[:, :, 64], 1e-30, None, ALU.max, None, r=[tps_of[0]], w=[t_rd])
                else:
                    k.cp("dve", rd[:, 0:4], pov[:, :, 64], r=[tps_of[0]], w=[t_rd])
                k.recip(rd[:, 4:8], rd[:, 0:4], r=[t_rd], w=[t_rd])
                k.tt("dve", rd[:, 8:12], rd[:, 4:8], gat[:, :, 3 * h + b], ALU.mult, r=[t_rd, t_gat], w=[t_rd])
                if first:
                    k.tt("dve", oacc[:, :, h, :], pov[:, :, 0:64], bcast_last(rd[:, 8:12], 64), ALU.mult,
                         r=[tps_of[0], t_rd], w=[t_oacc[g]])
                else:
                    for sub in range(4):
                        k.stt("dve", oacc[:, sub, h, :], pov[:, sub, 0:64], rd[:, 8 + sub:9 + sub], oacc[:, sub, h, :],
                              ALU.mult, ALU.add, r=[tps_of[0], t_rd, t_oacc[g]], w=[t_oacc[g]])
                return rd, t_rd

            tps_of = [None]
            for s in range(k.nseq):
                for hf in range(2):
                    k.ld(ks2[hf * 64:(hf + 1) * 64], T["ksT"][s].rearrange("(g d) t -> d g t", d=64), w=[t_res])
                    k.ld(kw2[hf * 64:(hf + 1) * 64], T["kwT"][s].rearrange("(g d) t -> d g t", d=64), w=[t_res])
                vsw_v = T["vsw"][s].rearrange("(kt p) (b g d) -> p kt b g d", p=128, b=2, g=4)
                for kt4 in range(0, 16, 4):
                    for g in range(4):
                        k.ld(vs[:, kt4:kt4 + 4, g, 0:64], vsw_v[:, kt4:kt4 + 4, 0, g], w=[t_res])
                        k.ld(vw[:, kt4:kt4 + 4, g, 0:64], vsw_v[:, kt4:kt4 + 4, 1, g], w=[t_res])
                for qt in range(4):
                    t0 = qt * 512
                    q, t_q = q_.next()
                    qr, t_qr = qr_.next()
                    gat, t_gat = gat_.next()
                    k.ld(q, T["qT"][s, :, t0:t0 + 512].rearrange("(c p) t -> p c t", p=128), w=[t_q])
                    k.ld(qr, T["qrT"][s, :, t0:t0 + 512].rearrange("(c p) t -> p c t", p=128), w=[t_qr])
                    k.ld(gat, T["gate"][s, t0:t0 + 512, :].rearrange("(a p) n -> p a n", p=128), w=[t_gat])
                    need_sel = qt >= 2
                    for g in range(4):
                        for hh in range(4):
                            h = 4 * g + hh
                            ch, base = h // 2, (h % 2) * 64
                            sb = h % 2
                            k.mm(ps[sb][0:127, :], kcmpT[base:base + 64, s, g, 0:127], q[base:base + 64, ch, :], True, False,
                                 r=[t_cmp, t_q], w=[tps[sb]])
                            k.mm(ps[sb][0:127, :], k.ident_bf[0:127, 0:127], cmpn[0:127, t0:t0 + 512], False, True,
                                 r=[t_c], w=[tps[sb]])
                            ex, t_ex = ex_.next()
                            k.act(ex[0:127, :], ps[sb][0:127, :], AF.Exp, r=[tps[sb]], w=[t_ex], scale=SCALE)
                            ob = 2 + (h % 2)
                            pov = ps[ob][:, 0:388].rearrange("p (a n) -> p a n", a=4)
                            for sub in range(4):
                                k.mm(pov[:, sub, :], ex[0:127, sub * 128:(sub + 1) * 128], vcmp[0:127, s, g, 0:97], True, True,
                                     r=[t_ex, t_cmp], w=[tps[ob]])
                            tps_of[0] = tps[ob]
                            rd, t_rd = evac(pov, h, 0, True, gat, t_gat, True, g)
                            if need_sel:
                                k.cp("dve", ucmp[:, :, hh, :], pov[:, :, 64:97], r=[tps[ob]], w=[t_ucmp])
                                k.cp("dve", rdc[:, :, hh], rd[:, 4:8], r=[t_rd], w=[t_ucmp])
                        if need_sel:
                            for sub in range(4):
                                imp, t_imp = imp_.next()
                                k.ts("dve", imp, ucmp[:, sub, 0, 1:33], rdc[:, sub, 0:1], None, ALU.mult, None,
                                     r=[t_ucmp], w=[t_imp])
                                for hh in range(1, 4):
                                    k.stt("dve", imp, ucmp[:, sub, hh, 1:33], rdc[:, sub, hh:hh + 1], imp, ALU.mult, ALU.add,
                                          r=[t_ucmp, t_imp], w=[t_imp])
                                tt_ = 4 * qt + sub
                                k.tt("dve", imp, imp, fmul[:, tt_, :], ALU.mult, r=[t_imp, t_c], w=[t_imp])
                                k.tt("dve", imp, imp, fadd[:, tt_, :], ALU.add, r=[t_imp], w=[t_imp])
                                rk, t_rk = rk_.next()
                                in0 = imp.unsqueeze(1).to_broadcast([128, 32, 32])
                                in1 = imp.unsqueeze(2).to_broadcast([128, 32, 32])
                                k.tt("dve", rk, in0, in1, ALU.is_gt, r=[t_imp], w=[t_rk])
                                rs, t_rs = rs_.next()
                                k.P.op("dve", (lambda o, i: (lambda e: e.reduce_sum(o, i, AX.X)))(rs, rk), r=[t_rk], w=[t_rs])
                                k.ts("dve", negm[:, sub, g, :], rs, 15.5, NEG, ALU.is_gt, ALU.mult, r=[t_rs], w=[t_negm[sub]])
                    if need_sel:
                        pv = ps[4].bitcast(BF16)[:, 0:512].rearrange("p (a t) -> p a t", a=4)
                        for sub in range(4):
                            k.tr(pv[:, sub, :], negm[:, sub, :, :].rearrange("p g n -> p (g n)"), k.ident_bf,
                                 r=[t_negm[sub]], w=[tps[4]])
                        k.cp("dve", negT, ps[4].bitcast(BF16)[:, 0:512], r=[tps[4]], w=[t_negT])
                    for br in range(2):
                        kk = ks2 if br == 0 else kw2
                        vv = vs if br == 0 else vw
                        kt_lo = 0 if br == 0 else max(0, 4 * qt - 4)
                        kt_hi = 4 * qt + 3
                        for g in range(4):
                            for hh in range(4):
                                h = 4 * g + hh
                                ch, base = h // 2, (h % 2) * 64
                                ob = 2 + (h % 2)
                                pov = ps[ob][:, 0:260].rearrange("p (a n) -> p a n", a=4)

                                def qk(kt):
                                    sb = kt % 2
                                    r_ = kt - 4 * qt
                                    extra = []
                                    if br == 0 and need_sel:
                                        extra.append((Ef[:, g, kt * 128:(kt + 1) * 128], negT, [t_c, t_negT]))
                                    k.mm(ps[sb], kk[base:base + 64, g, kt * 128:(kt + 1) * 128], qr[base:base + 64, ch, :],
                                         True, len(extra) == 0, r=[t_res, t_qr], w=[tps[sb]])
                                    for ei, (lt, rh, rt) in enumerate(extra):
                                        k.mm(ps[sb], lt, rh, False, ei == len(extra) - 1, r=rt, w=[tps[sb]])
                                    ex, t_ex = ex_.next()
                                    k.act(ex, ps[sb], AF.Exp, r=[tps[sb]], w=[t_ex], scale=SCALE)
                                    for sub in range(4):
                                        blk = ex[:, sub * 128:(sub + 1) * 128]
                                        if kt == 4 * qt + sub:
                                            k.tt("dve", blk, blk, k.caus_bf, ALU.mult, r=[t_ex], w=[t_ex])
                                        elif br == 1 and kt == 4 * qt + sub - 4:
                                            k.tt("dve", blk, blk, k.low_bf, ALU.mult, r=[t_ex], w=[t_ex])
                                    return ex, t_ex

                                exs = {}
                                for kt in range(kt_lo, kt_hi + 1):
                                    exs[kt] = qk(kt)
                                for sub in range(4):
                                    hi_s = 4 * qt + sub
                                    lo_s = 0 if br == 0 else max(0, hi_s - 4)
                                    for kt in range(lo_s, hi_s + 1):
                                        ex, t_ex = exs[kt]
                                        k.mm(pov[:, sub, :], ex[:, sub * 128:(sub + 1) * 128], vv[:, kt, g, :],
                                             kt == lo_s, kt == hi_s, r=[t_ex, t_res], w=[tps[ob]])
                                tps_of[0] = tps[ob]
                                evac(pov, h, 1 + br, False, gat, t_gat, False, g)
                    for sub in range(4):
                        r0 = t0 + sub * 128
                        szt, t_szt = szt_.next()
                        xr, t_xr = xr_.next()
                        k.ld(szt, T["szt"][s, r0:r0 + 128, :], w=[t_szt])
                        k.ld(xr, x_src[s, r0:r0 + 128, :], w=[t_xr])
                        og, t_og = og_.next()
                        k.tt("dve", og, oacc[:, sub, :, :].rearrange("p h d -> p (h d)"), szt, ALU.mult,
                             r=t_oacc + [t_szt], w=[t_og])
                        pv = ps[4].bitcast(BF16).rearrange("p (a t) -> p a t", a=8)
                        for c8 in range(8):
                            k.tr(pv[:, c8, :], og[:, c8 * 128:(c8 + 1) * 128], k.ident_bf, r=[t_og], w=[tps[4]])
                        ogT, t_ogT = ogT_.next()
                        k.cp("act", ogT, pv, r=[tps[4]], w=[t_ogT])
                        yo, t_yo = yo_.next()
                        for half in range(2):
                            pb = 5 + half
                            for c8 in range(8):
                                k.mm(ps[pb], ogT[:, c8, :], w_out[:, c8, half * 512:(half + 1) * 512], c8 == 0, c8 == 7,
                                     r=[t_ogT, t_wout], w=[tps[pb]])
                            k.tt("dve", yo[:, half * 512:(half + 1) * 512], ps[pb], xr[:, half * 512:(half + 1) * 512], ALU.add,
                                 r=[tps[pb], t_xr], w=[t_yo])
                        k.st(x_dst[s, r0:r0 + 128, :], yo, r=[t_yo])
    P.barrier()


def build_program(nseq=NSEQ, layers=(0, 1, 2, 3), final_norm=True, limit=None):
    nc = bass.Bass("TRN2", target_bir_lowering=False)
    k = K(nc, nseq)
    k.P.limit = limit

    def din(name, shape, dt=F32):
        return nc.dram_tensor(name, list(shape), dt, kind="ExternalInput").ap()

    def dscr(name, shape, dt):
        return nc.dram_tensor(name, list(shape), dt, kind="Internal").ap()

    x = din("x", [nseq, S, D])
    k.ml_norm = din("ml_norm", [2, D])
    k.ml_w_in = din("ml_w_in", [2, D, 4096])
    k.ml_bd = din("ml_bd", [2, 48, 128, 128])
    k.ml_vec = din("ml_vec", [2, 128, 16, 8])
    k.ml_wif = din("ml_wif", [2, 128, 48, 8])
    k.ml_bif = din("ml_bif", [2, 8, 1])
    k.ml_w_out = din("ml_w_out", [2, ML_INNER, D])
    k.final_norm = din("final_norm", [1, D])
    k.nsa_norm = din("nsa_norm", [2, D])
    k.nsa_w_in = din("nsa_w_in", [2, D, 3632])
    k.nsa_b_gate = din("nsa_b_gate", [2, 48])
    k.nsa_peT = din("nsa_peT", [2, 2, 64, 32])
    k.nsa_cmp_w1 = din("nsa_cmp_w1", [2, 2, 2048, 128])
    k.nsa_cmp_w2 = din("nsa_cmp_w2", [2, 2, 128, 64])
    k.nsa_w_out = din("nsa_w_out", [2, D, D])
    k.c_rot = din("c_rot", [128, 128])
    k.c_cos = din("c_cos", [128, S])
    k.c_sin = din("c_sin", [128, S])
    k.c_E = din("c_E", [128, 4 * S], BF16)
    k.c_causn = din("c_causn", [128, 4 * 512], BF16)
    k.c_lown = din("c_lown", [128, 4 * 512], BF16)
    k.c_cmpn = din("c_cmpn", [128, S], BF16)
    k.c_ovl = din("c_ovl", [128, 4 * 33], BF16)
    k.c_fmul = din("c_fmul", [128, 16, 32])
    k.c_fadd = din("c_fadd", [128, 16, 32])
    c_ident = din("c_ident", [128, 128])
    c_caus = din("c_caus", [128, 128])
    y = nc.dram_tensor("y", [nseq, S, D], F32, kind="ExternalOutput").ap()
    xres = dscr("xres", [nseq, S, D], F32)
    T = {
        "qT": dscr("s_qT", [nseq, ML_INNER, S], BF16),
        "kT": dscr("s_kT", [nseq, ML_INNER, S], BF16),
        "sxT": dscr("s_sxT", [nseq, ML_INNER, S], BF16),
        "szT": dscr("s_szT", [nseq, ML_INNER, S], BF16),
        "ktok": dscr("s_ktok", [nseq, S, ML_INNER], BF16),
        "vtok": dscr("s_vtok", [nseq, S, ML_INNER], BF16),
        "gtok": dscr("s_gtok", [nseq, S, 16], F32),
    }
    TN = {
        "qT": dscr("n_qT", [nseq, 1024, S], BF16),
        "qrT": dscr("n_qrT", [nseq, 1024, S], BF16),
        "kcT": dscr("n_kcT", [nseq, 256, S], BF16),
        "vcT": dscr("n_vcT", [nseq, 256, S], BF16),
        "ksT": dscr("n_ksT", [nseq, 256, S], BF16),
        "kwT": dscr("n_kwT", [nseq, 256, S], BF16),
        "vsw": dscr("n_vsw", [nseq, S, 512], BF16),
        "szt": dscr("n_szt", [nseq, S, 1024], BF16),
        "gate": dscr("n_gate", [nseq, S, 48], F32),
    }

    def C(name, shape, dt):
        return nc.alloc_sbuf_tensor(name, shape, dt).ap()
    k.ident_f = C("ident_f", [128, 128], F32)
    k.ident_bf = C("ident_bf", [128, 128], BF16)
    k.caus_f = C("caus_f", [128, 128], F32)
    k.ones_f = C("ones_f", [128, 128], F32)
    k.caus_bf = C("caus_bf", [128, 128], BF16)
    k.low_bf = C("low_bf", [128, 128], BF16)
    k.c_eps = C("c_eps", [128, 1], F32)
    k.c_one = C("c_one", [128, 1], F32)
    k.ps = [nc.alloc_psum_tensor(f"ps{i}", [128, 512], F32).ap() for i in range(8)]
    k.tps = [Tok(f"ps{i}", x=True) for i in range(8)]
    tc = Tok()
    k.ld(k.ident_f, c_ident, w=[tc])
    k.ld(k.caus_f, c_caus, w=[tc])
    k.cp("dve", k.ident_bf, k.ident_f, r=[tc], w=[tc])
    k.memset("dve", k.ones_f, 1.0, w=[tc])
    k.cp("dve", k.caus_bf, k.caus_f, r=[tc], w=[tc])
    k.ts("dve", k.low_bf, k.caus_f, -1.0, 1.0, ALU.mult, ALU.add, r=[tc], w=[tc])
    k.memset("dve", k.c_eps, RMS_EPS, w=[tc])
    k.memset("dve", k.c_one, 1.0, w=[tc])
    k.P.barrier()

    cur = x
    for L in layers:
        li = L // 2
        if L % 2 == 0:
            ml_phase_a(k, li, cur, T)
            ml_phase_b(k, li, cur, xres, T)
        else:
            nsa_phase_a(k, li, cur, TN)
            nsa_phase_c(k, li, cur, xres, TN)
        cur = xres

    print("n_ins before final", k.P.n_ins, flush=True)
    k.P.limit = None
    with ExitStack() as es:
        def A(name, shape, dt):
            return es.enter_context(nc.sbuf_tensor(f"fin_{name}", shape, dt)).ap()
        gb = A("gb", [128, D], F32)
        t_gb = Tok()
        k.ld(gb, k.final_norm[0].partition_broadcast(128), w=[t_gb])
        xr_ = Ring([A(f"x{i}", [128, D], F32) for i in range(3)])
        junk = A("junk", [128, D], F32)
        t_junk = Tok()
        st_ = Ring([A(f"st{i}", [128, 4], F32) for i in range(3)])
        t_y = Tok()
        for s in range(nseq):
            for c in range(NT):
                r0 = c * 128
                xt, t_xt = xr_.next()
                k.ld(xt, cur[s, r0:r0 + 128, :], w=[t_xt])
                if final_norm:
                    sv, t_sv = st_.next()
                    k.act(junk, xt, AF.Square, r=[t_xt], w=[t_junk, t_sv], accum=sv[:, 0:1])
                    k.act(sv[:, 1:2], sv[:, 0:1], AF.Ln, r=[t_sv], w=[t_sv], scale=1.0 / D, bias=k.c_eps[:, 0:1])
                    k.act(sv[:, 2:3], sv[:, 1:2], AF.Exp, r=[t_sv], w=[t_sv], scale=-0.5)
                    k.stt("dve", xt, xt, sv[:, 2:3], gb, ALU.mult, ALU.mult, r=[t_xt, t_sv, t_gb], w=[t_xt])
                k.P.dma("sp", y[s, r0:r0 + 128, :], xt, r=[t_xt], w=[t_y])
        k.P._emit_waits("sp", k.P._deps([t_y], [t_y]))
        deps = {kk: v for kk, v in k.P.cnt.items() if kk.startswith("d_sp") and v > 0}
        k.P._emit_waits("sp", deps)
    k.P.emit()
    return nc


def _block_diag(w):
    out = np.zeros((16, 128, 128), np.float32)
    w4 = w.reshape(16, 32, 4, 4)
    for n in range(32):
        out[:, 4 * n:4 * n + 4, 4 * n:4 * n + 4] = w4[:, n].transpose(0, 2, 1)
    return out


_CONSTS = {}


def _nsa_consts():
    if _CONSTS:
        return _CONSTS
    import ml_dtypes
    bf = ml_dtypes.bfloat16
    c = {}
    rot = np.zeros((128, 128), np.float32)
    for hb in (0, 64):
        for d in range(8):
            rot[hb + d + 8, hb + d] = -1.0
            rot[hb + d, hb + d + 8] = 1.0
    c["c_rot"] = rot
    pos = np.arange(S, dtype=np.float32)
    inv_freq = (np.float32(500000.0) ** (-np.arange(0, 16, 2, dtype=np.float32) / np.float32(16))).astype(np.float32)
    ang = (pos[:, None] * inv_freq[None, :]).astype(np.float32)
    cosf = np.ones((128, S), np.float32)
    sinf = np.zeros((128, S), np.float32)
    for hb in (0, 64):
        for d in range(16):
            cosf[hb + d] = np.cos(ang[:, d % 8])
            sinf[hb + d] = np.sin(ang[:, d % 8])
    c["c_cos"], c["c_sin"] = cosf, sinf
    E = np.zeros((128, 4, S), np.float32)
    key = np.arange(S)
    for g in range(4):
        E[g * 32 + key // 64, g, key] = 1.0
    c["c_E"] = E.reshape(128, 4 * S).astype(bf)
    kk = np.arange(128)[:, None]
    tt = np.arange(512)[None, :]
    causn = np.zeros((128, 4, 512), np.float32)
    lown = np.zeros((128, 4, 512), np.float32)
    for r in range(4):
        valid = kk <= tt - 128 * r
        causn[:, r, :] = np.where(valid, 0.0, NEG)
        lown[:, r, :] = np.where(valid, NEG, 0.0)
    c["c_causn"] = causn.reshape(128, 2048).astype(bf)
    c["c_lown"] = lown.reshape(128, 2048).astype(bf)
    cc = np.arange(128)[:, None]
    tok = np.arange(S)[None, :]
    c["c_cmpn"] = np.where(16 * cc + 31 <= tok, 0.0, NEG).astype(np.float32).astype(bf)
    cs = np.arange(127) * 16
    ss = np.arange(32) * 64
    ov = np.clip(np.minimum(cs[:, None] + 32, ss[None, :] + 64) - np.maximum(cs[:, None], ss[None, :]), 0, None) / 16.0
    ovl = np.zeros((128, 4, 33), np.float32)
    ovl[:, :, 0] = 1.0
    ovl[:127, :, 1:] = ov[:, None, :]
    c["c_ovl"] = ovl.reshape(128, 4 * 33).astype(bf)
    p = np.arange(S)
    blk = np.arange(32)
    dist = (p // 64)[:, None] - blk[None, :]
    forced = (blk[None, :] == 0) | ((dist >= 0) & (dist < 2))
    fmul = np.where(forced | (dist < 0), 0.0, 1.0).astype(np.float32)
    fadd = np.where(forced, 1e9, np.where(dist >= 0, 0.0, -1.0)).astype(np.float32)
    c["c_fmul"] = np.ascontiguousarray(fmul.reshape(16, 128, 32).transpose(1, 0, 2))
    c["c_fadd"] = np.ascontiguousarray(fadd.reshape(16, 128, 32).transpose(1, 0, 2))
    _CONSTS.update(c)
    return _CONSTS


def host_layout(inp):
    f = lambda a: np.ascontiguousarray(np.asarray(a, dtype=np.float32))
    d = {}
    d["ml_norm"] = f(inp["ml_norm"])
    d["ml_w_in"] = f(inp["ml_w_in"])
    bd = np.zeros((2, 48, 128, 128), np.float32)
    vec = np.zeros((2, 128, 16, 8), np.float32)
    for li in range(2):
        for g3, nm in enumerate(("ml_w_q", "ml_w_k", "ml_w_v")):
            bd[li, g3 * 16:(g3 + 1) * 16] = _block_diag(np.asarray(inp[nm][li], np.float32))
        cw = np.asarray(inp["ml_conv_w"][li], np.float32)
        for tap in range(4):
            vec[li, :, :, tap] = cw[tap].reshape(16, 128).T
        vec[li, :, :, 4] = np.asarray(inp["ml_conv_b"][li], np.float32).reshape(16, 128).T
        vec[li, :, :, 5] = np.asarray(inp["ml_ln_w"][li], np.float32).reshape(16, 128).T
        vec[li, :, :, 6] = np.asarray(inp["ml_skip"][li], np.float32).reshape(16, 128).T
    d["ml_bd"] = bd
    d["ml_vec"] = vec
    d["ml_wif"] = f(np.asarray(inp["ml_w_if"], np.float32).reshape(2, 48, 128, 8).transpose(0, 2, 1, 3))
    d["ml_bif"] = f(np.asarray(inp["ml_b_if"], np.float32).reshape(2, 8, 1))
    d["ml_w_out"] = f(inp["ml_w_out"])
    d["final_norm"] = f(np.asarray(inp["final_norm"], np.float32).reshape(1, D))
    d["nsa_norm"] = f(inp["nsa_norm"])
    d["nsa_w_in"] = f(inp["nsa_w_in"])
    d["nsa_b_gate"] = f(inp["nsa_b_gate"])
    d["nsa_peT"] = f(np.asarray(inp["nsa_cmp_pe"], np.float32).transpose(0, 1, 3, 2))
    d["nsa_cmp_w1"] = f(inp["nsa_cmp_w1"])
    d["nsa_cmp_w2"] = f(inp["nsa_cmp_w2"])
    d["nsa_w_out"] = f(inp["nsa_w_out"])
    d.update(_nsa_consts())
    d["c_ident"] = np.eye(128, dtype=np.float32)
    d["c_caus"] = np.triu(np.ones((128, 128), np.float32))
    return d


_NC_CACHE = {}


def kernel(**inputs):
    x = np.asarray(inputs["x"], np.float32)
    shared = host_layout(inputs)
    if "nc" not in _NC_CACHE:
        _NC_CACHE["nc"] = build_program()
    nc = _NC_CACHE["nc"]
    in_maps = []
    for c in range(NCORES):
        m = dict(shared)
        m["x"] = np.ascontiguousarray(x[c * NSEQ:(c + 1) * NSEQ])
        in_maps.append(m)
    res = run_bass_kernel_spmd(nc, in_maps, core_ids=list(range(NCORES)))
    return np.concatenate([r["y"] for r in res.results], axis=0)
```

```python
from contextlib import ExitStack
import numpy as np
import concourse.bass as bass
import concourse.mybir as mybir
from concourse.bass_utils import run_bass_kernel_spmd

F32 = mybir.dt.float32
BF16 = mybir.dt.bfloat16
AF = mybir.ActivationFunctionType
ALU = mybir.AluOpType
AX = mybir.AxisListType

S = 2048
D = 1024
NT = S // 128
NSEQ = 2
NCORES = 8
ML_INNER = 2048
NH = 4
DH = 512
RMS_EPS = 1e-6
LN_EPS = 1e-6
NEG = -30000.0


class Tok:
    __slots__ = ("name", "w", "r", "x")

    def __init__(self, name="", x=False):
        self.name = name
        self.w = None
        self.r = {}
        self.x = x


class Prog:
    ENG = ("pe", "dve", "act", "pool", "sp")

    def __init__(self, nc, n_dma_ring=12):
        self.nc = nc
        self.q = {e: [] for e in self.ENG}
        self.sems = {}
        self.cnt = {}
        for e in self.ENG:
            self.sems[e] = nc.alloc_semaphore(name=f"c_{e}")
            self.cnt[e] = 0
        self.ring = {}
        self.ring_pos = {}
        for qn in ("sp", "act", "pool"):
            ks = []
            for i in range(n_dma_ring):
                k = f"d_{qn}{i}"
                self.sems[k] = nc.alloc_semaphore(name=k)
                self.cnt[k] = 0
                ks.append(k)
            self.ring[qn] = ks
            self.ring_pos[qn] = 0
        self.seen = {e: {} for e in self.ENG}
        self.n_ins = 0

    def _deps(self, r, w):
        deps = {}

        def add(k, v):
            if deps.get(k, 0) < v:
                deps[k] = v
        for t in r:
            if t.w is not None:
                add(*t.w)
            if t.x:
                for k, v in t.r.items():
                    add(k, v)
        for t in w:
            if t.w is not None:
                add(*t.w)
            for k, v in t.r.items():
                add(k, v)
        return deps

    def _emit_waits(self, e, deps):
        seen = self.seen[e]
        for k, v in deps.items():
            if k == e and e == "pe":
                continue
            if seen.get(k, 0) >= v:
                continue
            seen[k] = v
            self.q[e].append(("w", self.sems[k], v))

    limit = None

    def op(self, e, fn, r=(), w=()):
        if self.limit is not None and self.n_ins >= self.limit:
            return
        self._emit_waits(e, self._deps(r, w))
        self.cnt[e] += 1
        c = self.cnt[e]
        self.q[e].append(("i", fn, self.sems[e]))
        self.n_ins += 1
        for t in w:
            t.w = (e, c)
            t.r = {}
        for t in r:
            if t.r.get(e, 0) < c:
                t.r[e] = c

    def dma(self, qn, out, in_, r=(), w=()):
        if self.limit is not None and self.n_ins >= self.limit:
            return
        deps = self._deps(r, w)
        k = self.ring[qn][self.ring_pos[qn]]
        self.ring_pos[qn] = (self.ring_pos[qn] + 1) % len(self.ring[qn])
        if self.cnt[k] > 0 and deps.get(k, 0) < self.cnt[k]:
            deps[k] = self.cnt[k]
        self._emit_waits(qn, deps)
        self.cnt[k] += 16
        c = self.cnt[k]
        self.q[qn].append(("d", out, in_, self.sems[k]))
        self.n_ins += 1
        for t in w:
            t.w = (k, c)
            t.r = {}
        for t in r:
            if t.r.get(k, 0) < c:
                t.r[k] = c

    def barrier(self):
        deps = {k: v for k, v in self.cnt.items() if v > 0}
        for e in self.ENG:
            self._emit_waits(e, dict(deps))

    def emit(self):
        nc = self.nc
        with nc.Block() as block:
            def mk(e):
                def body(engine):
                    for it in self.q[e]:
                        if it[0] == "w":
                            engine.wait_ge(it[1], it[2])
                        elif it[0] == "i":
                            it[1](engine).then_inc(it[2], 1)
                        else:
                            engine.dma_start(out=it[1], in_=it[2]).then_inc(it[3], 16)
                return body
            block.tensor(mk("pe"))
            block.vector(mk("dve"))
            block.scalar(mk("act"))
            block.gpsimd(mk("pool"))
            block.sync(mk("sp"))


class Ring:
    def __init__(self, aps):
        self.items = [(a, Tok()) for a in aps]
        self.i = 0

    def next(self):
        it = self.items[self.i]
        self.i = (self.i + 1) % len(self.items)
        return it


class K:
    def __init__(self, nc, nseq):
        self.nc = nc
        self.P = Prog(nc)
        self.nseq = nseq

    def mm(self, out, lhsT, rhs, start, stop, r, w):
        self.P.op("pe", lambda e: e.matmul(out, lhsT=lhsT, rhs=rhs, start=start, stop=stop), r, w)

    def tr(self, out, in_, ident, r, w):
        self.P.op("pe", lambda e: e.transpose(out, in_, ident), r, w)

    def cp(self, eng, out, in_, r, w):
        if eng == "act":
            self.P.op("act", lambda e: e.activation(out, in_, AF.Copy), r, w)
        else:
            self.P.op(eng, lambda e: e.tensor_copy(out, in_), r, w)

    def act(self, out, in_, func, r, w, bias=None, scale=None, accum=None):
        kw = {}
        if bias is not None:
            kw["bias"] = bias
        if scale is not None:
            kw["scale"] = scale
        if accum is not None:
            kw["accum_out"] = accum
        self.P.op("act", lambda e: e.activation(out, in_, func, **kw), r, w)

    def tt(self, eng, out, in0, in1, op, r, w):
        self.P.op(eng, lambda e: e.tensor_tensor(out, in0, in1, op), r, w)

    def ts(self, eng, out, in0, s1, s2, op0, op1, r, w):
        if s2 is None:
            self.P.op(eng, lambda e: e.tensor_scalar(out, in0, s1, None, op0), r, w)
        else:
            self.P.op(eng, lambda e: e.tensor_scalar(out, in0, s1, s2, op0, op1), r, w)

    def stt(self, eng, out, in0, scalar, in1, op0, op1, r, w):
        self.P.op(eng, lambda e: e.scalar_tensor_tensor(out, in0, scalar, in1, op0, op1), r, w)

    def memset(self, eng, out, val, w):
        self.P.op(eng, lambda e: e.memset(out, val), (), w)

    def recip(self, out, in_, r, w):
        self.P.op("dve", lambda e: e.reciprocal(out, in_), r, w)

    def ld(self, out, in_, w, r=()):
        self.P.dma("sp", out, in_, r, w)

    def st(self, out, in_, r, w=()):
        self.P.dma("sp", out, in_, r, w)


def bcast_last(ap, n):
    shp = list(ap.shape)
    return ap.unsqueeze(len(shp)).to_broadcast(shp + [n])


def ml_phase_a(k, li, x_src, T):
    nc, P = k.nc, k.P
    ps, tps = k.ps, k.tps
    with ExitStack() as es:
        def A(name, shape, dt):
            return es.enter_context(nc.sbuf_tensor(f"mA{li}_{name}", shape, dt)).ap()
        w_in = A("win", [128, 8, 4096], BF16)
        t_win = [Tok() for _ in range(8)]
        bd = A("bd", [128, 48, 128], BF16)
        t_bd = Tok()
        vec = A("vec", [128, 16, 8], F32)
        t_vec = Tok()
        wif32 = A("wif32", [128, 48, 8], F32)
        wif = A("wif", [128, 48, 8], BF16)
        t_wif = Tok()
        bif = A("bif", [8, 1], F32)
        t_bif = Tok()
        gb = A("gb", [128, 1024], F32)
        t_gb = Tok()
        stg = Ring([A(f"stg{i}", [128, 2048], F32) for i in range(2)])
        xr_ = Ring([A(f"xt{i}", [128, 1024], F32) for i in range(2)])
        hb_ = Ring([A(f"hb{i}", [128, 1024], BF16) for i in range(2)])
        junk = A("junk", [128, 1024], BF16)
        t_junk = Tok()
        st_ = Ring([A(f"st{i}", [128, 4], F32) for i in range(2)])
        hT = A("hT", [128, 8, 512], BF16)
        t_hT = Tok()
        xin = A("xin", [128, 16, 515], BF16)
        t_xin = [Tok() for _ in range(16)]
        acc_ = Ring([A(f"acc{i}", [128, 512], F32) for i in range(3)])
        xc = A("xc", [128, 16, 512], BF16)
        t_xc = [Tok() for _ in range(16)]
        sx_ = Ring([A(f"sx{i}", [128, 512], BF16) for i in range(2)])
        sz_ = Ring([A(f"sz{i}", [128, 512], BF16) for i in range(2)])
        qkv_ = Ring([A(f"qkv{i}", [128, 512], BF16) for i in range(8)])
        tok_ = Ring([A(f"tokm{i}", [128, 2048], BF16) for i in range(3)])
        gsb = A("gsb", [8, 2, 512], F32)
        t_gsb = Tok()
        gt_ = Ring([A(f"gt{i}", [128, 16], F32) for i in range(2)])

        for kc in range(8):
            for hf in range(2):
                s_ap, s_t = stg.next()
                k.ld(s_ap, k.ml_w_in[li, kc * 128:(kc + 1) * 128, hf * 2048:(hf + 1) * 2048], w=[s_t])
                k.cp(("dve", "act")[hf], w_in[:, kc, hf * 2048:(hf + 1) * 2048], s_ap, r=[s_t], w=[t_win[kc]])
        for g3 in range(3):
            s_ap, s_t = stg.next()
            sv = s_ap.rearrange("p (c n) -> p c n", c=16)
            k.ld(sv, k.ml_bd[li, g3 * 16:(g3 + 1) * 16].rearrange("c p n -> p c n"), w=[s_t])
            k.cp("dve", bd[:, g3 * 16:(g3 + 1) * 16, :], sv, r=[s_t], w=[t_bd])
        k.ld(vec, k.ml_vec[li], w=[t_vec])
        k.ld(wif32, k.ml_wif[li], w=[t_wif])
        k.ts("dve", wif32[:, 0:16, :], wif32[:, 0:16, :], float(DH ** 0.5), None, ALU.mult, None, r=[t_wif], w=[t_wif])
        k.cp("dve", wif, wif32, r=[t_wif], w=[t_wif])
        k.ld(bif, k.ml_bif[li], w=[t_bif])
        k.ld(gb, k.ml_norm[li].partition_broadcast(128), w=[t_gb])

        for s in range(k.nseq):
            for st in range(4):
                t0 = st * 512
                for sub in range(4):
                    r0 = t0 + sub * 128
                    xt, t_xt = xr_.next()
                    k.ld(xt, x_src[s, r0:r0 + 128, :], w=[t_xt])
                    sv, t_sv = st_.next()
                    k.act(junk, xt, AF.Square, r=[t_xt], w=[t_junk, t_sv], accum=sv[:, 0:1])
                    k.act(sv[:, 1:2], sv[:, 0:1], AF.Ln, r=[t_sv], w=[t_sv], scale=1.0 / D, bias=k.c_eps[:, 0:1])
                    k.act(sv[:, 2:3], sv[:, 1:2], AF.Exp, r=[t_sv], w=[t_sv], scale=-0.5)
                    hb, t_hb = hb_.next()
                    k.stt("dve", hb, xt, sv[:, 2:3], gb, ALU.mult, ALU.mult, r=[t_xt, t_sv, t_gb], w=[t_hb])
                    pv = ps[5].bitcast(BF16).rearrange("p (a t) -> p a t", a=8)
                    for kc in range(8):
                        k.tr(pv[:, kc, :], hb[:, kc * 128:(kc + 1) * 128], k.ident_bf, r=[t_hb], w=[tps[5]])
                    k.cp("dve", hT[:, :, sub * 128:(sub + 1) * 128], pv, r=[tps[5]], w=[t_hT])
                if st == 0:
                    k.memset("dve", xin[:, :, 0:3], 0.0, w=t_xin)
                else:
                    k.cp("dve", xin[:, :, 0:3], xin[:, :, 512:515], r=t_xin, w=t_xin)
                def proj_x1(fc):
                    bi = (0, 1, 7)[fc % 3]
                    for kc in range(8):
                        k.mm(ps[bi], w_in[:, kc, fc * 128:(fc + 1) * 128], hT[:, kc, :], kc == 0, kc == 7,
                             r=[t_win[kc], t_hT], w=[tps[bi]])
                    k.cp("act", xin[:, fc, 3:515], ps[bi], r=[tps[bi]], w=[t_xin[fc]])

                def proj_x2a(fc):
                    acc, t_acc = acc_.next()
                    k.act(acc, xin[:, fc, 0:512], AF.Copy, r=[t_xin[fc], t_vec], w=[t_acc], scale=vec[:, fc, 0:1])
                    for tap in range(1, 4):
                        k.stt("dve", acc, xin[:, fc, tap:tap + 512], vec[:, fc, tap:tap + 1], acc, ALU.mult, ALU.add,
                              r=[t_xin[fc], t_acc], w=[t_acc])
                    pend_acc[fc] = (acc, t_acc)

                def proj_x2b(fc):
                    acc, t_acc = pend_acc.pop(fc)
                    k.act(xc[:, fc, :], acc, AF.Silu, r=[t_acc], w=[t_xc[fc]], bias=vec[:, fc, 4:5])
                    sx, t_sx = sx_.next()
                    k.act(sx, xc[:, fc, :], AF.Copy, r=[t_xc[fc]], w=[t_sx], scale=vec[:, fc, 6:7])
                    k.st(T["sxT"][s, fc * 128:(fc + 1) * 128, t0:t0 + 512], sx, r=[t_sx])

                def bdmm(fc):
                    tiles = []
                    for which in range(3):
                        pb = (2, 3, 6)[which]
                        src = xc[:, fc, :] if which < 2 else xin[:, fc, 3:515]
                        tsrc = t_xc[fc] if which < 2 else t_xin[fc]
                        k.mm(ps[pb], bd[:, which * 16 + fc, :], src, True, True, r=[t_bd, tsrc], w=[tps[pb]])
                    for which in range(3):
                        pb = (2, 3, 6)[which]
                        qt, t_qt = qkv_.next()
                        if which == 0:
                            k.act(qt, ps[pb], AF.Copy, r=[tps[pb]], w=[t_qt], scale=float(DH ** -0.5))
                            k.st(T["qT"][s, fc * 128:(fc + 1) * 128, t0:t0 + 512], qt, r=[t_qt])
                        elif which == 1:
                            k.cp("dve", qt, ps[pb], r=[tps[pb]], w=[t_qt])
                            k.st(T["kT"][s, fc * 128:(fc + 1) * 128, t0:t0 + 512], qt, r=[t_qt])
                        else:
                            k.cp("dve", qt, ps[pb], r=[tps[pb]], w=[t_qt])
                        tiles.append((qt, t_qt))
                    pend_gates[fc] = tiles

                def gates(fc):
                    for which, (qt, t_qt) in enumerate(pend_gates.pop(fc)):
                        first = (fc == 0 and which == 0)
                        last = (fc == 15 and which == 2)
                        k.mm(ps[4][0:8, :], wif[:, which * 16 + fc, :], qt, first, last, r=[t_wif, t_qt], w=[tps[4]])

                def proj_z(fc):
                    bi = (0, 1, 7)[fc % 3]
                    for kc in range(8):
                        k.mm(ps[bi], w_in[:, kc, fc * 128:(fc + 1) * 128], hT[:, kc, :], kc == 0, kc == 7,
                             r=[t_win[kc], t_hT], w=[tps[bi]])
                    sz, t_sz = sz_.next()
                    k.act(sz, ps[bi], AF.Silu, r=[tps[bi]], w=[t_sz])
                    k.st(T["szT"][s, (fc - 16) * 128:(fc - 15) * 128, t0:t0 + 512], sz, r=[t_sz])

                pend_gates = {}
                pend_acc = {}
                for fc in range(32):
                    if fc < 16:
                        proj_x1(fc)
                    else:
                        proj_z(fc)
                    if 3 <= fc < 19:
                        bdmm(fc - 3)
                    if 4 <= fc < 20:
                        gates(fc - 4)
                    if fc < 16:
                        proj_x2a(fc)
                    if 1 <= fc < 17:
                        proj_x2b(fc - 1)
                for which in (1, 2):
                    for sub in range(4):
                        tm, t_tm = tok_.next()
                        for fg in range(4):
                            pb = 6 + (fg % 2)
                            for f4 in range(4):
                                fc = fg * 4 + f4
                                if which == 1:
                                    lhsT = xc[:, fc, sub * 128:(sub + 1) * 128]
                                    tsrc = t_xc[fc]
                                else:
                                    lhsT = xin[:, fc, 3 + sub * 128:3 + (sub + 1) * 128]
                                    tsrc = t_xin[fc]
                                k.mm(ps[pb][:, f4 * 128:(f4 + 1) * 128], lhsT, bd[:, which * 16 + fc, :], True, True,
                                     r=[t_bd, tsrc], w=[tps[pb]])
                            k.cp(("act", "dve")[fg % 2], tm[:, fg * 512:(fg + 1) * 512], ps[pb], r=[tps[pb]], w=[t_tm])
                        dst = T["ktok"] if which == 1 else T["vtok"]
                        r0 = t0 + sub * 128
                        k.st(dst[s, r0:r0 + 128, :], tm, r=[t_tm])
                k.act(gsb[:, 0, :], ps[4][0:8, :], AF.Identity, r=[tps[4], t_bif], w=[t_gsb], bias=bif[:, 0:1])
                k.act(gsb[:, 1, :], gsb[:, 0, :], AF.Exp, r=[t_gsb], w=[t_gsb], scale=-1.0)
                k.act(gsb[:, 1, :], gsb[:, 1, :], AF.Ln, r=[t_gsb], w=[t_gsb], bias=k.c_one[0:8, 0:1])
                for sub in range(4):
                    for a in range(2):
                        k.tr(ps[5][:, a * 8:(a + 1) * 8], gsb[:, a, sub * 128:(sub + 1) * 128], k.ident_f[0:8, 0:8],
                             r=[t_gsb], w=[tps[5]])
                    gt, t_gt = gt_.next()
                    k.cp("dve", gt, ps[5][:, 0:16], r=[tps[5]], w=[t_gt])
                    r0 = t0 + sub * 128
                    k.st(T["gtok"][s, r0:r0 + 128, :], gt, r=[t_gt])
    P.barrier()


def ml_phase_b(k, li, x_src, x_dst, T):
    nc, P = k.nc, k.P
    ps, tps = k.ps, k.tps
    with ExitStack() as es:
        def A(name, shape, dt):
            return es.enter_context(nc.sbuf_tensor(f"mB{li}_{name}", shape, dt)).ap()
        w_out = A("wout", [128, 16, 1024], BF16)
        t_wout = Tok()
        vec = A("vec", [128, 16, 8], F32)
        t_vec = Tok()
        stg = Ring([A(f"stg{i}", [128, 2048], F32) for i in range(1)])
        X = A("X", [128, 16, 513], F32)
        t_X = [Tok() for _ in range(4)]
        STb = A("STb", [128, 16, 513], BF16)
        t_STb = [Tok() for _ in range(4)]
        qT_ = Ring([A(f"qT{i}", [128, 16, 128], BF16) for i in range(2)])
        kT_ = Ring([A(f"kT{i}", [128, 16, 128], BF16) for i in range(2)])
        kt_ = Ring([A(f"kt{i}", [128, 2048], BF16) for i in range(2)])
        vt_ = Ring([A(f"vt{i}", [128, 2048], BF16) for i in range(2)])
        gt_ = Ring([A(f"gt{i}", [128, 16], F32) for i in range(2)])
        xr_ = Ring([A(f"xr{i}", [128, 1024], F32) for i in range(3)])
        sx_ = Ring([A(f"sx{i}", [128, 16, 128], BF16) for i in range(3)])
        sz_ = Ring([A(f"sz{i}", [128, 16, 128], BF16) for i in range(3)])
        ve_ = Ring([A(f"ve{i}", [128, 4, 520], BF16) for i in range(2)])
        PT_ = Ring([A(f"PT{i}", [128, 128], BF16) for i in range(4)])
        gm_ = Ring([A(f"gm{i}", [128, 4, 4], F32) for i in range(3)])
        sm_ = Ring([A(f"sm{i}", [128, 8, 4], F32) for i in range(2)])
        mv_ = Ring([A(f"mv{i}", [128, 4, 2], F32) for i in range(2)])
        bs_ = Ring([A(f"bs{i}", [128, 6], F32) for i in range(4)])
        hn_ = Ring([A(f"hn{i}", [128, 2048], BF16) for i in range(2)])
        t1 = A("t1", [128, 16, 128], F32)
        t_t1 = Tok()
        gT_ = Ring([A(f"gT{i}", [128, 16, 128], BF16) for i in range(2)])
        yo_ = Ring([A(f"yo{i}", [128, 1024], F32) for i in range(2)])

        for fc in range(0, 16, 2):
            s_ap, s_t = stg.next()
            sv = s_ap.rearrange("p (c n) -> p c n", c=2)
            k.ld(sv, k.ml_w_out[li, fc * 128:(fc + 2) * 128, :].rearrange("(c p) n -> p c n", p=128), w=[s_t])
            k.cp("dve", w_out[:, fc:fc + 2, :], sv, r=[s_t], w=[t_wout])
        k.ld(vec, k.ml_vec[li], w=[t_vec])

        def issue_loads(s, c):
            r0 = c * 128
            d = {}
            for nm, ring in (("gt", gt_), ("kT", kT_), ("qT", qT_), ("vt", vt_), ("kt", kt_), ("sx", sx_), ("sz", sz_),
                             ("xr", xr_)):
                d[nm] = ring.next()
            k.ld(d["gt"][0], T["gtok"][s, r0:r0 + 128, :], w=[d["gt"][1]])
            k.ld(d["kT"][0], T["kT"][s, :, r0:r0 + 128].rearrange("(a p) t -> p a t", p=128), w=[d["kT"][1]])
            k.ld(d["qT"][0], T["qT"][s, :, r0:r0 + 128].rearrange("(a p) t -> p a t", p=128), w=[d["qT"][1]])
            k.ld(d["vt"][0], T["vtok"][s, r0:r0 + 128, :], w=[d["vt"][1]])
            k.ld(d["kt"][0], T["ktok"][s, r0:r0 + 128, :], w=[d["kt"][1]])
            k.ld(d["sx"][0], T["sxT"][s, :, r0:r0 + 128].rearrange("(a p) t -> p a t", p=128), w=[d["sx"][1]])
            k.ld(d["sz"][0], T["szT"][s, :, r0:r0 + 128].rearrange("(a p) t -> p a t", p=128), w=[d["sz"][1]])
            k.ld(d["xr"][0], x_src[s, r0:r0 + 128, :], w=[d["xr"][1]])
            return d

        def epilogue(pe_, fillers=()):
            fillers = list(fillers)
            hn, t_hn, sx, t_sx, sz, t_sz, xr, t_xr, s, r0 = pe_
            for half in range(2):
                pb = 6 + half
                pv = ps[pb].bitcast(BF16).rearrange("p (a t) -> p a t", a=8)
                for a in range(8):
                    fc = half * 8 + a
                    k.tr(pv[:, a, :], hn[:, fc * 128:(fc + 1) * 128], k.ident_bf, r=[t_hn], w=[tps[pb]])
                k.tt("dve", t1[:, half * 8:(half + 1) * 8, :], pv, bcast_last(vec[:, half * 8:(half + 1) * 8, 5], 128),
                     ALU.mult, r=[tps[pb], t_vec], w=[t_t1])
            gT, t_gT = gT_.next()
            k.tt("dve", t1, t1, sx, ALU.add, r=[t_sx, t_t1], w=[t_t1])
            k.tt("dve", gT, t1, sz, ALU.mult, r=[t_sz, t_t1], w=[t_gT])
            yo, t_yo = yo_.next()
            for half in range(2):
                pb = 6 + half
                for fc in range(16):
                    k.mm(ps[pb], gT[:, fc, :], w_out[:, fc, half * 512:(half + 1) * 512], fc == 0, fc == 15,
                         r=[t_gT, t_wout], w=[tps[pb]])
                    if fc % 2 == 1 and fillers:
                        fillers.pop(0)()
                k.tt("dve", yo[:, half * 512:(half + 1) * 512], ps[pb], xr[:, half * 512:(half + 1) * 512], ALU.add,
                     r=[tps[pb], t_xr], w=[t_yo])
            k.st(x_dst[s, r0:r0 + 128, :], yo, r=[t_yo])
            for f in fillers:
                f()


        work = [(s, c) for s in range(k.nseq) for c in range(NT)]
        nxt = issue_loads(*work[0])
        prev_gm = None
        pend_epi = None
        for wi, (s, c) in enumerate(work):
            r0 = c * 128
            L = nxt
            if wi + 1 < len(work):
                nxt = issue_loads(*work[wi + 1])
            (gt, t_gt), (kT, t_kT), (qT, t_qT), (vt, t_vt) = L["gt"], L["kT"], L["qT"], L["vt"]
            (kt, t_kt), (sx, t_sx), (sz, t_sz), (xr, t_xr) = L["kt"], L["sx"], L["sz"], L["xr"]
            if c == 0:
                prev_gm = None
            gm, t_gm = gm_.next()
            k.mm(ps[0][:, 0:4], k.caus_f, gt[:, 12:16], True, True, r=[t_gt], w=[tps[0]])
            k.mm(ps[0][:, 4:8], k.ones_f, gt[:, 12:16], True, True, r=[t_gt], w=[tps[0]])
            k.tt("dve", gm[:, 3, :], ps[0][:, 0:4], gt[:, 0:4], ALU.add, r=[tps[0], t_gt], w=[t_gm])
            k.act(gm[:, 0, :], gm[:, 3, :], AF.Exp, r=[t_gm], w=[t_gm])
            k.act(gm[:, 1:3, :], ps[0][:, 0:8].rearrange("p (a b) -> p a b", a=2), AF.Exp, r=[tps[0]], w=[t_gm],
                  scale=-1.0)
            ve, t_ve = ve_.next()
            for h in range(NH):
                k.act(ve[:, h, 0:512], vt[:, h * 512:(h + 1) * 512], AF.Copy, r=[t_vt, t_gm], w=[t_ve],
                      scale=gm[:, 0, h:h + 1])
            k.cp("dve", ve[:, :, 512], gm[:, 0, :], r=[t_gm], w=[t_ve])
            for h in range(NH):
                for dc in range(4):
                    k.mm(ps[1][:, 0:128], kT[:, h * 4 + dc, :], qT[:, h * 4 + dc, :], dc == 0, dc == 3,
                         r=[t_kT, t_qT], w=[tps[1]])
                PT, t_PT = PT_.next()
                k.tt("dve", PT, ps[1][:, 0:128], k.caus_f, ALU.mult, r=[tps[1]], w=[t_PT])
                nb = 2 + h
                k.mm(ps[nb], PT, ve[:, h, 0:512], True, c == 0, r=[t_PT, t_ve], w=[tps[nb]])
                if c > 0:
                    for dc in range(4):
                        k.mm(ps[nb], qT[:, h * 4 + dc, :], STb[:, h * 4 + dc, 0:512], False, dc == 3,
                             r=[t_qT, t_STb[h]], w=[tps[nb]])
                k.mm(ps[0][:, 8 + h:9 + h], PT, ve[:, h, 512:513], True, c == 0, r=[t_PT, t_ve], w=[tps[0]])
                if c > 0:
                    for dc in range(4):
                        k.mm(ps[0][:, 8 + h:9 + h], qT[:, h * 4 + dc, :], STb[:, h * 4 + dc, 512:513], False, dc == 3,
                             r=[t_qT, t_STb[h]], w=[tps[0]])
            def mk_filler(h, dc, kt, t_kt, ve, t_ve, gm, t_gm, pg, c):
                def f():
                    ub = 1
                    lhsT = kt[:, h * 512 + dc * 128:h * 512 + (dc + 1) * 128]
                    k.mm(ps[ub], lhsT, ve[:, h, 0:512], True, True, r=[t_kt, t_ve], w=[tps[ub]])
                    k.mm(ps[0][:, 16 + h * 4 + dc:17 + h * 4 + dc], lhsT, ve[:, h, 512:513], True, True,
                         r=[t_kt, t_ve], w=[tps[0]])
                    if c == 0:
                        k.cp("dve", X[:, h * 4 + dc, 0:512], ps[ub], r=[tps[ub]], w=[t_X[h]])
                    else:
                        k.stt("dve", X[:, h * 4 + dc, 0:512], X[:, h * 4 + dc, 0:512], pg[0][:, 2, h:h + 1], ps[ub],
                              ALU.mult, ALU.add, r=[tps[ub], pg[1], t_X[h]], w=[t_X[h]])
                    if c < NT - 1:
                        k.act(STb[:, h * 4 + dc, 0:512], X[:, h * 4 + dc, 0:512], AF.Copy, r=[t_X[h], t_gm], w=[t_STb[h]],
                              scale=gm[:, 2, h:h + 1])
                    if dc == 3:
                        xn = X[:, h * 4:(h + 1) * 4, 512:513].rearrange("p a b -> p (a b)")
                        if c == 0:
                            k.cp("dve", xn, ps[0][:, 16 + h * 4:20 + h * 4], r=[tps[0]], w=[t_X[h]])
                        else:
                            k.stt("dve", xn, xn, pg[0][:, 2, h:h + 1], ps[0][:, 16 + h * 4:20 + h * 4],
                                  ALU.mult, ALU.add, r=[tps[0], pg[1], t_X[h]], w=[t_X[h]])
                        if c < NT - 1:
                            sn = STb[:, h * 4:(h + 1) * 4, 512:513].rearrange("p a b -> p (a b)")
                            k.act(sn, xn, AF.Copy, r=[t_X[h], t_gm], w=[t_STb[h]], scale=gm[:, 2, h:h + 1])
                return f

            fillers = [mk_filler(h, dc, kt, t_kt, ve, t_ve, gm, t_gm, prev_gm, c) for h in range(NH) for dc in range(4)]
            if pend_epi is not None:
                epilogue(pend_epi, fillers)
                pend_epi = None
            else:
                for f in fillers:
                    f()
            prev_gm = (gm, t_gm)
            sm, t_sm = sm_.next()
            mv, t_mv = mv_.next()
            for h in range(NH):
                bs, t_bs = bs_.next()
                k.P.op("dve", (lambda o, i: (lambda e: e.bn_stats(o, i)))(bs, ps[2 + h]), r=[tps[2 + h]], w=[t_bs])
                k.P.op("dve", (lambda o, i: (lambda e: e.bn_aggr(o, i)))(mv[:, h, :], bs), r=[t_bs], w=[t_mv])
            k.tt("dve", sm[:, 0, :], ps[0][:, 8:12], gm[:, 1, :], ALU.mult, r=[tps[0], t_gm], w=[t_sm])
            k.stt("dve", sm[:, 1, :], sm[:, 0, :], -1.0, sm[:, 0, :], ALU.mult, ALU.max, r=[t_sm], w=[t_sm])
            k.ts("dve", sm[:, 1, :], sm[:, 1, :], 1.0, None, ALU.max, None, r=[t_sm], w=[t_sm])
            k.recip(sm[:, 2, :], sm[:, 1, :], r=[t_sm], w=[t_sm])
            k.tt("dve", sm[:, 3, :], gm[:, 1, :], sm[:, 2, :], ALU.mult, r=[t_sm, t_gm], w=[t_sm])
            k.tt("dve", sm[:, 4, :], sm[:, 3, :], sm[:, 3, :], ALU.mult, r=[t_sm], w=[t_sm])
            k.tt("dve", sm[:, 4, :], sm[:, 4, :], mv[:, :, 1], ALU.mult, r=[t_sm, t_mv], w=[t_sm])
            k.act(sm[:, 5, :], sm[:, 4, :], AF.Ln, r=[t_sm], w=[t_sm], bias=k.c_eps[:, 0:1])
            k.act(sm[:, 5, :], sm[:, 5, :], AF.Exp, r=[t_sm], w=[t_sm], scale=-0.5)
            k.tt("dve", sm[:, 6, :], sm[:, 5, :], sm[:, 3, :], ALU.mult, r=[t_sm], w=[t_sm])
            k.stt("dve", sm[:, 7, :], mv[:, :, 0], -1.0, sm[:, 6, :], ALU.mult, ALU.mult, r=[t_sm, t_mv], w=[t_sm])
            hn, t_hn = hn_.next()
            for h in range(NH):
                k.act(hn[:, h * 512:(h + 1) * 512], ps[2 + h], AF.Identity, r=[tps[2 + h], t_sm], w=[t_hn],
                      scale=sm[:, 6, h:h + 1], bias=sm[:, 7, h:h + 1])
            pend_epi = (hn, t_hn, sx, t_sx, sz, t_sz, xr, t_xr, s, r0)
        epilogue(pend_epi)
    P.barrier()


NSA_FM = [(0, 1024), (1024, 1280), (1280, 1536), (1536, 1792), (2048, 2304)]
SCALE = 0.125


def nsa_phase_a(k, li, x_src, T):
    nc, P = k.nc, k.P
    ps, tps = k.ps, k.tps
    with ExitStack() as es:
        def A(name, shape, dt):
            return es.enter_context(nc.sbuf_tensor(f"nA{li}_{name}", shape, dt)).ap()
        w_in = A("win", [128, 8, 3648], BF16)
        t_win = [Tok() for _ in range(8)]
        gb = A("gb", [128, 1024], F32)
        t_gb = Tok()
        bg = A("bg", [128, 48], F32)
        t_bg = Tok()
        rot = A("rot", [128, 128], BF16)
        cosf = A("cosf", [128, S], F32)
        sinf = A("sinf", [128, S], F32)
        t_c = Tok()
        stg = Ring([A(f"stg{i}", [128, 1816], F32) for i in range(2)])
        xr_ = Ring([A(f"xt{i}", [128, 1024], F32) for i in range(2)])
        hb_ = Ring([A(f"hb{i}", [128, 1024], BF16) for i in range(2)])
        junk = A("junk", [128, 1024], BF16)
        t_junk = Tok()
        st_ = Ring([A(f"st{i}", [128, 4], F32) for i in range(2)])
        hT = A("hT", [128, 8, 512], BF16)
        t_hT = Tok()
        xb_ = Ring([A(f"xb{i}", [128, 512], BF16) for i in range(3)])
        ta_ = Ring([A(f"ta{i}", [128, 512], F32) for i in range(2)])
        tb_ = Ring([A(f"tb{i}", [128, 512], F32) for i in range(2)])
        ro_ = Ring([A(f"ro{i}", [128, 512], BF16) for i in range(3)])
        tm_ = Ring([A(f"tm{i}", [128, 1536], BF16) for i in range(2)])
        gl_ = Ring([A(f"gl{i}", [128, 48], F32) for i in range(2)])

        for kc in range(8):
            for hf in range(2):
                s_ap, s_t = stg.next()
                k.ld(s_ap, k.nsa_w_in[li, kc * 128:(kc + 1) * 128, hf * 1816:(hf + 1) * 1816], w=[s_t])
                k.cp(("dve", "act")[hf], w_in[:, kc, hf * 1816:(hf + 1) * 1816], s_ap, r=[s_t], w=[t_win[kc]])
        k.ld(gb, k.nsa_norm[li].partition_broadcast(128), w=[t_gb])
        k.ld(bg, k.nsa_b_gate[li].partition_broadcast(128), w=[t_bg])
        s_ap, s_t = stg.next()
        k.ld(s_ap[:, 0:128], k.c_rot, w=[s_t])
        k.cp("dve", rot, s_ap[:, 0:128], r=[s_t], w=[t_c])
        k.ld(cosf, k.c_cos, w=[t_c])
        k.ld(sinf, k.c_sin, w=[t_c])

        fm = []
        for c8 in range(8):
            fm.append((c8 * 128, "qT", c8 * 128, True))
        for c2 in range(2):
            fm.append((1024 + c2 * 128, "kcT", c2 * 128, False))
            fm.append((1280 + c2 * 128, "vcT", c2 * 128, False))
            fm.append((1536 + c2 * 128, "ksT", c2 * 128, True))
            fm.append((2048 + c2 * 128, "kwT", c2 * 128, True))

        for s in range(k.nseq):
            for st in range(4):
                t0 = st * 512
                for sub in range(4):
                    r0 = t0 + sub * 128
                    xt, t_xt = xr_.next()
                    k.ld(xt, x_src[s, r0:r0 + 128, :], w=[t_xt])
                    sv, t_sv = st_.next()
                    k.act(junk, xt, AF.Square, r=[t_xt], w=[t_junk, t_sv], accum=sv[:, 0:1])
                    k.act(sv[:, 1:2], sv[:, 0:1], AF.Ln, r=[t_sv], w=[t_sv], scale=1.0 / D, bias=k.c_eps[:, 0:1])
                    k.act(sv[:, 2:3], sv[:, 1:2], AF.Exp, r=[t_sv], w=[t_sv], scale=-0.5)
                    hb, t_hb = hb_.next()
                    k.stt("dve", hb, xt, sv[:, 2:3], gb, ALU.mult, ALU.mult, r=[t_xt, t_sv, t_gb], w=[t_hb])
                    pv = ps[5].bitcast(BF16).rearrange("p (a t) -> p a t", a=8)
                    for kc in range(8):
                        k.tr(pv[:, kc, :], hb[:, kc * 128:(kc + 1) * 128], k.ident_bf, r=[t_hb], w=[tps[5]])
                    k.cp("dve", hT[:, :, sub * 128:(sub + 1) * 128], pv, r=[tps[5]], w=[t_hT])
                for i, (c0, dst, d0, rope) in enumerate(fm):
                    bi = (0, 1, 4)[i % 3]
                    for kc in range(8):
                        k.mm(ps[bi], w_in[:, kc, c0:c0 + 128], hT[:, kc, :], kc == 0, kc == 7,
                             r=[t_win[kc], t_hT], w=[tps[bi]])
                    xb, t_xb = xb_.next()
                    k.cp("act", xb, ps[bi], r=[tps[bi]], w=[t_xb])
                    if dst == "qT" or not rope:
                        k.st(T[dst][s, d0:d0 + 128, t0:t0 + 512], xb, r=[t_xb])
                    if rope:
                        pb = 2 + (i % 2)
                        k.mm(ps[pb], rot, xb, True, True, r=[t_c, t_xb], w=[tps[pb]])
                        ta, t_ta = ta_.next()
                        tb, t_tb = tb_.next()
                        k.tt("dve", ta, ps[bi], cosf[:, t0:t0 + 512], ALU.mult, r=[tps[bi], t_c, t_xb], w=[t_ta])
                        k.tt("dve", tb, ps[pb], sinf[:, t0:t0 + 512], ALU.mult, r=[tps[pb], t_c], w=[t_tb])
                        ro, t_ro = ro_.next()
                        k.tt("dve", ro, ta, tb, ALU.add, r=[t_ta, t_tb], w=[t_ro])
                        rd = "qrT" if dst == "qT" else dst
                        k.st(T[rd][s, d0:d0 + 128, t0:t0 + 512], ro, r=[t_ro])
                for sub in range(4):
                    r0 = t0 + sub * 128
                    tm, t_tm = tm_.next()
                    lh = [hT[:, kc, sub * 128:(sub + 1) * 128] for kc in range(8)]
                    for half, c0 in enumerate((1792, 2304)):
                        for kc in range(8):
                            k.mm(ps[6][:, half * 256:(half + 1) * 256], lh[kc], w_in[:, kc, c0:c0 + 256], kc == 0, kc == 7,
                                 r=[t_win[kc], t_hT], w=[tps[6]])
                    k.cp("dve", tm[:, 0:512], ps[6], r=[tps[6]], w=[t_tm])
                    for zi in range(2):
                        pb = 7 if zi == 0 else 6
                        c0 = 2560 + zi * 512
                        for kc in range(8):
                            k.mm(ps[pb], lh[kc], w_in[:, kc, c0:c0 + 512], kc == 0, kc == 7,
                                 r=[t_win[kc], t_hT], w=[tps[pb]])
                        k.act(tm[:, 512 + zi * 512:1024 + zi * 512], ps[pb], AF.Silu, r=[tps[pb]], w=[t_tm])
                    for kc in range(8):
                        k.mm(ps[7][:, 0:48], lh[kc], w_in[:, kc, 3584:3632], kc == 0, kc == 7,
                             r=[t_win[kc], t_hT], w=[tps[7]])
                    gl, t_gl = gl_.next()
                    k.tt("dve", gl, ps[7][:, 0:48], bg, ALU.add, r=[tps[7], t_bg], w=[t_gl])
                    k.act(gl, gl, AF.Sigmoid, r=[t_gl], w=[t_gl])
                    k.st(T["vsw"][s, r0:r0 + 128, :], tm[:, 0:512], r=[t_tm])
                    k.st(T["szt"][s, r0:r0 + 128, :], tm[:, 512:1536], r=[t_tm])
                    k.st(T["gate"][s, r0:r0 + 128, :], gl, r=[t_gl])
    P.barrier()


def nsa_phase_c(k, li, x_src, x_dst, T):
    nc, P = k.nc, k.P
    ps, tps = k.ps, k.tps
    with ExitStack() as es0:
        def A0(name, shape, dt):
            return es0.enter_context(nc.sbuf_tensor(f"nC{li}_{name}", shape, dt)).ap()
        w_out = A0("wout", [128, 8, 1024], BF16)
        t_wout = Tok()
        kcmpT = A0("kcmpT", [128, k.nseq, 4, 128], BF16)
        vcmp = A0("vcmp", [128, k.nseq, 4, 128], BF16)
        t_cmp = Tok()
        with ExitStack() as es:
            def A(name, shape, dt):
                return es.enter_context(nc.sbuf_tensor(f"nB{li}_{name}", shape, dt)).ap()
            stg = Ring([A(f"stg{i}", [128, 2048], F32) for i in range(2)])
            w1 = A("w1", [128, 2, 32, 128], BF16)
            w2k = A("w2k", [128, 128], BF16)
            w2v = A("w2v", [128, 64], BF16)
            peT = A("peT", [128, 2, 32], BF16)
            bh = A("bh", [128, 2], F32)
            t_w = Tok()
            kc2 = A("kc2", [128, 2, S], BF16)
            vc2 = A("vc2", [128, 2, S], BF16)
            t_kv = Tok()
            hTs_ = Ring([A(f"hTs{i}", [128, 128], BF16) for i in range(2)])
            for c8 in range(0, 8, 2):
                s_ap, s_t = stg.next()
                sv = s_ap.rearrange("p (c n) -> p c n", c=2)
                k.ld(sv, k.nsa_w_out[li, c8 * 128:(c8 + 2) * 128, :].rearrange("(c p) n -> p c n", p=128), w=[s_t])
                k.cp("dve", w_out[:, c8:c8 + 2, :], sv, r=[s_t], w=[t_wout])
            for kv in range(2):
                for hf in range(2):
                    s_ap, s_t = stg.next()
                    sv = s_ap.rearrange("p (l n) -> p l n", l=16)
                    src = k.nsa_cmp_w1[li, kv, hf * 1024:(hf + 1) * 1024, :].rearrange("(l d) n -> d l n", d=64)
                    k.ld(sv[0:64], src, w=[s_t])
                    k.ld(sv[64:128], src, w=[s_t])
                    k.cp("dve", w1[:, kv, hf * 16:(hf + 1) * 16, :], sv, r=[s_t], w=[t_w])
            s_ap, s_t = stg.next()
            k.ld(s_ap[:, 0:64], k.nsa_cmp_w2[li, 0], w=[s_t])
            k.ld(s_ap[:, 64:128], k.nsa_cmp_w2[li, 1], w=[s_t])
            pe_v = s_ap[:, 128:192].rearrange("p (a l) -> p a l", a=2)
            k.ld(pe_v[0:64], k.nsa_peT[li].rearrange("a d l -> d a l"), w=[s_t])
            k.ld(pe_v[64:128], k.nsa_peT[li].rearrange("a d l -> d a l"), w=[s_t])
            k.cp("dve", w2k[:, 0:64], s_ap[:, 0:64], r=[s_t], w=[t_w])
            k.cp("dve", w2k[:, 64:128], s_ap[:, 0:64], r=[s_t], w=[t_w])
            k.cp("dve", w2v, s_ap[:, 64:128], r=[s_t], w=[t_w])
            k.cp("dve", peT, pe_v, r=[s_t], w=[t_w])
            for kv in range(2):
                for l in range(32):
                    k.mm(ps[7][:, kv:kv + 1], w1[0:64, kv, l, :], peT[0:64, kv, l:l + 1], l == 0, l == 31, r=[t_w], w=[tps[7]])
            k.cp("dve", bh, ps[7][:, 0:2], r=[tps[7]], w=[t_w])
            for s in range(k.nseq):
                k.ld(vcmp[:, s, :, 64:97], k.c_ovl.rearrange("p (g n) -> p g n", g=4), w=[t_cmp])
                k.ld(kc2, T["kcT"][s].rearrange("(c p) t -> p c t", p=128), w=[t_kv])
                k.ld(vc2, T["vcT"][s].rearrange("(c p) t -> p c t", p=128), w=[t_kv])
                for kv in range(2):
                    src = kc2 if kv == 0 else vc2
                    for g in range(4):
                        base = (g % 2) * 64
                        for l in range(32):
                            k.mm(ps[6][:, 0:127], w1[base:base + 64, kv, l, :], src[base:base + 64, g // 2, l:l + 16 * 126 + 1:16],
                                 l == 0, l == 31, r=[t_w, t_kv], w=[tps[6]])
                        hTs, t_hTs = hTs_.next()
                        k.act(hTs[:, 0:127], ps[6][:, 0:127], AF.Silu, r=[tps[6], t_w], w=[t_hTs], bias=bh[:, kv:kv + 1])
                        if kv == 0:
                            k.mm(ps[7][:, 0:127], w2k, hTs[:, 0:127], True, True, r=[t_w, t_hTs], w=[tps[7]])
                            k.cp("dve", kcmpT[:, s, g, 0:127], ps[7][:, 0:127], r=[tps[7]], w=[t_cmp])
                        else:
                            k.mm(ps[7][0:127, 0:64], hTs[:, 0:127], w2v, True, True, r=[t_w, t_hTs], w=[tps[7]])
                            k.cp("dve", vcmp[0:127, s, g, 0:64], ps[7][0:127, 0:64], r=[tps[7]], w=[t_cmp])
        P.barrier()
        with ExitStack() as es:
            def A(name, shape, dt):
                return es.enter_context(nc.sbuf_tensor(f"nC{li}_{name}", shape, dt)).ap()
            t_c = Tok()
            Ef = A("Ef", [128, 4, S], BF16)
            cmpn = A("cmpn", [128, S], BF16)
            fmul = A("fmul", [128, 16, 32], F32)
            fadd = A("fadd", [128, 16, 32], F32)
            ks2 = A("ks2", [128, 4, S], BF16)
            kw2 = A("kw2", [128, 4, S], BF16)
            vs = A("vs", [128, 16, 4, 65], BF16)
            vw = A("vw", [128, 16, 4, 65], BF16)
            t_res = Tok()
            q_ = Ring([A(f"q{i}", [128, 8, 512], BF16) for i in range(1)])
            qr_ = Ring([A(f"qr{i}", [128, 8, 512], BF16) for i in range(1)])
            ex_ = Ring([A(f"ex{i}", [128, 512], BF16) for i in range(18)])
            oacc = A("oacc", [128, 4, 16, 64], F32)
            t_oacc = [Tok() for _ in range(4)]
            ucmp = A("ucmp", [128, 4, 4, 33], F32)
            rdc = A("rdc", [128, 4, 4], F32)
            t_ucmp = Tok()
            tmp_ = Ring([A(f"tmp{i}", [128, 4, 64], F32) for i in range(3)])
            imp_ = Ring([A(f"imp{i}", [128, 32], F32) for i in range(2)])
            rk_ = Ring([A(f"rk{i}", [128, 32, 32], F32) for i in range(2)])
            rs_ = Ring([A(f"rs{i}", [128, 32], F32) for i in range(2)])
            rd_ = Ring([A(f"rd{i}", [128, 16], F32) for i in range(4)])
            negm = A("negm", [128, 4, 4, 32], BF16)
            t_negm = [Tok() for _ in range(4)]
            negT = A("negT", [128, 512], BF16)
            t_negT = Tok()
            gat_ = Ring([A(f"gat{i}", [128, 4, 48], F32) for i in range(2)])
            szt_ = Ring([A(f"szt{i}", [128, 1024], BF16) for i in range(2)])
            xr_ = Ring([A(f"xr{i}", [128, 1024], F32) for i in range(2)])
            og_ = Ring([A(f"og{i}", [128, 1024], BF16) for i in range(2)])
            ogT_ = Ring([A(f"ogT{i}", [128, 8, 128], BF16) for i in range(2)])
            yo_ = Ring([A(f"yo{i}", [128, 1024], F32) for i in range(2)])

            k.ld(Ef, k.c_E.rearrange("p (g n) -> p g n", g=4), w=[t_c])
            k.ld(cmpn, k.c_cmpn, w=[t_c])
            k.ld(fmul, k.c_fmul, w=[t_c])
            k.ld(fadd, k.c_fadd, w=[t_c])
            k.memset("pool", vs[:, :, :, 64:65], 1.0, w=[t_res])
            k.memset("pool", vw[:, :, :, 64:65], 1.0, w=[t_res])

            def evac(pov, h, b, first, gat, t_gat, clamp, g):
                rd, t_rd = rd_.next()
                if clamp:
                    k.ts("dve", rd[:, 0:4], pov[:, :, 64], 1e-30, None, ALU.max, None, r=[tps_of[0]], w=[t_rd])
                else:
                    k.cp("dve", rd[:, 0:4], pov[:, :, 64], r=[tps_of[0]], w=[t_rd])
                k.recip(rd[:, 4:8], rd[:, 0:4], r=[t_rd], w=[t_rd])
                k.tt("dve", rd[:, 8:12], rd[:, 4:8], gat[:, :, 3 * h + b], ALU.mult, r=[t_rd, t_gat], w=[t_rd])
                if first:
                    k.tt("dve", oacc[:, :, h, :], pov[:, :, 0:64], bcast_last(rd[:, 8:12], 64), ALU.mult,
                         r=[tps_of[0], t_rd], w=[t_oacc[g]])
                else:
                    for sub in range(4):
                        k.stt("dve", oacc[:, sub, h, :], pov[:, sub, 0:64], rd[:, 8 + sub:9 + sub], oacc[:, sub, h, :],
                              ALU.mult, ALU.add, r=[tps_of[0], t_rd, t_oacc[g]], w=[t_oacc[g]])
                return rd, t_rd

            tps_of = [None]
            for s in range(k.nseq):
                for hf in range(2):
                    k.ld(ks2[hf * 64:(hf + 1) * 64], T["ksT"][s].rearrange("(g d) t -> d g t", d=64), w=[t_res])
                    k.ld(kw2[hf * 64:(hf + 1) * 64], T["kwT"][s].rearrange("(g d) t -> d g t", d=64), w=[t_res])
                vsw_v = T["vsw"][s].rearrange("(kt p) (b g d) -> p kt b g d", p=128, b=2, g=4)
                for kt4 in range(0, 16, 4):
                    for g in range(4):
                        k.ld(vs[:, kt4:kt4 + 4, g, 0:64], vsw_v[:, kt4:kt4 + 4, 0, g], w=[t_res])
                        k.ld(vw[:, kt4:kt4 + 4, g, 0:64], vsw_v[:, kt4:kt4 + 4, 1, g], w=[t_res])
                for qt in range(4):
                    t0 = qt * 512
                    q, t_q = q_.next()
                    qr, t_qr = qr_.next()
                    gat, t_gat = gat_.next()
                    k.ld(q, T["qT"][s, :, t0:t0 + 512].rearrange("(c p) t -> p c t", p=128), w=[t_q])
                    k.ld(qr, T["qrT"][s, :, t0:t0 + 512].rearrange("(c p) t -> p c t", p=128), w=[t_qr])
                    k.ld(gat, T["gate"][s, t0:t0 + 512, :].rearrange("(a p) n -> p a n", p=128), w=[t_gat])
                    need_sel = qt >= 2
                    for g in range(4):
                        for hh in range(4):
                            h = 4 * g + hh
                            ch, base = h // 2, (h % 2) * 64
                            sb = h % 2
                            k.mm(ps[sb][0:127, :], kcmpT[base:base + 64, s, g, 0:127], q[base:base + 64, ch, :], True, False,
                                 r=[t_cmp, t_q], w=[tps[sb]])
                            k.mm(ps[sb][0:127, :], k.ident_bf[0:127, 0:127], cmpn[0:127, t0:t0 + 512], False, True,
                                 r=[t_c], w=[tps[sb]])
                            ex, t_ex = ex_.next()
                            k.act(ex[0:127, :], ps[sb][0:127, :], AF.Exp, r=[tps[sb]], w=[t_ex], scale=SCALE)
                            ob = 2 + (h % 2)
                            pov = ps[ob][:, 0:388].rearrange("p (a n) -> p a n", a=4)
                            for sub in range(4):
                                k.mm(pov[:, sub, :], ex[0:127, sub * 128:(sub + 1) * 128], vcmp[0:127, s, g, 0:97], True, True,
                                     r=[t_ex, t_cmp], w=[tps[ob]])
                            tps_of[0] = tps[ob]
                            rd, t_rd = evac(pov, h, 0, True, gat, t_gat, True, g)
                            if need_sel:
                                k.cp("dve", ucmp[:, :, hh, :], pov[:, :, 64:97], r=[tps[ob]], w=[t_ucmp])
                                k.cp("dve", rdc[:, :, hh], rd[:, 4:8], r=[t_rd], w=[t_ucmp])
                        if need_sel:
                            for sub in range(4):
                                imp, t_imp = imp_.next()
                                k.ts("dve", imp, ucmp[:, sub, 0, 1:33], rdc[:, sub, 0:1], None, ALU.mult, None,
                                     r=[t_ucmp], w=[t_imp])
                                for hh in range(1, 4):
                                    k.stt("dve", imp, ucmp[:, sub, hh, 1:33], rdc[:, sub, hh:hh + 1], imp, ALU.mult, ALU.add,
                                          r=[t_ucmp, t_imp], w=[t_imp])
                                tt_ = 4 * qt + sub
                                k.tt("dve", imp, imp, fmul[:, tt_, :], ALU.mult, r=[t_imp, t_c], w=[t_imp])
                                k.tt("dve", imp, imp, fadd[:, tt_, :], ALU.add, r=[t_imp], w=[t_imp])
                                rk, t_rk = rk_.next()
                                in0 = imp.unsqueeze(1).to_broadcast([128, 32, 32])
                                in1 = imp.unsqueeze(2).to_broadcast([128, 32, 32])
                                k.tt("dve", rk, in0, in1, ALU.is_gt, r=[t_imp], w=[t_rk])
                                rs, t_rs = rs_.next()
                                k.P.op("dve", (lambda o, i: (lambda e: e.reduce_sum(o, i, AX.X)))(rs, rk), r=[t_rk], w=[t_rs])
                                k.ts("dve", negm[:, sub, g, :], rs, 15.5, NEG, ALU.is_gt, ALU.mult, r=[t_rs], w=[t_negm[sub]])
                    if need_sel:
                        pv = ps[4].bitcast(BF16)[:, 0:512].rearrange("p (a t) -> p a t", a=4)
                        for sub in range(4):
                            k.tr(pv[:, sub, :], negm[:, sub, :, :].rearrange("p g n -> p (g n)"), k.ident_bf,
                                 r=[t_negm[sub]], w=[tps[4]])
                        k.cp("dve", negT, ps[4].bitcast(BF16)[:, 0:512], r=[tps[4]], w=[t_negT])
                    for br in range(2):
                        kk = ks2 if br == 0 else kw2
                        vv = vs if br == 0 else vw
                        kt_lo = 0 if br == 0 else max(0, 4 * qt - 4)
                        kt_hi = 4 * qt + 3
                        for g in range(4):
                            for hh in range(4):
                                h = 4 * g + hh
                                ch, base = h // 2, (h % 2) * 64
                                ob = 2 + (h % 2)
                                pov = ps[ob][:, 0:260].rearrange("p (a n) -> p a n", a=4)

                                def qk(kt):
                                    sb = (0, 1, 7)[kt % 3]
                                    r_ = kt - 4 * qt
                                    extra = []
                                    if br == 0 and need_sel:
                                        extra.append((Ef[:, g, kt * 128:(kt + 1) * 128], negT, [t_c, t_negT]))
                                    k.mm(ps[sb], kk[base:base + 64, g, kt * 128:(kt + 1) * 128], qr[base:base + 64, ch, :],
                                         True, len(extra) == 0, r=[t_res, t_qr], w=[tps[sb]])
                                    for ei, (lt, rh, rt) in enumerate(extra):
                                        k.mm(ps[sb], lt, rh, False, ei == len(extra) - 1, r=rt, w=[tps[sb]])
                                    ex, t_ex = ex_.next()
                                    k.act(ex, ps[sb], AF.Exp, r=[tps[sb]], w=[t_ex], scale=SCALE)
                                    for sub in range(4):
                                        blk = ex[:, sub * 128:(sub + 1) * 128]
                                        if kt == 4 * qt + sub:
                                            k.tt("dve", blk, blk, k.caus_bf, ALU.mult, r=[t_ex], w=[t_ex])
                                        elif br == 1 and kt == 4 * qt + sub - 4:
                                            k.tt("dve", blk, blk, k.low_bf, ALU.mult, r=[t_ex], w=[t_ex])
                                    return ex, t_ex

                                exs = {}
                                for kt in range(kt_lo, kt_hi + 1):
                                    exs[kt] = qk(kt)
                                for sub in range(4):
                                    hi_s = 4 * qt + sub
                                    lo_s = 0 if br == 0 else max(0, hi_s - 4)
                                    for kt in range(lo_s, hi_s + 1):
                                        ex, t_ex = exs[kt]
                                        k.mm(pov[:, sub, :], ex[:, sub * 128:(sub + 1) * 128], vv[:, kt, g, :],
                                             kt == lo_s, kt == hi_s, r=[t_ex, t_res], w=[tps[ob]])
                                tps_of[0] = tps[ob]
                                evac(pov, h, 1 + br, False, gat, t_gat, False, g)
                    for sub in range(4):
                        r0 = t0 + sub * 128
                        szt, t_szt = szt_.next()
                        xr, t_xr = xr_.next()
                        k.ld(szt, T["szt"][s, r0:r0 + 128, :], w=[t_szt])
                        k.ld(xr, x_src[s, r0:r0 + 128, :], w=[t_xr])
                        og, t_og = og_.next()
                        k.tt("dve", og, oacc[:, sub, :, :].rearrange("p h d -> p (h d)"), szt, ALU.mult,
                             r=t_oacc + [t_szt], w=[t_og])
                        pv = ps[4].bitcast(BF16).rearrange("p (a t) -> p a t", a=8)
                        for c8 in range(8):
                            k.tr(pv[:, c8, :], og[:, c8 * 128:(c8 + 1) * 128], k.ident_bf, r=[t_og], w=[tps[4]])
                        ogT, t_ogT = ogT_.next()
                        k.cp("act", ogT, pv, r=[tps[4]], w=[t_ogT])
                        yo, t_yo = yo_.next()
                        for half in range(2):
                            pb = 5 + half
                            for c8 in range(8):
                                k.mm(ps[pb], ogT[:, c8, :], w_out[:, c8, half * 512:(half + 1) * 512], c8 == 0, c8 == 7,
                                     r=[t_ogT, t_wout], w=[tps[pb]])
                            k.tt("dve", yo[:, half * 512:(half + 1) * 512], ps[pb], xr[:, half * 512:(half + 1) * 512], ALU.add,
                                 r=[tps[pb], t_xr], w=[t_yo])
                        k.st(x_dst[s, r0:r0 + 128, :], yo, r=[t_yo])
    P.barrier()


def build_program(nseq=NSEQ, layers=(0, 1, 2, 3), final_norm=True, limit=None):
    nc = bass.Bass("TRN2", target_bir_lowering=False)
    k = K(nc, nseq)
    k.P.limit = limit

    def din(name, shape, dt=F32):
        return nc.dram_tensor(name, list(shape), dt, kind="ExternalInput").ap()

    def dscr(name, shape, dt):
        return nc.dram_tensor(name, list(shape), dt, kind="Internal").ap()

    x = din("x", [nseq, S, D])
    k.ml_norm = din("ml_norm", [2, D])
    k.ml_w_in = din("ml_w_in", [2, D, 4096])
    k.ml_bd = din("ml_bd", [2, 48, 128, 128])
    k.ml_vec = din("ml_vec", [2, 128, 16, 8])
    k.ml_wif = din("ml_wif", [2, 128, 48, 8])
    k.ml_bif = din("ml_bif", [2, 8, 1])
    k.ml_w_out = din("ml_w_out", [2, ML_INNER, D])
    k.final_norm = din("final_norm", [1, D])
    k.nsa_norm = din("nsa_norm", [2, D])
    k.nsa_w_in = din("nsa_w_in", [2, D, 3632])
    k.nsa_b_gate = din("nsa_b_gate", [2, 48])
    k.nsa_peT = din("nsa_peT", [2, 2, 64, 32])
    k.nsa_cmp_w1 = din("nsa_cmp_w1", [2, 2, 2048, 128])
    k.nsa_cmp_w2 = din("nsa_cmp_w2", [2, 2, 128, 64])
    k.nsa_w_out = din("nsa_w_out", [2, D, D])
    k.c_rot = din("c_rot", [128, 128])
    k.c_cos = din("c_cos", [128, S])
    k.c_sin = din("c_sin", [128, S])
    k.c_E = din("c_E", [128, 4 * S], BF16)
    k.c_causn = din("c_causn", [128, 4 * 512], BF16)
    k.c_lown = din("c_lown", [128, 4 * 512], BF16)
    k.c_cmpn = din("c_cmpn", [128, S], BF16)
    k.c_ovl = din("c_ovl", [128, 4 * 33], BF16)
    k.c_fmul = din("c_fmul", [128, 16, 32])
    k.c_fadd = din("c_fadd", [128, 16, 32])
    c_ident = din("c_ident", [128, 128])
    c_caus = din("c_caus", [128, 128])
    y = nc.dram_tensor("y", [nseq, S, D], F32, kind="ExternalOutput").ap()
    xres = dscr("xres", [nseq, S, D], F32)
    T = {
        "qT": dscr("s_qT", [nseq, ML_INNER, S], BF16),
        "kT": dscr("s_kT", [nseq, ML_INNER, S], BF16),
        "sxT": dscr("s_sxT", [nseq, ML_INNER, S], BF16),
        "szT": dscr("s_szT", [nseq, ML_INNER, S], BF16),
        "ktok": dscr("s_ktok", [nseq, S, ML_INNER], BF16),
        "vtok": dscr("s_vtok", [nseq, S, ML_INNER], BF16),
        "gtok": dscr("s_gtok", [nseq, S, 16], F32),
    }
    TN = {
        "qT": dscr("n_qT", [nseq, 1024, S], BF16),
        "qrT": dscr("n_qrT", [nseq, 1024, S], BF16),
        "kcT": dscr("n_kcT", [nseq, 256, S], BF16),
        "vcT": dscr("n_vcT", [nseq, 256, S], BF16),
        "ksT": dscr("n_ksT", [nseq, 256, S], BF16),
        "kwT": dscr("n_kwT", [nseq, 256, S], BF16),
        "vsw": dscr("n_vsw", [nseq, S, 512], BF16),
        "szt": dscr("n_szt", [nseq, S, 1024], BF16),
        "gate": dscr("n_gate", [nseq, S, 48], F32),
    }

    def C(name, shape, dt):
        return nc.alloc_sbuf_tensor(name, shape, dt).ap()
    k.ident_f = C("ident_f", [128, 128], F32)
    k.ident_bf = C("ident_bf", [128, 128], BF16)
    k.caus_f = C("caus_f", [128, 128], F32)
    k.ones_f = C("ones_f", [128, 128], F32)
    k.caus_bf = C("caus_bf", [128, 128], BF16)
    k.low_bf = C("low_bf", [128, 128], BF16)
    k.c_eps = C("c_eps", [128, 1], F32)
    k.c_one = C("c_one", [128, 1], F32)
    k.ps = [nc.alloc_psum_tensor(f"ps{i}", [128, 512], F32).ap() for i in range(8)]
    k.tps = [Tok(f"ps{i}", x=True) for i in range(8)]
    tc = Tok()
    k.ld(k.ident_f, c_ident, w=[tc])
    k.ld(k.caus_f, c_caus, w=[tc])
    k.cp("dve", k.ident_bf, k.ident_f, r=[tc], w=[tc])
    k.memset("dve", k.ones_f, 1.0, w=[tc])
    k.cp("dve", k.caus_bf, k.caus_f, r=[tc], w=[tc])
    k.ts("dve", k.low_bf, k.caus_f, -1.0, 1.0, ALU.mult, ALU.add, r=[tc], w=[tc])
    k.memset("dve", k.c_eps, RMS_EPS, w=[tc])
    k.memset("dve", k.c_one, 1.0, w=[tc])
    k.P.barrier()

    cur = x
    for L in layers:
        li = L // 2
        if L % 2 == 0:
            ml_phase_a(k, li, cur, T)
            ml_phase_b(k, li, cur, xres, T)
        else:
            nsa_phase_a(k, li, cur, TN)
            nsa_phase_c(k, li, cur, xres, TN)
        cur = xres

    print("n_ins before final", k.P.n_ins, flush=True)
    k.P.limit = None
    with ExitStack() as es:
        def A(name, shape, dt):
            return es.enter_context(nc.sbuf_tensor(f"fin_{name}", shape, dt)).ap()
        gb = A("gb", [128, D], F32)
        t_gb = Tok()
        k.ld(gb, k.final_norm[0].partition_broadcast(128), w=[t_gb])
        xr_ = Ring([A(f"x{i}", [128, D], F32) for i in range(3)])
        junk = A("junk", [128, D], F32)
        t_junk = Tok()
        st_ = Ring([A(f"st{i}", [128, 4], F32) for i in range(3)])
        t_y = Tok()
        for s in range(nseq):
            for c in range(NT):
                r0 = c * 128
                xt, t_xt = xr_.next()
                k.ld(xt, cur[s, r0:r0 + 128, :], w=[t_xt])
                if final_norm:
                    sv, t_sv = st_.next()
                    k.act(junk, xt, AF.Square, r=[t_xt], w=[t_junk, t_sv], accum=sv[:, 0:1])
                    k.act(sv[:, 1:2], sv[:, 0:1], AF.Ln, r=[t_sv], w=[t_sv], scale=1.0 / D, bias=k.c_eps[:, 0:1])
                    k.act(sv[:, 2:3], sv[:, 1:2], AF.Exp, r=[t_sv], w=[t_sv], scale=-0.5)
                    k.stt("dve", xt, xt, sv[:, 2:3], gb, ALU.mult, ALU.mult, r=[t_xt, t_sv, t_gb], w=[t_xt])
                k.P.dma("sp", y[s, r0:r0 + 128, :], xt, r=[t_xt], w=[t_y])
        k.P._emit_waits("sp", k.P._deps([t_y], [t_y]))
        deps = {kk: v for kk, v in k.P.cnt.items() if kk.startswith("d_sp") and v > 0}
        k.P._emit_waits("sp", deps)
    k.P.emit()
    return nc


def _block_diag(w):
    out = np.zeros((16, 128, 128), np.float32)
    w4 = w.reshape(16, 32, 4, 4)
    for n in range(32):
        out[:, 4 * n:4 * n + 4, 4 * n:4 * n + 4] = w4[:, n].transpose(0, 2, 1)
    return out


_CONSTS = {}


def _nsa_consts():
    if _CONSTS:
        return _CONSTS
    import ml_dtypes
    bf = ml_dtypes.bfloat16
    c = {}
    rot = np.zeros((128, 128), np.float32)
    for hb in (0, 64):
        for d in range(8):
            rot[hb + d + 8, hb + d] = -1.0
            rot[hb + d, hb + d + 8] = 1.0
    c["c_rot"] = rot
    pos = np.arange(S, dtype=np.float32)
    inv_freq = (np.float32(500000.0) ** (-np.arange(0, 16, 2, dtype=np.float32) / np.float32(16))).astype(np.float32)
    ang = (pos[:, None] * inv_freq[None, :]).astype(np.float32)
    cosf = np.ones((128, S), np.float32)
    sinf = np.zeros((128, S), np.float32)
    for hb in (0, 64):
        for d in range(16):
            cosf[hb + d] = np.cos(ang[:, d % 8])
            sinf[hb + d] = np.sin(ang[:, d % 8])
    c["c_cos"], c["c_sin"] = cosf, sinf
    E = np.zeros((128, 4, S), np.float32)
    key = np.arange(S)
    for g in range(4):
        E[g * 32 + key // 64, g, key] = 1.0
    c["c_E"] = E.reshape(128, 4 * S).astype(bf)
    kk = np.arange(128)[:, None]
    tt = np.arange(512)[None, :]
    causn = np.zeros((128, 4, 512), np.float32)
    lown = np.zeros((128, 4, 512), np.float32)
    for r in range(4):
        valid = kk <= tt - 128 * r
        causn[:, r, :] = np.where(valid, 0.0, NEG)
        lown[:, r, :] = np.where(valid, NEG, 0.0)
    c["c_causn"] = causn.reshape(128, 2048).astype(bf)
    c["c_lown"] = lown.reshape(128, 2048).astype(bf)
    cc = np.arange(128)[:, None]
    tok = np.arange(S)[None, :]
    c["c_cmpn"] = np.where(16 * cc + 31 <= tok, 0.0, NEG).astype(np.float32).astype(bf)
    cs = np.arange(127) * 16
    ss = np.arange(32) * 64
    ov = np.clip(np.minimum(cs[:, None] + 32, ss[None, :] + 64) - np.maximum(cs[:, None], ss[None, :]), 0, None) / 16.0
    ovl = np.zeros((128, 4, 33), np.float32)
    ovl[:, :, 0] = 1.0
    ovl[:127, :, 1:] = ov[:, None, :]
    c["c_ovl"] = ovl.reshape(128, 4 * 33).astype(bf)
    p = np.arange(S)
    blk = np.arange(32)
    dist = (p // 64)[:, None] - blk[None, :]
    forced = (blk[None, :] == 0) | ((dist >= 0) & (dist < 2))
    fmul = np.where(forced | (dist < 0), 0.0, 1.0).astype(np.float32)
    fadd = np.where(forced, 1e9, np.where(dist >= 0, 0.0, -1.0)).astype(np.float32)
    c["c_fmul"] = np.ascontiguousarray(fmul.reshape(16, 128, 32).transpose(1, 0, 2))
    c["c_fadd"] = np.ascontiguousarray(fadd.reshape(16, 128, 32).transpose(1, 0, 2))
    _CONSTS.update(c)
    return _CONSTS


def host_layout(inp):
    f = lambda a: np.ascontiguousarray(np.asarray(a, dtype=np.float32))
    d = {}
    d["ml_norm"] = f(inp["ml_norm"])
    d["ml_w_in"] = f(inp["ml_w_in"])
    bd = np.zeros((2, 48, 128, 128), np.float32)
    vec = np.zeros((2, 128, 16, 8), np.float32)
    for li in range(2):
        for g3, nm in enumerate(("ml_w_q", "ml_w_k", "ml_w_v")):
            bd[li, g3 * 16:(g3 + 1) * 16] = _block_diag(np.asarray(inp[nm][li], np.float32))
        cw = np.asarray(inp["ml_conv_w"][li], np.float32)
        for tap in range(4):
            vec[li, :, :, tap] = cw[tap].reshape(16, 128).T
        vec[li, :, :, 4] = np.asarray(inp["ml_conv_b"][li], np.float32).reshape(16, 128).T
        vec[li, :, :, 5] = np.asarray(inp["ml_ln_w"][li], np.float32).reshape(16, 128).T
        vec[li, :, :, 6] = np.asarray(inp["ml_skip"][li], np.float32).reshape(16, 128).T
    d["ml_bd"] = bd
    d["ml_vec"] = vec
    d["ml_wif"] = f(np.asarray(inp["ml_w_if"], np.float32).reshape(2, 48, 128, 8).transpose(0, 2, 1, 3))
    d["ml_bif"] = f(np.asarray(inp["ml_b_if"], np.float32).reshape(2, 8, 1))
    d["ml_w_out"] = f(inp["ml_w_out"])
    d["final_norm"] = f(np.asarray(inp["final_norm"], np.float32).reshape(1, D))
    d["nsa_norm"] = f(inp["nsa_norm"])
    d["nsa_w_in"] = f(inp["nsa_w_in"])
    d["nsa_b_gate"] = f(inp["nsa_b_gate"])
    d["nsa_peT"] = f(np.asarray(inp["nsa_cmp_pe"], np.float32).transpose(0, 1, 3, 2))
    d["nsa_cmp_w1"] = f(inp["nsa_cmp_w1"])
    d["nsa_cmp_w2"] = f(inp["nsa_cmp_w2"])
    d["nsa_w_out"] = f(inp["nsa_w_out"])
    d.update(_nsa_consts())
    d["c_ident"] = np.eye(128, dtype=np.float32)
    d["c_caus"] = np.triu(np.ones((128, 128), np.float32))
    return d


_NC_CACHE = {}


def kernel(**inputs):
    x = np.asarray(inputs["x"], np.float32)
    shared = host_layout(inputs)
    if "nc" not in _NC_CACHE:
        _NC_CACHE["nc"] = build_program()
    nc = _NC_CACHE["nc"]
    in_maps = []
    for c in range(NCORES):
        m = dict(shared)
        m["x"] = np.ascontiguousarray(x[c * NSEQ:(c + 1) * NSEQ])
        in_maps.append(m)
    res = run_bass_kernel_spmd(nc, in_maps, core_ids=list(range(NCORES)))
    return np.concatenate([r["y"] for r in res.results], axis=0)
```
